# Optimizing a Trainium2 kernel written in Bass

```python
import math
import jax, jax.numpy as jnp
from jax import lax
import numpy as np

D_MODEL = 1024
BATCH = 4
SEQ = 4096
DEPTH = 4

N_MIXERS = 3
N_HEADS = 16
HEAD_DIM = 64
ATTN_SCALE = HEAD_DIM ** -0.5
BLOCK = 128
NUM_BUCKETS = 32
MAX_DISTANCE = 2048
RMS_EPS = 1e-6
NEG_INF = -1e30
FORCE_SCORE = 1e9
DIL_CONFIGS = ((128, 1), (512, 4), (2048, 16))
N_DIL = len(DIL_CONFIGS)
A_IN = N_DIL * 3 * N_HEADS * HEAD_DIM
B_KV_HEADS = 2
B_WINDOW = 128
B_IN = (N_HEADS + 2 * B_KV_HEADS) * HEAD_DIM
C_KV_HEADS = 2
CMP_BLOCK = 32
CMP_STRIDE = 16
SEL_BLOCK = 64
N_SELECT = 16
C_WINDOW = 512
CMP_HIDDEN = 256
C_IN = N_HEADS * HEAD_DIM + 6 * C_KV_HEADS * HEAD_DIM + 3 * N_HEADS
D_FF = 2816
CONV_WIDTH = 3
N_A = (DEPTH + 2) // 3
N_B = (DEPTH + 1) // 3
N_C = DEPTH // 3

kernel_name = 'hybrid_dilated_sink_nsa_trunk'


def rms_norm(x, g):
    xf = x.astype(jnp.float32)
    y = xf * lax.rsqrt(jnp.mean(xf * xf, axis=-1, keepdims=True) + RMS_EPS)
    return (y * g.astype(jnp.float32)).astype(x.dtype)


def t5_bucket(dist):
    max_exact = NUM_BUCKETS // 2
    d = jnp.maximum(dist, 0)
    df = jnp.maximum(d, 1).astype(jnp.float32)
    large = max_exact + (jnp.log(df / max_exact) / math.log(MAX_DISTANCE / max_exact)
                         * (NUM_BUCKETS - max_exact)).astype(jnp.int32)
    large = jnp.minimum(large, NUM_BUCKETS - 1)
    return jnp.where(d < max_exact, d, large)


def banded_attention(q, k, v, max_dist, stride, table, sinks=None, with_lse=False):
    N, L, H, D = q.shape
    G = k.shape[2]
    R = H // G
    nb = -(-L // BLOCK)
    Lp = nb * BLOCK
    pad = Lp - L
    n_prev = -(-max_dist // BLOCK)
    KB = (n_prev + 1) * BLOCK
    qb = jnp.pad(q, ((0, 0), (0, pad), (0, 0), (0, 0))).reshape(N, nb, BLOCK, G, R, D)

    def band(t):
        tp = jnp.pad(t, ((0, 0), (n_prev * BLOCK, pad), (0, 0), (0, 0)))
        return jnp.concatenate([tp[:, j * BLOCK:j * BLOCK + Lp].reshape(N, nb, BLOCK, G, D)
                                for j in range(n_prev + 1)], axis=2)

    kb, vb = band(k), band(v)
    dist = jnp.arange(BLOCK)[:, None] + n_prev * BLOCK - jnp.arange(KB)[None, :]
    key_pos = (jnp.arange(nb)[:, None, None] - n_prev) * BLOCK + jnp.arange(KB)[None, None, :]
    valid = (dist >= 0) & (dist <= max_dist) & (key_pos >= 0)
    bias = table[t5_bucket(dist * stride)].astype(jnp.float32).transpose(2, 0, 1).reshape(G, R, BLOCK, KB)
    logits = jnp.einsum('nbqgrd,nbkgd->nbgrqk', qb, kb).astype(jnp.float32) * ATTN_SCALE + bias
    logits = jnp.where(valid[None, :, None, None], logits, NEG_INF)
    m = jnp.max(logits, axis=-1, keepdims=True)
    if sinks is not None:
        s = sinks.astype(jnp.float32).reshape(1, 1, G, R, 1, 1)
        m = jnp.maximum(m, s)
    p = jnp.exp(logits - m)
    den = jnp.sum(p, axis=-1, keepdims=True)
    norm = den if sinks is None else den + jnp.exp(s - m)
    out = jnp.einsum('nbgrqk,nbkgd->nbqgrd', (p / norm).astype(v.dtype), vb).reshape(N, Lp, H, D)[:, :L]
    if not with_lse:
        return out
    lse = (m + jnp.log(den))[..., 0].transpose(0, 1, 4, 2, 3).reshape(N, Lp, H)[:, :L]
    return out, lse


def dilated_attention(h, w_in, w_o, table):
    B, S, _ = h.shape
    qkv = (h @ w_in).reshape(B, S, N_DIL, 3, N_HEADS, HEAD_DIM)
    outs, lses = [], []
    for g, (window, dil) in enumerate(DIL_CONFIGS):
        L = S // dil

        def to_strided(t):
            return t.reshape(B, L, dil, N_HEADS, HEAD_DIM).transpose(0, 2, 1, 3, 4).reshape(B * dil, L, N_HEADS, HEAD_DIM)

        q, k, v = (to_strided(qkv[:, :, g, i]) for i in range(3))
        o, lse = banded_attention(q, k, v, window // dil, dil, table, with_lse=True)
        outs.append(o.reshape(B, dil, L, N_HEADS, HEAD_DIM).transpose(0, 2, 1, 3, 4).reshape(B, S, N_HEADS, HEAD_DIM))
        lses.append(lse.reshape(B, dil, L, N_HEADS).transpose(0, 2, 1, 3).reshape(B, S, N_HEADS))
    wts = jax.nn.softmax(jnp.stack(lses, axis=0), axis=0)
    o = outs[0] * wts[0, ..., None].astype(outs[0].dtype)
    for g in range(1, N_DIL):
        o = o + outs[g] * wts[g, ..., None].astype(outs[g].dtype)
    return o.reshape(B, S, N_HEADS * HEAD_DIM) @ w_o


def sink_window_attention(h, w_in, sinks, w_o, table):
    B, S, _ = h.shape
    HQ, HK = N_HEADS * HEAD_DIM, B_KV_HEADS * HEAD_DIM
    qkv = h @ w_in
    q = qkv[..., :HQ].reshape(B, S, N_HEADS, HEAD_DIM)
    k = qkv[..., HQ:HQ + HK].reshape(B, S, B_KV_HEADS, HEAD_DIM)
    v = qkv[..., HQ + HK:].reshape(B, S, B_KV_HEADS, HEAD_DIM)
    o = banded_attention(q, k, v, B_WINDOW - 1, 1, table, sinks=sinks)
    return o.reshape(B, S, HQ) @ w_o


def nsa_attention(h, w_in, cmp_pos, cmp_w1, cmp_w2, w_o, table):
    B, S, _ = h.shape
    G, R = C_KV_HEADS, N_HEADS // C_KV_HEADS
    HQ, HK = N_HEADS * HEAD_DIM, C_KV_HEADS * HEAD_DIM
    proj = h @ w_in
    q = proj[..., :HQ].reshape(B, S, G, R, HEAD_DIM)
    kv = proj[..., HQ:HQ + 6 * HK].reshape(B, S, 6, G, HEAD_DIM)
    gates = jax.nn.sigmoid(proj[..., HQ + 6 * HK:].reshape(B, S, 3, N_HEADS, 1))
    k_c, v_c, k_s, v_s, k_w, v_w = (kv[:, :, i] for i in range(6))
    pos = jnp.arange(S)

    nc = (S - CMP_BLOCK) // CMP_STRIDE + 1
    starts = jnp.arange(nc) * CMP_STRIDE
    idx = starts[:, None] + jnp.arange(CMP_BLOCK)[None, :]

    def compress(t, i):
        blocks = t[:, idx] + cmp_pos[i][None, None, :, None, :]
        flat = blocks.transpose(0, 1, 3, 2, 4).reshape(B, nc, G, CMP_BLOCK * HEAD_DIM)
        return jax.nn.gelu(flat @ cmp_w1[i], approximate=True) @ cmp_w2[i]

    kc, vc = compress(k_c, 0), compress(v_c, 1)
    logits_c = jnp.einsum('bsgrd,bcgd->bgrsc', q, kc).astype(jnp.float32) * ATTN_SCALE
    valid_c = (starts + CMP_BLOCK - 1)[None, :] <= pos[:, None]
    p_c = jax.nn.softmax(jnp.where(valid_c, logits_c, NEG_INF), axis=-1) \
        * jnp.any(valid_c, axis=-1)[:, None].astype(jnp.float32)
    o_c = jnp.einsum('bgrsc,bcgd->bsgrd', p_c.astype(vc.dtype), vc).reshape(B, S, N_HEADS, HEAD_DIM)

    ns = S // SEL_BLOCK
    k_sel = min(N_SELECT, ns)
    blk = jnp.arange(ns)
    overlap = ((starts[:, None] < (blk[None, :] + 1) * SEL_BLOCK)
               & (starts[:, None] + CMP_BLOCK > blk[None, :] * SEL_BLOCK)).astype(jnp.float32)
    imp = jnp.einsum('bgrsc,cj->bsgj', p_c, overlap)
    cur = (pos // SEL_BLOCK)[:, None]
    forced = ((blk[None, :] == 0) | (blk[None, :] == cur) | (blk[None, :] == cur - 1))[:, None, :]
    allowed = (blk[None, :] <= cur)[:, None, :]
    score = jnp.where(forced, FORCE_SCORE, jnp.where(allowed, imp, NEG_INF))
    sel = lax.top_k(score, k_sel)[1]

    nqb = S // BLOCK
    q_blocks = q.reshape(B, nqb, BLOCK, G, R, HEAD_DIM).transpose(1, 0, 2, 3, 4, 5)
    sel_blocks = sel.reshape(B, nqb, BLOCK, G, k_sel).transpose(1, 0, 2, 3, 4)
    pos_blocks = pos.reshape(nqb, BLOCK)
    b_ix = jnp.arange(B)[:, None, None, None]
    g_ix = jnp.arange(G)[None, None, :, None]
    table_g = table.reshape(NUM_BUCKETS, G, R)

    def selected_block(args):
        qb, sb, pb = args
        kpos = (sb[..., None] * SEL_BLOCK + jnp.arange(SEL_BLOCK)).reshape(B, BLOCK, G, k_sel * SEL_BLOCK)
        kg = k_s[b_ix, kpos, g_ix]
        vg = v_s[b_ix, kpos, g_ix]
        dist = pb[None, :, None, None] - kpos
        bias = table_g[t5_bucket(dist), g_ix].astype(jnp.float32).transpose(0, 2, 4, 1, 3)
        lg = jnp.einsum('bqgrd,bqgkd->bgrqk', qb, kg).astype(jnp.float32) * ATTN_SCALE + bias
        lg = jnp.where((dist >= 0).transpose(0, 2, 1, 3)[:, :, None], lg, NEG_INF)
        p = jax.nn.softmax(lg, axis=-1)
        return jnp.einsum('bgrqk,bqgkd->bqgrd', p.astype(vg.dtype), vg)

    o_s = lax.map(selected_block, (q_blocks, sel_blocks, pos_blocks))
    o_s = o_s.transpose(1, 0, 2, 3, 4, 5).reshape(B, S, N_HEADS, HEAD_DIM)

    o_w = banded_attention(q.reshape(B, S, N_HEADS, HEAD_DIM), k_w, v_w, C_WINDOW - 1, 1, table)

    o = gates[:, :, 0] * o_c + gates[:, :, 1] * o_s + gates[:, :, 2] * o_w
    return o.reshape(B, S, HQ) @ w_o


def conv_ffn(h, w_up, conv_w, conv_b, w_down):
    S = h.shape[1]
    u = h @ w_up
    up = jnp.pad(u, ((0, 0), (CONV_WIDTH - 1, 0), (0, 0)))
    c = conv_b + up[:, CONV_WIDTH - 1:] * conv_w[CONV_WIDTH - 1]
    for j in range(CONV_WIDTH - 1):
        c = c + up[:, j:j + S] * conv_w[j]
    gate, val = jnp.split(c, 2, axis=-1)
    return (jax.nn.gelu(gate, approximate=True) * val) @ w_down


def setup_inputs(seed: int = 0) -> dict:
    key = jax.random.key(seed)
    ks = jax.random.split(key, 17)
    HQ = N_HEADS * HEAD_DIM

    def nrm(k, shape, scale):
        return jax.random.normal(k, shape, jnp.float32) * scale

    return {
        'x': nrm(ks[0], (BATCH, SEQ, D_MODEL), 1.0),
        'rel_table': nrm(ks[1], (NUM_BUCKETS, N_HEADS), 0.3),
        'norm_gains': 1.0 + nrm(ks[2], (DEPTH, 4, D_MODEL), 0.05),
        'a_w_in': nrm(ks[3], (N_A, D_MODEL, A_IN), D_MODEL ** -0.5),
        'a_w_o': nrm(ks[4], (N_A, HQ, D_MODEL), HQ ** -0.5),
        'b_w_in': nrm(ks[5], (N_B, D_MODEL, B_IN), D_MODEL ** -0.5),
        'b_sinks': nrm(ks[6], (N_B, N_HEADS), 0.5),
        'b_w_o': nrm(ks[7], (N_B, HQ, D_MODEL), HQ ** -0.5),
        'c_w_in': nrm(ks[8], (N_C, D_MODEL, C_IN), D_MODEL ** -0.5),
        'c_cmp_pos': nrm(ks[9], (N_C, 2, CMP_BLOCK, HEAD_DIM), 0.1),
        'c_cmp_w1': nrm(ks[10], (N_C, 2, CMP_BLOCK * HEAD_DIM, CMP_HIDDEN), (CMP_BLOCK * HEAD_DIM) ** -0.5),
        'c_cmp_w2': nrm(ks[11], (N_C, 2, CMP_HIDDEN, HEAD_DIM), CMP_HIDDEN ** -0.5),
        'c_w_o': nrm(ks[12], (N_C, HQ, D_MODEL), HQ ** -0.5),
        'ffn_w_up': nrm(ks[13], (DEPTH, D_MODEL, 2 * D_FF), D_MODEL ** -0.5),
        'ffn_conv_w': nrm(ks[14], (DEPTH, CONV_WIDTH, 2 * D_FF), 0.5),
        'ffn_conv_b': nrm(ks[15], (DEPTH, 2 * D_FF), 0.01),
        'ffn_w_down': nrm(ks[16], (DEPTH, D_FF, D_MODEL), D_FF ** -0.5),
    }


def reference(x, rel_table, norm_gains, a_w_in, a_w_o, b_w_in, b_sinks, b_w_o,
              c_w_in, c_cmp_pos, c_cmp_w1, c_cmp_w2, c_w_o,
              ffn_w_up, ffn_conv_w, ffn_conv_b, ffn_w_down):
    h = x
    for i in range(DEPTH):
        kind, j = i % N_MIXERS, i // N_MIXERS
        g = norm_gains[i]
        y = rms_norm(h, g[0])
        if kind == 0:
            y = dilated_attention(y, a_w_in[j], a_w_o[j], rel_table)
        elif kind == 1:
            y = sink_window_attention(y, b_w_in[j], b_sinks[j], b_w_o[j], rel_table)
        else:
            y = nsa_attention(y, c_w_in[j], c_cmp_pos[j], c_cmp_w1[j], c_cmp_w2[j], c_w_o[j], rel_table)
        h = h + rms_norm(y, g[1])
        y = conv_ffn(rms_norm(h, g[2]), ffn_w_up[i], ffn_conv_w[i], ffn_conv_b[i], ffn_w_down[i])
        h = h + rms_norm(y, g[3])
    return h
```

```python
import contextlib
import numpy as np
import concourse.bass as bass
import concourse.mybir as mybir
from concourse.bass_utils import run_bass_kernel_spmd

F32 = mybir.dt.float32
BF16 = mybir.dt.bfloat16
AF = mybir.ActivationFunctionType
ALU = mybir.AluOpType
AX = mybir.AxisListType

SAME_ENGINE_SYNC = True
SEM_PAGE = 30000


class Prog:
    ENGS = ('pe', 'act', 'dve', 'pool', 'sp')

    def __init__(self, nc):
        self.nc = nc
        self.q = {e: [] for e in self.ENGS}
        self.lastw = {}
        self.readers = {}
        self.dma_cnt = {}
        self.seen = {e: {} for e in self.ENGS}
        self.needed = {e: set() for e in self.ENGS}
        self.final_events = []

    def _deps(self, eng, reads, writes):
        deps = []
        for k in reads:
            w = self.lastw.get(k)
            if w is not None:
                deps.append(w)
        for k in writes:
            w = self.lastw.get(k)
            if w is not None:
                deps.append(w)
            deps.extend(self.readers.get(k, ()))
        waits = []
        seen = self.seen[eng]
        for ev in deps:
            if ev[0] == 'eng':
                _, e2, j = ev
                if e2 == eng and (eng == 'pe' or not SAME_ENGINE_SYNC):
                    continue
                if seen.get(('eng', e2), -1) >= j:
                    continue
                seen[('eng', e2)] = j
                self.needed[e2].add(j)
                waits.append(ev)
            else:
                _, slot, cnt = ev
                if seen.get(('dma', slot), -1) >= cnt:
                    continue
                seen[('dma', slot)] = cnt
                waits.append(ev)
        best = {}
        for ev in waits:
            src = ev[:2]
            if src not in best or best[src][2] < ev[2]:
                best[src] = ev
        return list(best.values())

    def _commit(self, ev, reads, writes):
        for k in reads:
            self.readers.setdefault(k, []).append(ev)
        for k in writes:
            self.lastw[k] = ev
            self.readers[k] = []

    def op(self, eng, fn, reads=(), writes=()):
        waits = self._deps(eng, reads, writes)
        idx = len(self.q[eng])
        self.q[eng].append(dict(fn=fn, waits=waits, dma=None))
        ev = ('eng', eng, idx)
        self._commit(ev, reads, writes)
        return ev

    def dma(self, eng, out, in_, reads=(), writes=(), slot=None, **kw):
        waits = self._deps(eng, reads, writes)
        if slot is None:
            slot = writes[0]
        cnt = self.dma_cnt.get(slot, 0) + 16
        self.dma_cnt[slot] = cnt
        fn = (lambda e, out=out, in_=in_, kw=kw: e.dma_start(out=out, in_=in_, **kw))
        self.q[eng].append(dict(fn=fn, waits=waits, dma=slot))
        ev = ('dma', slot, cnt)
        self._commit(ev, reads, writes)
        return ev

    def finish_on(self, eng, events):
        events = list(events)
        for ev in events:
            if ev[0] == 'eng':
                self.needed[ev[1]].add(ev[2])
        self.final_events.append((eng, events))

    def emit(self):
        nc = self.nc
        import contextlib
        count_at = {}
        npages = {}
        for e in self.ENGS:
            c = 0
            for j in range(len(self.q[e])):
                if j in self.needed[e]:
                    c += 1
                    count_at[(e, j)] = c
            npages[e] = (c + SEM_PAGE - 1) // SEM_PAGE
        for eng, evs in self.final_events:
            pass
        with contextlib.ExitStack() as st:
            esems = {e: [st.enter_context(nc.semaphore(f"s_{e}_{i}")) for i in range(npages[e])]
                     for e in self.ENGS}
            dsems = {}
            for i, slot in enumerate(self.dma_cnt):
                dsems[slot] = st.enter_context(nc.semaphore(f"d_{i}"))
            self.n_sems = sum(npages.values()) + len(dsems)
            block = st.enter_context(nc.Block())

            def resolve(ev):
                if ev[0] == 'eng':
                    c = count_at[(ev[1], ev[2])]
                    return esems[ev[1]][(c - 1) // SEM_PAGE], (c - 1) % SEM_PAGE + 1
                return dsems[ev[1]], ev[2]

            def run(ename):
                def body(eobj):
                    for j, o in enumerate(self.q[ename]):
                        for ev in o['waits']:
                            s, v = resolve(ev)
                            eobj.wait_ge(s, v)
                        ins = o['fn'](eobj)
                        if o['dma'] is not None:
                            ins.then_inc(dsems[o['dma']], 16)
                        elif (ename, j) in count_at:
                            c = count_at[(ename, j)]
                            ins.then_inc(esems[ename][(c - 1) // SEM_PAGE], 1)
                    for eng, evs in self.final_events:
                        if eng == ename:
                            for ev in evs:
                                s, v = resolve(ev)
                                eobj.wait_ge(s, v)
                return body

            block.tensor(run('pe'))
            block.scalar(run('act'))
            block.vector(run('dve'))
            block.gpsimd(run('pool'))
            block.sync(run('sp'))


D = 1024
DFF = 2816
NT = 2048
TG = 512
NTG = NT // TG
KC = D // 128
NJ = DFF // 128
EPS = 1e-6


def emit_prenorm(p, nc, hT, xT, gcol, ncols, sq, rstd, ones_bf, ps_stat, epsc, xoff=0):
    nq = 0
    t0 = 0
    while t0 < ncols:
        n = min(TG, ncols - t0)
        for c in range(KC):
            s = sq[nq % 3]
            sk = s.name
            nq += 1
            p.op('act', lambda e, s=s, c=c, t0=t0, n=n: e.activation(out=s[:, 0:n], in_=hT[:, c, t0:t0 + n], func=AF.Square),
                 reads=[(hT.name, t0 // TG)], writes=[sk])
            p.op('pe', lambda e, s=s, c=c, n=n: e.matmul(ps_stat[:, 0:n], lhsT=ones_bf[:], rhs=s[:, 0:n], start=(c == 0), stop=(c == KC - 1)),
                 reads=[sk, 'ones'], writes=['ps_stat'])
        p.op('act', lambda e, n=n: e.activation(out=rstd[:, 0:n], in_=ps_stat[:, 0:n], func=AF.Sqrt, scale=1.0 / D, bias=epsc[:, 0:1]),
             reads=['ps_stat', 'epsc'], writes=[rstd.name])
        p.op('dve', lambda e, n=n: e.reciprocal(out=rstd[:, 0:n], in_=rstd[:, 0:n]),
             reads=[rstd.name], writes=[rstd.name])
        for c in range(KC):
            p.op('dve', lambda e, c=c, t0=t0, n=n: e.scalar_tensor_tensor(out=xT[:, c, xoff + t0:xoff + t0 + n], in0=hT[:, c, t0:t0 + n], scalar=gcol[:, c:c + 1], in1=rstd[:, 0:n], op0=ALU.mult, op1=ALU.mult),
                 reads=[(hT.name, t0 // TG), rstd.name, 'gains'], writes=[(xT.name, (xoff + t0) // TG)])
        t0 += n


def emit_postnorm_residual(p, nc, hT, ysb, gcol, tg, ones_bf, ps_stat, sqs, rstd, tmp, sqctr, epsc):
    for c in range(KC):
        s = sqs[sqctr[0] % len(sqs)]
        sk = s.name
        sqctr[0] += 1
        p.op('act', lambda e, s=s, c=c: e.activation(out=s[:], in_=ysb[:, c, :], func=AF.Square),
             reads=[('ysb', c)], writes=[sk])
        p.op('pe', lambda e, s=s, c=c: e.matmul(ps_stat[:], lhsT=ones_bf[:], rhs=s[:], start=(c == 0), stop=(c == KC - 1)),
             reads=[sk, 'ones'], writes=['ps_stat'])
    p.op('act', lambda e: e.activation(out=rstd[:], in_=ps_stat[:], func=AF.Sqrt, scale=1.0 / D, bias=epsc[:, 0:1]),
         reads=['ps_stat', 'epsc'], writes=[rstd.name])
    p.op('dve', lambda e: e.reciprocal(out=rstd[:], in_=rstd[:]),
         reads=[rstd.name], writes=[rstd.name])
    for c in range(KC):
        p.op('dve', lambda e, c=c: e.scalar_tensor_tensor(out=tmp[:], in0=ysb[:, c, :], scalar=gcol[:, c:c + 1], in1=rstd[:], op0=ALU.mult, op1=ALU.mult),
             reads=[('ysb', c), rstd.name, 'gains'], writes=[tmp.name])
        p.op('dve', lambda e, c=c: e.tensor_tensor(out=hT[:, c, tg * TG:(tg + 1) * TG], in0=hT[:, c, tg * TG:(tg + 1) * TG], in1=tmp[:], op=ALU.add),
             reads=[tmp.name, (hT.name, tg)], writes=[(hT.name, tg)])


def build_ffn():
    nc = bass.Bass("TRN2", target_bir_lowering=False)
    hT_d = nc.dram_tensor("hT", [KC, 128, NT], F32, kind="ExternalInput").ap()
    halo_d = nc.dram_tensor("halo", [KC, 128, 2], F32, kind="ExternalInput").ap()
    g_d = nc.dram_tensor("gains", [128, 2 * KC], F32, kind="ExternalInput").ap()
    wup_d = nc.dram_tensor("w_up", [D, 2 * DFF], F32, kind="ExternalInput").ap()
    cw_d = nc.dram_tensor("conv_wb", [128, 2 * NJ, 4], F32, kind="ExternalInput").ap()
    wdn_d = nc.dram_tensor("w_down", [DFF, D], F32, kind="ExternalInput").ap()
    out_d = nc.dram_tensor("out", [KC, 128, NT], F32, kind="ExternalOutput").ap()

    with contextlib.ExitStack() as st:
        sb = lambda name, shape, dt: st.enter_context(nc.sbuf_tensor(name, shape, dt))
        ps = lambda name, shape, dt: st.enter_context(nc.psum_tensor(name, shape, dt))
        hT = sb("hT_sb", [128, KC, NT], F32)
        hh = sb("hh_sb", [128, KC, 2], F32)
        xT = sb("xT_sb", [128, KC, NT], BF16)
        xh = sb("xh_sb", [128, KC, 2], BF16)
        gains = sb("gains_sb", [128, 2 * KC], F32)
        cw = sb("cw_sb", [128, 2 * NJ, 4], F32)
        ones_bf = sb("ones_bf", [128, 128], BF16)
        carry = sb("carry", [128, 2 * NJ, 2], F32)
        epsc = sb("epsc", [128, 1], F32)
        NWB = 4
        wu = [sb(f"wu{i}", [128, KC, 256], BF16) for i in range(NWB)]
        NDB = 3
        wd = [sb(f"wd{i}", [128, NJ, 128], BF16) for i in range(NDB)]
        ubuf = [sb(f"ubuf{i}", [128, TG + 2], F32) for i in range(4)]
        cbuf = [sb(f"cbuf{i}", [128, TG], F32) for i in range(4)]
        gbuf = [sb(f"gbuf{i}", [128, TG], F32) for i in range(2)]
        gT = sb("gT", [128, NJ, TG], BF16)
        ysb = sb("ysb", [128, KC, TG], F32)
        sqs = [sb(f"sqp{i}", [128, TG], BF16) for i in range(3)]
        rstd2 = sb("rstd2", [128, TG], F32)
        tmp = sb("tmpn", [128, TG], F32)
        ps_stat = ps("ps_stat", [128, TG], F32)
        ps_u = [ps(f"ps_u{i}", [128, TG], F32) for i in range(4)]
        ps_y = [ps(f"ps_y{i}", [128, TG], F32) for i in range(2)]
        ps_c = ps("ps_c", [128, 2 * NJ * 2], F32)

        p = Prog(nc)
        for c in range(KC):
            for tg in range(NTG):
                p.dma('sp', hT[:, c, tg * TG:(tg + 1) * TG], hT_d[c, :, tg * TG:(tg + 1) * TG], writes=[(hT.name, tg)], slot=f"ld_h{tg}")
        p.dma('sp', hh[:], halo_d.rearrange("c p t -> p c t"), writes=[(hh.name, 0)])
        p.dma('sp', gains[:], g_d[:, :], writes=['gains'])
        p.dma('sp', cw[:], cw_d[:, :, :], writes=['cw'])
        p.op('dve', lambda e: e.memset(ones_bf[:], 1.0), writes=['ones'])
        p.op('dve', lambda e: e.memset(epsc[:], EPS), writes=['epsc'])
        emit_prenorm(p, nc, hh, xh, gains[:, 0:KC], 2, sqs, rstd2, ones_bf, ps_stat, epsc)
        emit_prenorm(p, nc, hT, xT, gains[:, 0:KC], NT, sqs, rstd2, ones_bf, ps_stat, epsc)

        wu_n = [0]

        def load_wu(j):
            i = wu_n[0] % NWB
            wu_n[0] += 1
            w = wu[i]
            for half, col0 in ((0, j * 128), (1, DFF + j * 128)):
                p.dma('pool', w[:, :, half * 128:(half + 1) * 128],
                      wup_d[:, col0:col0 + 128].rearrange("(kc p) n -> p kc n", p=128),
                      writes=[w.name], slot=w.name)
            return w

        wd_n = [0]

        def load_wd(c):
            i = wd_n[0] % NDB
            wd_n[0] += 1
            w = wd[i]
            p.dma('pool', w[:], wdn_d[:, c * 128:(c + 1) * 128].rearrange("(j p) n -> p j n", p=128), writes=[w.name])
            return w

        for j in range(NJ):
            w = load_wu(j)
            for half in range(2):
                jg = half * NJ + j
                for kc in range(KC):
                    p.op('pe', lambda e, w=w, half=half, kc=kc, jg=jg: e.matmul(ps_c[:, jg * 2:jg * 2 + 2], lhsT=w[:, kc, half * 128:(half + 1) * 128], rhs=xh[:, kc, :], start=(kc == 0), stop=(kc == KC - 1)),
                         reads=[w.name, (xh.name, 0)], writes=['ps_c'])
        p.op('dve', lambda e: e.tensor_copy(out=carry[:].rearrange("p a b -> p (a b)"), in_=ps_c[:]), reads=['ps_c'], writes=['carry'])

        un = [0]
        sqctr = [0]
        for tg in range(NTG):
            tsl = slice(tg * TG, (tg + 1) * TG)
            for j in range(NJ):
                w = load_wu(j)
                cres = []
                for half in range(2):
                    jg = half * NJ + j
                    k = un[0] % 4
                    un[0] += 1
                    pu, ub, cb = ps_u[k], ubuf[k], cbuf[k]
                    for kc in range(KC):
                        p.op('pe', lambda e, w=w, half=half, kc=kc, pu=pu, tsl=tsl: e.matmul(pu[:], lhsT=w[:, kc, half * 128:(half + 1) * 128], rhs=xT[:, kc, tsl], start=(kc == 0), stop=(kc == KC - 1)),
                             reads=[w.name, (xT.name, tg)], writes=[pu.name])
                    p.op('dve', lambda e, ub=ub, jg=jg: e.tensor_copy(out=ub[:, 0:2], in_=carry[:, jg, :]), reads=['carry'], writes=[ub.name])
                    p.op('act', lambda e, ub=ub, pu=pu: e.activation(out=ub[:, 2:TG + 2], in_=pu[:], func=AF.Copy), reads=[pu.name], writes=[ub.name + "m"])
                    p.op('act', lambda e, cb=cb, pu=pu, jg=jg: e.activation(out=cb[:], in_=pu[:], func=AF.Identity, scale=cw[:, jg, 2:3], bias=cw[:, jg, 3:4]),
                         reads=[pu.name, 'cw'], writes=[cb.name])
                    p.op('dve', lambda e, cb=cb, ub=ub, jg=jg: e.scalar_tensor_tensor(out=cb[:], in0=ub[:, 1:TG + 1], scalar=cw[:, jg, 1:2], in1=cb[:], op0=ALU.mult, op1=ALU.add),
                         reads=[ub.name, ub.name + "m", cb.name, 'cw'], writes=[cb.name])
                    p.op('dve', lambda e, cb=cb, ub=ub, jg=jg: e.scalar_tensor_tensor(out=cb[:], in0=ub[:, 0:TG], scalar=cw[:, jg, 0:1], in1=cb[:], op0=ALU.mult, op1=ALU.add),
                         reads=[ub.name, ub.name + "m", cb.name, 'cw'], writes=[cb.name])
                    p.op('dve', lambda e, ub=ub, jg=jg: e.tensor_copy(out=carry[:, jg, :], in_=ub[:, TG:TG + 2]), reads=[ub.name + "m"], writes=['carry'])
                    cres.append(cb)
                gb = gbuf[j % 2]
                p.op('act', lambda e, gb=gb, cb=cres[0]: e.activation(out=gb[:], in_=cb[:], func=AF.Gelu_apprx_tanh), reads=[cres[0].name], writes=[gb.name])
                p.op('dve', lambda e, gb=gb, cb=cres[1], j=j: e.tensor_tensor(out=gT[:, j, :], in0=gb[:], in1=cb[:], op=ALU.mult),
                     reads=[gb.name, cres[1].name], writes=[('gT', j)])
            for c in range(KC):
                w = load_wd(c)
                py = ps_y[c % 2]
                for j in range(NJ):
                    p.op('pe', lambda e, w=w, j=j, py=py: e.matmul(py[:], lhsT=w[:, j, :], rhs=gT[:, j, :], start=(j == 0), stop=(j == NJ - 1)),
                         reads=[w.name, ('gT', j)], writes=[py.name])
                p.op('act', lambda e, c=c, py=py: e.activation(out=ysb[:, c, :], in_=py[:], func=AF.Copy), reads=[py.name], writes=[('ysb', c)])
            emit_postnorm_residual(p, nc, hT, ysb, gains[:, KC:2 * KC], tg, ones_bf, ps_stat, sqs, rstd2, tmp, sqctr, epsc)
        evs = []
        for c in range(KC):
            for tg in range(NTG):
                evs.append(p.dma('sp', out_d[c, :, tg * TG:(tg + 1) * TG], hT[:, c, tg * TG:(tg + 1) * TG], reads=[(hT.name, tg)], writes=[('out', c, tg)], slot="st_out"))
        p.finish_on('sp', [('dma', 'st_out', p.dma_cnt['st_out'])])
        p.emit()
    return nc


MASKV = -30000.0
DEBUG = {}
SCALE = 0.125
NQ = 2048
NQT = 16


def build_lin(nco):
    NOC = nco // 128
    nc = bass.Bass("TRN2", target_bir_lowering=False)
    hT_d = nc.dram_tensor("hT", [KC, 128, NT], F32, kind="ExternalInput").ap()
    g_d = nc.dram_tensor("gains", [128, KC], F32, kind="ExternalInput").ap()
    w_d = nc.dram_tensor("w", [D, nco], F32, kind="ExternalInput").ap()
    out_d = nc.dram_tensor("out", [NOC, 128, NT], BF16, kind="ExternalOutput").ap()
    with contextlib.ExitStack() as st:
        sb = lambda name, shape, dt: st.enter_context(nc.sbuf_tensor(name, shape, dt))
        ps = lambda name, shape, dt: st.enter_context(nc.psum_tensor(name, shape, dt))
        hT = sb("hT_sb", [128, KC, NT], F32)
        xT = sb("xT_sb", [128, KC, NT], BF16)
        gains = sb("gains_sb", [128, KC], F32)
        ones_bf = sb("ones_bf", [128, 128], BF16)
        epsc = sb("epsc", [128, 1], F32)
        sqs = [sb(f"sqp{i}", [128, TG], BF16) for i in range(3)]
        rstd = sb("rstd", [128, TG], F32)
        wb = [sb(f"wb{i}", [128, KC, 128], BF16) for i in range(4)]
        ob = [sb(f"ob{i}", [128, NT], BF16) for i in range(3)]
        ps_stat = ps("ps_stat", [128, TG], F32)
        ps_o = [ps(f"ps_o{i}", [128, TG], F32) for i in range(4)]
        p = Prog(nc)
        for c in range(KC):
            for tg in range(NTG):
                p.dma('sp', hT[:, c, tg * TG:(tg + 1) * TG], hT_d[c, :, tg * TG:(tg + 1) * TG], writes=[(hT.name, tg)], slot=f"ld_h{tg}")
        p.dma('sp', gains[:], g_d[:, :], writes=['gains'])
        p.op('dve', lambda e: e.memset(ones_bf[:], 1.0), writes=['ones'])
        p.op('dve', lambda e: e.memset(epsc[:], EPS), writes=['epsc'])
        emit_prenorm(p, nc, hT, xT, gains[:, 0:KC], NT, sqs, rstd, ones_bf, ps_stat, epsc)
        n = 0
        evs = []
        for oc in range(NOC):
            w = wb[oc % 4]
            p.dma('pool', w[:], w_d[:, oc * 128:(oc + 1) * 128].rearrange("(kc p) n -> p kc n", p=128), writes=[w.name])
            o = ob[oc % 3]
            for tg in range(NTG):
                po = ps_o[n % 4]
                n += 1
                for kc in range(KC):
                    p.op('pe', lambda e, w=w, kc=kc, po=po, tg=tg: e.matmul(po[:], lhsT=w[:, kc, :], rhs=xT[:, kc, tg * TG:(tg + 1) * TG], start=(kc == 0), stop=(kc == KC - 1)),
                         reads=[w.name, (xT.name, tg)], writes=[po.name])
                eng = 'act' if n % 2 else 'dve'
                if eng == 'act':
                    p.op('act', lambda e, o=o, po=po, tg=tg: e.activation(out=o[:, tg * TG:(tg + 1) * TG], in_=po[:], func=AF.Copy), reads=[po.name], writes=[(o.name, tg)])
                else:
                    p.op('dve', lambda e, o=o, po=po, tg=tg: e.tensor_copy(out=o[:, tg * TG:(tg + 1) * TG], in_=po[:]), reads=[po.name], writes=[(o.name, tg)])
            evs.append(p.dma('sp', out_d[oc, :, :], o[:], reads=[(o.name, tg) for tg in range(NTG)], writes=[(o.name + "st")], slot="st_" + o.name))
            for tg in range(NTG):
                p.readers.setdefault((o.name, tg), []).append(evs[-1])
        p.finish_on('sp', [('dma', "st_" + ob[i].name, p.dma_cnt["st_" + ob[i].name]) for i in range(3) if ("st_" + ob[i].name) in p.dma_cnt])
        p.emit()
    return nc


def build_bias(ltot):
    nc = bass.Bass("TRN2", target_bir_lowering=False)
    t_d = nc.dram_tensor("table", [32, 16], F32, kind="ExternalInput").ap()
    oh_d = nc.dram_tensor("onehot", [33, ltot], F32, kind="ExternalInput").ap()
    out_d = nc.dram_tensor("out", [16, ltot], F32, kind="ExternalOutput").ap()
    nchk = (ltot + 511) // 512
    with contextlib.ExitStack() as st:
        sb = lambda name, shape, dt: st.enter_context(nc.sbuf_tensor(name, shape, dt))
        ps = lambda name, shape, dt: st.enter_context(nc.psum_tensor(name, shape, dt))
        tf = sb("tf", [64, 16], F32)
        tb = sb("tb", [64, 16], BF16)
        oh = sb("oh", [64, ltot], BF16)
        fo = sb("fo", [16, ltot], F32)
        pp = [ps(f"pp{i}", [16, 512], F32) for i in range(2)]
        p = Prog(nc)
        p.op('dve', lambda e: e.memset(tf[32:64, :], MASKV), writes=['tfm'])
        p.dma('sp', tf[0:32, :], t_d[:, :], writes=['tf'])
        p.dma('pool', oh[0:33, :], oh_d[:, :], writes=['oh'])
        p.op('dve', lambda e: e.tensor_scalar(out=tb[0:32, :], in0=tf[0:32, :], scalar1=1.0 / SCALE, scalar2=None, op0=ALU.mult), reads=['tf'], writes=['tb'])
        p.op('dve', lambda e: e.tensor_copy(out=tb[32:64, :], in_=tf[32:64, :]), reads=['tfm'], writes=['tbm'])
        for k in range(nchk):
            n = min(512, ltot - k * 512)
            q = pp[k % 2]
            p.op('pe', lambda e, k=k, n=n, q=q: e.matmul(q[:, 0:n], lhsT=tb[0:33, :], rhs=oh[0:33, k * 512:k * 512 + n], start=True, stop=True),
                 reads=['tb', 'tbm', 'oh'], writes=[q.name])
            p.op('act', lambda e, k=k, n=n, q=q: e.activation(out=fo[:, k * 512:k * 512 + n], in_=q[:, 0:n], func=AF.Copy), reads=[q.name], writes=['fo'])
        ev = p.dma('sp', out_d[:, :], fo[:], reads=['fo'], writes=['out'])
        p.finish_on('sp', [ev])
        p.emit()
    return nc


def build_att(cfg):
    npr, nkch, nkt, nkeys = cfg['npr'], cfg['nkch'], cfg['nkt'], cfg['nkeys']
    ltot, lc = cfg.get('ltot', 0), cfg.get('lc', 0)
    imp = cfg.get('imp', False)
    anymask = cfg.get('mask', False)
    nc = bass.Bass("TRN2", target_bir_lowering=False)
    q_d = nc.dram_tensor("QT", [KC, 128, NQ], BF16, kind="ExternalInput").ap() if cfg.get('sharedq', False) else None
    qs_d = None if q_d is not None else nc.dram_tensor("QT", [npr, KC, 128, NQ], BF16, kind="ExternalInput").ap()
    k_d = nc.dram_tensor("KT", [npr, nkch, 128, 2, nkeys], BF16, kind="ExternalInput").ap()
    v_d = nc.dram_tensor("V", [npr, nkch, 128, nkt, 128], BF16, kind="ExternalInput").ap()
    hf_d = nc.dram_tensor("hflag", [128, 128], BF16, kind="ExternalInput").ap()
    jm_d = nc.dram_tensor("jmat", [128, 128], BF16, kind="ExternalInput").ap()
    fv_d = nc.dram_tensor("fvec", [16, ltot], F32, kind="ExternalInput") if ltot else None
    cv_d = nc.dram_tensor("cvec", [2, lc], F32, kind="ExternalInput") if lc else None
    if anymask:
        e_d = nc.dram_tensor("emat", [64, nkt * 128], BF16, kind="ExternalInput").ap()
        nm_d = nc.dram_tensor("nmT", [2, 64, NQ], BF16, kind="ExternalInput").ap()
    if imp:
        ov_d = nc.dram_tensor("ov65", [128, 2, 65], BF16, kind="ExternalInput").ap()
        sbias_d = nc.dram_tensor("selbias", [128, NQT, 64], F32, kind="ExternalInput").ap()
        nmo_d = nc.dram_tensor("negmask", [128, NQT, 2, 64], BF16, kind="ExternalOutput").ap()
    u_d = nc.dram_tensor("U", [npr, KC, 128, NQT, 256], F32, kind="ExternalOutput").ap()
    d_d = nc.dram_tensor("Dn", [npr, KC, 128, NQT, 256], F32, kind="ExternalOutput").ap()
    maxb = max(len(b) for b in cfg['bias'])
    with contextlib.ExitStack() as st:
        sb = lambda name, shape, dt: st.enter_context(nc.sbuf_tensor(name, shape, dt))
        ps = lambda name, shape, dt: st.enter_context(nc.psum_tensor(name, shape, dt))
        qt = [sb(f"qt{i}", [128, NQ], BF16) for i in range(2)]
        kt = [sb(f"kt{i}", [128, 2, nkeys], BF16) for i in range(2)]
        vt = [sb(f"vt{i}", [128, nkt, 128], BF16) for i in range(2)]
        bt = [sb(f"bt{i}", [128, maxb, 256], BF16) for i in range(2)]
        hflag = sb("hflag_sb", [128, 128], BF16)
        ones1 = sb("ones1", [128, 128], BF16)
        jm = sb("jm_sb", [128, 128], BF16)
        pb = [sb(f"pb{i}", [128, 512], BF16) for i in range(4)]
        ub = [sb(f"ub{i}", [128, NQT, 256], F32) for i in range(2)]
        db = [sb(f"db{i}", [128, NQT, 256], F32) for i in range(2)]
        if anymask:
            em = sb("em_sb", [64, nkt * 128], BF16)
            nm = sb("nm_sb", [64, 2, NQ], BF16)
        if imp:
            ov = sb("ov_sb", [128, 2, 65], BF16)
            sbias = sb("sbias_sb", [128, NQT, 64], F32)
            impacc = sb("impacc", [128, NQT, 2, 64], F32)
            nmb = sb("nmb", [128, NQT, 2, 64], BF16)
            rden = sb("rden", [128, 2], F32)
            mx = sb("mx", [128, 16], F32)
            scr = sb("scr", [128, 64], F32)
            scr2 = sb("scr2", [128, 64], F32)
            ps_i = [ps(f"ps_i{i}", [128, 2, 65], F32) for i in range(2)]
        ps_s = [ps(f"ps_s{i}", [128, 512], F32) for i in range(2)]
        ps_a = [ps(f"ps_a{i}", [128, 256], F32) for i in range(2)]
        ps_d = [ps(f"ps_d{i}", [128, 256], F32) for i in range(2)]

        p = Prog(nc)
        p.dma('sp', hflag[:], hf_d[:, :], writes=['hflag'])
        p.dma('sp', jm[:], jm_d[:, :], writes=['jm'])
        p.op('dve', lambda e: e.memset(ones1[:], 1.0), writes=['ones1'])
        if anymask:
            p.dma('sp', em[:], e_d[:, :], writes=['em'])
            p.dma('sp', nm[:], nm_d.rearrange("g j q -> j g q"), writes=['nm'])
        if imp:
            p.dma('sp', ov[:], ov_d[:, :, :], writes=['ov'])
            p.dma('sp', sbias[:], sbias_d[:, :, :], writes=['sbias'])
            p.op('pool', lambda e: e.memset(impacc[:], 0.0), writes=['impacc'])
        cnt = dict(q=0, k=0, b=0, s=0, a=0, pb=0, u=0, i=0)
        lastk = [None, None]
        out_evs = []
        for pr in range(npr):
            blist = cfg['bias'][pr]
            plan = cfg['plans'][pr]
            for c in range(KC):
                Q = qt[cnt['q'] % 2]
                cnt['q'] += 1
                src = q_d[c, :, :] if q_d is not None else qs_d[pr, c, :, :]
                p.dma('sp', Q[:], src, writes=[Q.name])
                kc = cfg['kch'](c)
                if lastk[0] != (pr, kc):
                    Kt = kt[cnt['k'] % 2]
                    Vt = vt[cnt['k'] % 2]
                    cnt['k'] += 1
                    for h_ in range(2):
                        for k0_ in range(0, nkeys, 2048):
                            k1_ = min(nkeys, k0_ + 2048)
                            p.dma('sp', Kt[:, h_, k0_:k1_], k_d[pr, kc, :, h_, k0_:k1_], writes=[Kt.name], slot=Kt.name)
                    for t0_ in range(0, nkt, 16):
                        t1_ = min(nkt, t0_ + 16)
                        p.dma('sp', Vt[:, t0_:t1_, :], v_d[pr, kc, :, t0_:t1_, :], writes=[Vt.name], slot=Vt.name)
                    lastk = [(pr, kc), (Kt, Vt)]
                Kt, Vt = lastk[1]
                B = bt[cnt['b'] % 2]
                cnt['b'] += 1
                for bi, (kind, a, b_) in enumerate(blist):
                    if DEBUG.get('nobias'):
                        p.dma('sp', B[:, bi, :], jm_d[:, :].rearrange("p (a q) -> p a q", a=1).broadcast_to([128, 2, 128]) if False else hf_d[:, :], writes=[B.name], slot=B.name) if False else None
                        continue
                    if kind == 'f':
                        srcb = bass.AP(fv_d, (2 * c) * ltot + a + b_, [[1, 128], [ltot, 2], [1, 128]])
                        p.dma('pool', B[:, bi, :].rearrange("p (h q) -> p h q", h=2), srcb, writes=[B.name], slot=B.name)
                    else:
                        for h in range(2):
                            srcb = bass.AP(cv_d, a * lc + b_, [[16, 128], [1, 128]])
                            p.dma('pool', B[:, bi, h * 128:(h + 1) * 128], srcb, writes=[B.name], slot=B.name)
                U = ub[cnt['u'] % 2]
                Dd = db[cnt['u'] % 2]
                cnt['u'] += 1
                g = c // 4
                for i in range(NQT):
                    kts = plan(i)
                    acc = ps_a[cnt['a'] % 2]
                    accd = ps_d[cnt['a'] % 2]
                    cnt['a'] += 1
                    qsl = slice(i * 128, (i + 1) * 128)
                    nk = len(kts)
                    for k0 in range(0, nk, 2):
                        grp = kts[k0:k0 + 2]
                        S = ps_s[cnt['s'] % 2]
                        cnt['s'] += 1
                        for s_, (ktile, bidx, um, halo) in enumerate(grp):
                            ksl = slice(ktile * 128, (ktile + 1) * 128)
                            col = s_ * 256
                            for h in range(2):
                                p.op('pe', lambda e, S=S, col=col, B=B, bidx=bidx, h=h: e.matmul(S[:, col + h * 128:col + (h + 1) * 128], lhsT=jm[:], rhs=B[:, bidx, h * 128:(h + 1) * 128], start=True, stop=False),
                                     reads=['jm', B.name], writes=[S.name])
                                p.op('pe', lambda e, S=S, col=col, h=h, Kt=Kt, Q=Q, ksl=ksl, qsl=qsl, last=(not um): e.matmul(S[:, col + h * 128:col + (h + 1) * 128], lhsT=Kt[:, h, ksl], rhs=Q[:, qsl], start=False, stop=last),
                                     reads=[Kt.name, Q.name], writes=[S.name])
                                if um:
                                    p.op('pe', lambda e, S=S, col=col, h=h, ksl=ksl, qsl=qsl, g=g: e.matmul(S[:, col + h * 128:col + (h + 1) * 128], lhsT=em[:, ksl], rhs=nm[:, g, qsl], start=False, stop=True),
                                         reads=['em', 'nm'], writes=[S.name])
                        P = pb[cnt['pb'] % 4]
                        cnt['pb'] += 1
                        ncol = 256 * len(grp)
                        p.op('act', lambda e, P=P, S=S, ncol=ncol: e.activation(out=P[:, 0:ncol], in_=S[:, 0:ncol], func=AF.Exp, scale=SCALE),
                             reads=[S.name], writes=[P.name])
                        for s_, (ktile, bidx, um, halo) in enumerate(grp):
                            kidx = k0 + s_
                            col = s_ * 256
                            on = hflag if halo else ones1
                            p.op('pe', lambda e, acc=acc, Vt=Vt, ktile=ktile, P=P, col=col, kidx=kidx, nk=nk: e.matmul(acc[:, 0:256], lhsT=Vt[:, ktile, :], rhs=P[:, col:col + 256], start=(kidx == 0), stop=(kidx == nk - 1)),
                                 reads=[Vt.name, P.name], writes=[acc.name])
                            p.op('pe', lambda e, accd=accd, on=on, P=P, col=col, kidx=kidx, nk=nk: e.matmul(accd[:, 0:256], lhsT=on[:], rhs=P[:, col:col + 256], start=(kidx == 0), stop=(kidx == nk - 1)),
                                 reads=['hflag', 'ones1', P.name], writes=[accd.name])
                        if imp:
                            pi = ps_i[cnt['i'] % 2]
                            cnt['i'] += 1
                            for h in range(2):
                                for s_, (ktile, bidx, um, halo) in enumerate(grp):
                                    p.op('pe', lambda e, pi=pi, h=h, P=P, s_=s_, ktile=ktile, ng=len(grp): e.matmul(pi[:, h, :], lhsT=P[:, s_ * 256 + h * 128:s_ * 256 + (h + 1) * 128], rhs=ov[:, ktile, :], start=(s_ == 0), stop=(s_ == ng - 1)),
                                         reads=[P.name, 'ov'], writes=[pi.name])
                            p.op('dve', lambda e, pi=pi: e.tensor_scalar(out=rden[:], in0=pi[:, :, 64], scalar1=1e-30, scalar2=None, op0=ALU.add), reads=[pi.name], writes=['rden'])
                            p.op('dve', lambda e: e.reciprocal(out=rden[:], in_=rden[:]), reads=['rden'], writes=['rden'])
                            for h in range(2):
                                p.op('dve', lambda e, pi=pi, h=h, i=i, g=g: e.scalar_tensor_tensor(out=impacc[:, i, g, :], in0=pi[:, h, 0:64], scalar=rden[:, h:h + 1], in1=impacc[:, i, g, :], op0=ALU.mult, op1=ALU.add),
                                     reads=[pi.name, 'rden', 'impacc'], writes=['impacc'])
                    p.op('act', lambda e, U=U, acc=acc, i=i: e.activation(out=U[:, i, :], in_=acc[:, 0:256], func=AF.Copy), reads=[acc.name], writes=[U.name])
                    p.op('dve', lambda e, Dd=Dd, accd=accd, i=i: e.tensor_copy(out=Dd[:, i, :], in_=accd[:, 0:256]), reads=[accd.name], writes=[Dd.name])
                for i0_ in range(0, NQT, 4):
                    ev1 = p.dma('sp', u_d[pr, c, :, i0_:i0_ + 4, :], U[:, i0_:i0_ + 4, :], reads=[U.name], writes=[U.name + "st"], slot="st_" + U.name)
                    ev2 = p.dma('sp', d_d[pr, c, :, i0_:i0_ + 4, :], Dd[:, i0_:i0_ + 4, :], reads=[Dd.name], writes=[Dd.name + "st"], slot="st_" + Dd.name)
                out_evs += [ev1, ev2]
        if imp:
            for i in range(NQT):
                for g in range(2):
                    p.op('dve', lambda e, i=i, g=g: e.tensor_tensor(out=scr[:], in0=impacc[:, i, g, :], in1=sbias[:, i, :], op=ALU.add), reads=['impacc', 'sbias'], writes=['scr'])
                    p.op('dve', lambda e: e.max(out=mx[:, 0:8], in_=scr[:]), reads=['scr'], writes=['mx'])
                    p.op('dve', lambda e: e.match_replace(out=scr2[:], in_to_replace=mx[:, 0:8], in_values=scr[:], imm_value=-3e38), reads=['scr', 'mx'], writes=['scr2'])
                    p.op('dve', lambda e: e.max(out=mx[:, 8:16], in_=scr2[:]), reads=['scr2'], writes=['mx2'])
                    p.op('dve', lambda e, i=i, g=g: e.tensor_scalar(out=impacc[:, i, g, :], in0=scr[:], scalar1=mx[:, 15:16], scalar2=MASKV, op0=ALU.is_lt, op1=ALU.mult),
                         reads=['scr', 'mx2'], writes=['impacc'])
            p.op('dve', lambda e: e.tensor_copy(out=nmb[:], in_=impacc[:]), reads=['impacc'], writes=['nmb'])
            out_evs.append(p.dma('sp', nmo_d[:, :, :, :], nmb[:], reads=['nmb'], writes=['nmo']))
        p.finish_on('sp', out_evs[-6:])
        p.emit()
    return nc


def build_oproj(branches, has_gate, has_sink):
    nu = sum(len(b) for b in branches)
    nb = len(branches)
    nc = bass.Bass("TRN2", target_bir_lowering=False)
    u_d = nc.dram_tensor("U", [nu, KC, 128, NT], F32, kind="ExternalInput").ap()
    d_d = nc.dram_tensor("Dn", [nu, KC, 128, NT], F32, kind="ExternalInput").ap()
    if has_gate:
        gt_d = nc.dram_tensor("gate", [nb, KC, 128, NT], BF16, kind="ExternalInput").ap()
    if has_sink:
        sk_d = nc.dram_tensor("sink", [128, KC], F32, kind="ExternalInput").ap()
    wo_d = nc.dram_tensor("w_o", [D, D], F32, kind="ExternalInput").ap()
    hT_d = nc.dram_tensor("hT", [KC, 128, NT], F32, kind="ExternalInput").ap()
    g_d = nc.dram_tensor("gains", [128, KC], F32, kind="ExternalInput").ap()
    out_d = nc.dram_tensor("out", [KC, 128, NT], F32, kind="ExternalOutput").ap()
    with contextlib.ExitStack() as st:
        sb = lambda name, shape, dt: st.enter_context(nc.sbuf_tensor(name, shape, dt))
        ps = lambda name, shape, dt: st.enter_context(nc.psum_tensor(name, shape, dt))
        wo = sb("wo_sb", [128, KC, D], BF16)
        gains = sb("gains_sb", [128, KC], F32)
        ones_bf = sb("ones_bf", [128, 128], BF16)
        epsc = sb("epsc", [128, 1], F32)
        sink = sb("sink_sb", [128, KC], F32)
        ul = [sb(f"ul{i}", [128, TG], F32) for i in range(4)]
        dl = [sb(f"dl{i}", [128, TG], F32) for i in range(4)]
        gl = [sb(f"gl{i}", [128, TG], BF16) for i in range(2)]
        sg = sb("sg", [128, TG], F32)
        us = sb("us", [128, TG], F32)
        ds = sb("ds", [128, TG], F32)
        oacc = sb("oacc", [128, TG], F32)
        oT = sb("oT", [128, KC, TG], BF16)
        hres = sb("hres", [128, KC, TG], F32)
        ysb = sb("ysb", [128, KC, TG], F32)
        sqs = [sb(f"sqp{i}", [128, TG], BF16) for i in range(3)]
        rstd = sb("rstd", [128, TG], F32)
        tmp = sb("tmpn", [128, TG], F32)
        ps_stat = ps("ps_stat", [128, TG], F32)
        ps_y = [ps(f"ps_y{i}", [128, TG], F32) for i in range(2)]
        p = Prog(nc)
        for kc in range(KC):
            p.dma('pool', wo[:, kc, :], wo_d[kc * 128:(kc + 1) * 128, :], writes=['wo'], slot='wo')
        p.dma('sp', gains[:], g_d[:, :], writes=['gains'])
        p.op('dve', lambda e: e.memset(ones_bf[:], 1.0), writes=['ones'])
        p.op('dve', lambda e: e.memset(epsc[:], EPS), writes=['epsc'])
        if has_sink:
            p.dma('sp', sink[:], sk_d[:, :], writes=['sink'])
            p.op('act', lambda e: e.activation(out=sink[:], in_=sink[:], func=AF.Exp), reads=['sink'], writes=['sink'])
        n = dict(u=0, g=0)
        sqctr = [0]
        evs = []
        for tg in range(NTG):
            tsl = slice(tg * TG, (tg + 1) * TG)
            p.dma('sp', hres[:], hT_d[:, :, tsl].rearrange("c p t -> p c t"), writes=[(hres.name, 0)])
            for c in range(KC):
                ui = 0
                for b, idxs in enumerate(branches):
                    for k, _ in enumerate(idxs):
                        u_ = ul[n['u'] % 4]
                        d_ = dl[n['u'] % 4]
                        n['u'] += 1
                        p.dma('sp', u_[:], u_d[ui, c, :, tsl], writes=[u_.name])
                        p.dma('sp', d_[:], d_d[ui, c, :, tsl], writes=[d_.name])
                        ui += 1
                        if k == 0:
                            p.op('dve', lambda e, u_=u_: e.tensor_copy(out=us[:], in_=u_[:]), reads=[u_.name], writes=['us'])
                            p.op('pool', lambda e, d_=d_: e.tensor_scalar(out=ds[:], in0=d_[:], scalar1=1e-30, scalar2=None, op0=ALU.add), reads=[d_.name], writes=['ds'])
                        else:
                            p.op('dve', lambda e, u_=u_: e.tensor_tensor(out=us[:], in0=us[:], in1=u_[:], op=ALU.add), reads=[u_.name, 'us'], writes=['us'])
                            p.op('pool', lambda e, d_=d_: e.tensor_tensor(out=ds[:], in0=ds[:], in1=d_[:], op=ALU.add), reads=[d_.name, 'ds'], writes=['ds'])
                    if has_sink:
                        p.op('pool', lambda e, c=c: e.tensor_scalar(out=ds[:], in0=ds[:], scalar1=sink[:, c:c + 1], scalar2=None, op0=ALU.add), reads=['ds', 'sink'], writes=['ds'])
                    p.op('dve', lambda e: e.reciprocal(out=ds[:], in_=ds[:]), reads=['ds'], writes=['ds'])
                    p.op('dve', lambda e: e.tensor_tensor(out=us[:], in0=us[:], in1=ds[:], op=ALU.mult), reads=['us', 'ds'], writes=['us'])
                    if has_gate:
                        g_ = gl[n['g'] % 2]
                        n['g'] += 1
                        p.dma('sp', g_[:], gt_d[b, c, :, tsl], writes=[g_.name])
                        p.op('act', lambda e, g_=g_: e.activation(out=sg[:], in_=g_[:], func=AF.Sigmoid), reads=[g_.name], writes=['sg'])
                        p.op('dve', lambda e: e.tensor_tensor(out=us[:], in0=us[:], in1=sg[:], op=ALU.mult), reads=['us', 'sg'], writes=['us'])
                    last = (b == nb - 1)
                    dst = oT[:, c, :] if (last and nb == 1) else oacc[:]
                    dkey = ('oT', c) if (last and nb == 1) else 'oacc'
                    if b == 0:
                        p.op('dve', lambda e, dst=dst: e.tensor_copy(out=dst, in_=us[:]), reads=['us'], writes=[dkey])
                    elif not last:
                        p.op('dve', lambda e: e.tensor_tensor(out=oacc[:], in0=oacc[:], in1=us[:], op=ALU.add), reads=['us', 'oacc'], writes=['oacc'])
                    else:
                        p.op('dve', lambda e, c=c: e.tensor_tensor(out=oT[:, c, :], in0=oacc[:], in1=us[:], op=ALU.add), reads=['us', 'oacc'], writes=[('oT', c)])
            for oc in range(KC):
                py = ps_y[oc % 2]
                for c in range(KC):
                    p.op('pe', lambda e, py=py, c=c, oc=oc: e.matmul(py[:], lhsT=wo[:, c, oc * 128:(oc + 1) * 128], rhs=oT[:, c, :], start=(c == 0), stop=(c == KC - 1)),
                         reads=['wo', ('oT', c)], writes=[py.name])
                p.op('act', lambda e, oc=oc, py=py: e.activation(out=ysb[:, oc, :], in_=py[:], func=AF.Copy), reads=[py.name], writes=[('ysb', oc)])
            emit_postnorm_residual(p, nc, hres, ysb, gains[:, 0:KC], 0, ones_bf, ps_stat, sqs, rstd, tmp, sqctr, epsc)
            evs.append(p.dma('sp', out_d[:, :, tsl].rearrange("c p t -> p c t"), hres[:], reads=[(hres.name, 0)], writes=[('out', tg)], slot='st_out'))
            p.readers.setdefault((hres.name, 0), []).append(evs[-1])
        p.finish_on('sp', [('dma', 'st_out', p.dma_cnt['st_out'])])
        p.emit()
    return nc


def build_cmp():
    NB = 512
    nc = bass.Bass("TRN2", target_bir_lowering=False)
    b_d = nc.dram_tensor("blocksT", [16, 128, NB], BF16, kind="ExternalInput").ap()
    pos_d = nc.dram_tensor("posT", [128, 16], F32, kind="ExternalInput").ap()
    w1_d = nc.dram_tensor("w1", [2048, 256], F32, kind="ExternalInput").ap()
    w2_d = nc.dram_tensor("w2", [256, 64], F32, kind="ExternalInput").ap()
    out_d = nc.dram_tensor("out", [64, NB], BF16, kind="ExternalOutput").ap()
    with contextlib.ExitStack() as st:
        sb = lambda name, shape, dt: st.enter_context(nc.sbuf_tensor(name, shape, dt))
        ps = lambda name, shape, dt: st.enter_context(nc.psum_tensor(name, shape, dt))
        bl = sb("bl", [128, 16, NB], BF16)
        posf = sb("posf", [128, 16], F32)
        posb = sb("posb", [128, 16], BF16)
        w1 = sb("w1_sb", [128, 16, 256], BF16)
        w2 = sb("w2_sb", [128, 2, 64], BF16)
        b1 = sb("b1", [128, 2], F32)
        g1 = sb("g1", [128, 2, NB], BF16)
        ob = sb("ob", [64, NB], BF16)
        ps_h = [ps(f"ps_h{i}", [128, NB], F32) for i in range(2)]
        ps_b = ps("ps_b", [128, 2], F32)
        ps_o = ps("ps_o", [64, NB], F32)
        p = Prog(nc)
        p.dma('sp', bl[:], b_d.rearrange("c p n -> p c n"), writes=['bl'])
        p.dma('sp', posf[:], pos_d[:, :], writes=['posf'])
        p.dma('pool', w1[:], w1_d.rearrange("(c p) n -> p c n", p=128), writes=['w1'])
        p.dma('pool', w2[:], w2_d.rearrange("(c p) n -> p c n", p=128), writes=['w2'])
        p.op('dve', lambda e: e.tensor_copy(out=posb[:], in_=posf[:]), reads=['posf'], writes=['posb'])
        for m in range(2):
            for c in range(16):
                p.op('pe', lambda e, m=m, c=c: e.matmul(ps_b[:, m:m + 1], lhsT=w1[:, c, m * 128:(m + 1) * 128], rhs=posb[:, c:c + 1], start=(c == 0), stop=(c == 15)),
                     reads=['w1', 'posb'], writes=['ps_b'])
        p.op('dve', lambda e: e.tensor_copy(out=b1[:], in_=ps_b[:]), reads=['ps_b'], writes=['b1'])
        for m in range(2):
            for c in range(16):
                p.op('pe', lambda e, m=m, c=c: e.matmul(ps_h[m][:], lhsT=w1[:, c, m * 128:(m + 1) * 128], rhs=bl[:, c, :], start=(c == 0), stop=(c == 15)),
                     reads=['w1', 'bl'], writes=[ps_h[m].name])
            p.op('act', lambda e, m=m: e.activation(out=g1[:, m, :], in_=ps_h[m][:], func=AF.Gelu_apprx_tanh, bias=b1[:, m:m + 1]),
                 reads=[ps_h[m].name, 'b1'], writes=[('g1', m)])
        for m in range(2):
            p.op('pe', lambda e, m=m: e.matmul(ps_o[:], lhsT=w2[:, m, :], rhs=g1[:, m, :], start=(m == 0), stop=(m == 1)), reads=['w2', ('g1', m)], writes=['ps_o'])
        p.op('act', lambda e: e.activation(out=ob[:], in_=ps_o[:], func=AF.Copy), reads=['ps_o'], writes=['ob'])
        ev = p.dma('sp', out_d[:, :], ob[:], reads=['ob'], writes=['out'])
        p.finish_on('sp', [ev])
        p.emit()
    return nc


import ml_dtypes
BF = ml_dtypes.bfloat16

DIL = (1, 4, 16)
VARIANTS = [("A1", 1, 128, 2), ("A2", 4, 128, 2), ("A3", 16, 128, 2), ("B", 1, 127, 2), ("CW", 1, 511, 5), ("CS", 1, 10 ** 9, 14)]
VBASE = {}
_o = 0
for _n, _s, _m, _k in VARIANTS:
    VBASE[_n] = _o
    _o += 128 * _k + 128
LTOT = _o
LC = 4224
_PROGS = {}


def _prog(key, fn):
    if key not in _PROGS:
        _PROGS[key] = fn()
    return _PROGS[key]


def _run(nc, maps):
    res = run_bass_kernel_spmd(nc, maps, core_ids=list(range(8)))
    return res.results


def _t5_bucket(d):
    d = np.maximum(d, 0)
    df = np.maximum(d, 1).astype(np.float32)
    large = 16 + (np.log(df / np.float32(16)) / np.float32(np.log(128.0)) * np.float32(16)).astype(np.int32)
    large = np.minimum(large, 31)
    return np.where(d < 16, d, large)


def _onehot():
    oh = np.zeros((33, LTOT), np.float32)
    for name, stride, maxd, noff in VARIANTS:
        L = 128 * noff + 128
        d = np.arange(L) - 127
        ok = (d >= 0) & (d <= maxd)
        b = _t5_bucket(np.clip(d, 0, None) * stride)
        rows = np.where(ok, b, 32)
        oh[rows, VBASE[name] + np.arange(L)] = 1.0
    return oh


def _to_cores(h):
    out = []
    for k in range(8):
        b, half = k // 2, k % 2
        own = h[b, half * 2048:(half + 1) * 2048]
        out.append(np.ascontiguousarray(own.T.reshape(8, 128, 2048)))
    return out


def _from_cores(lst, dtype=np.float32):
    ncol = lst[0].shape[0] * 128
    out = np.zeros((4, 4096, ncol), dtype)
    for k in range(8):
        b, half = k // 2, k % 2
        out[b, half * 2048:(half + 1) * 2048] = lst[k].reshape(ncol, 2048).T
    return out


def _gcol(g):
    return np.ascontiguousarray(g.reshape(8, 128).T.astype(np.float32))


def _lin(h, gain, w):
    nco = ((w.shape[1] + 127) // 128) * 128
    if nco != w.shape[1]:
        w = np.concatenate([w, np.zeros((w.shape[0], nco - w.shape[1]), np.float32)], axis=1)
    nc = _prog(("lin", nco), lambda: build_lin(nco))
    hT = _to_cores(h)
    w = np.ascontiguousarray(w)
    res = _run(nc, [dict(hT=hT[k], gains=_gcol(gain), w=w) for k in range(8)])
    return _from_cores([r["out"] for r in res], BF)


def _jmat():
    return np.ascontiguousarray(np.eye(128, dtype=np.float32)[::-1]).astype(BF)


def _hflag(half):
    return np.full((128, 128), float(half), np.float32).astype(BF)


def _oproj(key, branches, U, Dn, h, gain, w_o, gate=None, sink=None):
    nc = _prog(("oproj", key), lambda: build_oproj(branches, gate is not None, sink is not None))
    hT = _to_cores(h)
    maps = []
    for k in range(8):
        m = dict(U=U[k], Dn=Dn[k], w_o=np.ascontiguousarray(w_o), hT=hT[k], gains=_gcol(gain))
        if gate is not None:
            m['gate'] = gate[k]
        if sink is not None:
            m['sink'] = sink
        maps.append(m)
    res = _run(nc, maps)
    return _from_cores([r["out"] for r in res])


def _split_k(kT):
    out = np.zeros(kT.shape[:-1] + (2, kT.shape[-1]), kT.dtype)
    out[..., :64, 0, :] = kT[..., :64, :]
    out[..., 64:, 1, :] = kT[..., 64:, :]
    return out


def _sel(u):
    npr = u.shape[0]
    out = np.empty((npr, 8, 128, 2048), u.dtype)
    out[:, :, :64] = u[:, :, :64, :, 0:128].reshape(npr, 8, 64, 2048)
    out[:, :, 64:] = u[:, :, 64:, :, 128:256].reshape(npr, 8, 64, 2048)
    return out


def _dup_kT(kk):
    t = np.ascontiguousarray(kk.T)
    return np.concatenate([t, t], axis=0)


def _v_tiles(vv128, nkt_pad):
    nkt = vv128.shape[0] // 128
    out = np.zeros((128, nkt_pad, 128), vv128.dtype)
    out[:, :nkt] = vv128.reshape(nkt, 128, 128).transpose(1, 0, 2)
    return out


def _mixer_A(h, gains, w_in, w_o, fvec):
    P = _lin(h, gains[0], w_in)
    qkv = P.reshape(4, 4096, 3, 3, 16, 64)
    plans, biases = [], []
    for gi, dil in enumerate(DIL):
        tpr = 1 + 16 // dil
        nq = 16 // dil

        def plan(i, tpr=tpr, nq=nq):
            r, jq = i // nq, i % nq
            return [(r * tpr + jq, 1, False, jq == 0), (r * tpr + jq + 1, 0, False, False)]
        plans.append(plan)
        vb = VBASE["A%d" % (gi + 1)]
        biases.append([('f', vb, 0), ('f', vb, 128)])
    cfg = dict(npr=3, nkch=8, nkt=32, nkeys=4096, kch=lambda c: c, plans=plans, bias=biases, ltot=LTOT)
    nc = _prog("attA", lambda: build_att(cfg))
    maps = []
    for k in range(8):
        b, half = k // 2, k % 2
        QT = np.zeros((3, 8, 128, 2048), BF)
        KT = np.zeros((3, 8, 128, 4096), BF)
        V = np.zeros((3, 8, 128, 32, 128), BF)
        for gi, dil in enumerate(DIL):
            L, Lo = 4096 // dil, 2048 // dil

            def strided(t):
                return t.reshape(L, dil, 16, 64).transpose(1, 0, 2, 3)
            q, kk, vv = (strided(qkv[b, :, gi, i]) for i in range(3))
            own = slice(half * Lo, (half + 1) * Lo)
            QT[gi] = q[:, own].reshape(2048, 8, 128).transpose(1, 2, 0)
            if half == 0:
                kh = np.zeros((dil, 128, 16, 64), BF)
                vh = np.zeros((dil, 128, 16, 64), BF)
            else:
                kh, vh = kk[:, Lo - 128:Lo], vv[:, Lo - 128:Lo]
            kc = np.concatenate([kh, kk[:, own]], axis=1)
            vc = np.concatenate([vh, vv[:, own]], axis=1)
            nkeys = dil * (128 + Lo)
            KT[gi, :, :, :nkeys] = kc.reshape(nkeys, 8, 128).transpose(1, 2, 0)
            nkt = nkeys // 128
            V[gi, :, :, :nkt] = vc.reshape(nkt, 128, 8, 128).transpose(2, 1, 0, 3)
        maps.append(dict(QT=QT, KT=_split_k(KT), V=V, hflag=_hflag(half), jmat=_jmat(), fvec=fvec))
    res = _run(nc, maps)
    U, Dn = [], []
    for k in range(8):
        u, d = _sel(res[k]["U"]), _sel(res[k]["Dn"])
        uu = np.zeros_like(u)
        dd = np.zeros_like(d)
        for gi, dil in enumerate(DIL):
            Lo = 2048 // dil
            uu[gi] = u[gi].reshape(8, 128, dil, Lo).transpose(0, 1, 3, 2).reshape(8, 128, 2048)
            dd[gi] = d[gi].reshape(8, 128, dil, Lo).transpose(0, 1, 3, 2).reshape(8, 128, 2048)
        U.append(uu)
        Dn.append(dd)
    return _oproj("A", [[0, 1, 2]], U, Dn, h, gains[1], w_o)


def _mixer_B(h, gains, w_in, sinks, w_o, fvec):
    P = _lin(h, gains[0], w_in)
    q = P[..., :1024]
    kx = P[..., 1024:1152].reshape(4, 4096, 2, 64)
    vx = P[..., 1152:1280].reshape(4, 4096, 2, 64)
    vb = VBASE["B"]
    cfg = dict(npr=1, nkch=2, nkt=17, nkeys=2176, kch=lambda c: c // 4,
               plans=[lambda i: [(i, 1, False, i == 0), (i + 1, 0, False, False)]],
               bias=[[('f', vb, 0), ('f', vb, 128)]], ltot=LTOT)
    nc = _prog("attB", lambda: build_att(cfg))
    maps = []
    for k in range(8):
        b, half = k // 2, k % 2
        S0 = half * 2048
        QT = np.ascontiguousarray(q[b, S0:S0 + 2048].reshape(2048, 8, 128).transpose(1, 2, 0))[None]
        KT = np.zeros((1, 2, 128, 2176), BF)
        V = np.zeros((1, 2, 128, 17, 128), BF)
        for g in range(2):
            if half == 0:
                kh, vh = np.zeros((128, 64), BF), np.zeros((128, 64), BF)
            else:
                kh, vh = kx[b, S0 - 128:S0, g], vx[b, S0 - 128:S0, g]
            kk = np.concatenate([kh, kx[b, S0:S0 + 2048, g]], axis=0)
            vv = np.concatenate([vh, vx[b, S0:S0 + 2048, g]], axis=0)
            KT[0, g] = _dup_kT(kk)
            V[0, g] = _v_tiles(np.concatenate([vv, vv], axis=1), 17)
        maps.append(dict(QT=QT, KT=_split_k(KT), V=V, hflag=_hflag(half), jmat=_jmat(), fvec=fvec))
    res = _run(nc, maps)
    U = [_sel(r["U"]) for r in res]
    Dn = [_sel(r["Dn"]) for r in res]
    sink = np.ascontiguousarray(np.repeat(sinks.reshape(8, 2), 64, axis=1).T.astype(np.float32))
    return _oproj("B", [[0]], U, Dn, h, gains[1], w_o, sink=sink)


def _mixer_C(h, gains, w_in, cmp_pos, cmp_w1, cmp_w2, w_o, fvec):
    P = _lin(h, gains[0], w_in)
    q = P[..., :1024]
    kv = P[..., 1024:1792].reshape(4, 4096, 6, 2, 64)
    gates = P[..., 1792:1840].reshape(4, 4096, 3, 16)
    nc = _prog("cmp", build_cmp)
    idx = np.arange(255)[:, None] * 16 + np.arange(32)[None, :]
    maps = []
    for k in range(8):
        b, i = k // 2, k % 2
        t = kv[b, :, i]
        blk = t[idx]
        blk = blk.transpose(2, 0, 1, 3).reshape(2, 255, 2048)
        full = np.zeros((2, 256, 2048), BF)
        full[:, :255] = blk
        blocksT = np.ascontiguousarray(full.reshape(512, 2048).T.reshape(16, 128, 512))
        posT = np.ascontiguousarray(cmp_pos[i].reshape(16, 128).T.astype(np.float32))
        maps.append(dict(blocksT=blocksT, posT=posT, w1=np.ascontiguousarray(cmp_w1[i]), w2=np.ascontiguousarray(cmp_w2[i])))
    res = _run(nc, maps)
    kcv = [[res[2 * b + i]["out"].reshape(64, 2, 256) for i in range(2)] for b in range(4)]
    cfg = dict(npr=1, nkch=2, nkt=2, nkeys=256, kch=lambda c: c // 4,
               plans=[lambda i: [(0, 2 * i, False, False), (1, 2 * i + 1, False, False)]],
               bias=[[('c', t, 128 * i) for i in range(16) for t in range(2)]], lc=LC, imp=True)
    nc = _prog("attCc", lambda: build_att(cfg))
    n_ = np.arange(256)
    j_ = np.arange(64)
    ovl = ((16 * n_[:, None] < (j_[None, :] + 1) * 64) & (16 * n_[:, None] + 32 > j_[None, :] * 64) & (n_[:, None] < 255)).astype(np.float32)
    ov65 = np.ones((128, 2, 65), np.float32)
    ov65[:, :, :64] = ovl.reshape(2, 128, 64).transpose(1, 0, 2)
    ov65 = ov65.astype(BF)
    maps = []
    QTs = []
    for k in range(8):
        b, half = k // 2, k % 2
        S0 = half * 2048
        QT = np.ascontiguousarray(q[b, S0:S0 + 2048].reshape(2048, 8, 128).transpose(1, 2, 0))
        QTs.append(QT)
        KT = np.zeros((1, 2, 128, 256), BF)
        V = np.zeros((1, 2, 128, 2, 128), BF)
        for g in range(2):
            kc = kcv[b][0][:, g, :]
            KT[0, g] = np.concatenate([kc, kc], axis=0)
            vc = kcv[b][1][:, g, :].T
            V[0, g] = _v_tiles(np.concatenate([vc, vc], axis=1), 2)
        cvec = np.zeros((2, LC), np.float32)
        for t in range(2):
            Z = 2048 * t + 2063 - S0
            cvec[t, :max(0, min(LC, Z))] = MASKV
        pos = S0 + np.arange(16)[None, :] * 128 + np.arange(128)[:, None]
        cur = pos // 64
        jj = np.arange(64)[None, None, :]
        forced = (jj == 0) | (jj == cur[..., None]) | (jj == cur[..., None] - 1)
        allowed = jj <= cur[..., None]
        selbias = np.where(forced, 1e9, np.where(allowed, 0.0, -1e30)).astype(np.float32)
        maps.append(dict(QT=QT[None], KT=_split_k(KT), V=V, hflag=_hflag(half), jmat=_jmat(), cvec=cvec, ov65=ov65, selbias=selbias))
    res = _run(nc, maps)
    Uc = [_sel(r["U"])[0] for r in res]
    Dc = [_sel(r["Dn"])[0] for r in res]
    negm = [r["negmask"] for r in res]
    sel_plan = lambda i: [(t, min(16 + i - t, 13), True, t < 16) for t in range(17 + i)]
    win_plan = lambda i: [(t, 4 + i - t, False, t < 4) for t in range(i, i + 5)]
    cfg = dict(npr=2, nkch=2, nkt=32, nkeys=4096, kch=lambda c: c // 4, plans=[sel_plan, win_plan],
               bias=[[('f', VBASE["CS"], 128 * o) for o in range(14)], [('f', VBASE["CW"], 128 * o) for o in range(5)]],
               ltot=LTOT, mask=True, sharedq=True)
    nc = _prog("attCs", lambda: build_att(cfg))
    maps = []
    for k in range(8):
        b, half = k // 2, k % 2
        S0 = half * 2048
        KT = np.zeros((2, 2, 128, 4096), BF)
        V = np.zeros((2, 2, 128, 32, 128), BF)
        for g in range(2):
            ks, vs, kw, vw = kv[b, :, 2, g], kv[b, :, 3, g], kv[b, :, 4, g], kv[b, :, 5, g]
            if half == 0:
                z = np.zeros((2048, 64), BF)
                kk, vv = np.concatenate([z, ks[:2048]], 0), np.concatenate([z, vs[:2048]], 0)
                kk2, vv2 = np.concatenate([z[:512], kw[:2048]], 0), np.concatenate([z[:512], vw[:2048]], 0)
            else:
                kk, vv = ks, vs
                kk2, vv2 = kw[S0 - 512:], vw[S0 - 512:]
            KT[0, g] = _dup_kT(kk)
            V[0, g] = _v_tiles(np.concatenate([vv, vv], axis=1), 32)
            KT[1, g, :, :2560] = _dup_kT(kk2)
            V[1, g] = _v_tiles(np.concatenate([vv2, vv2], axis=1), 32)
        u_ = np.arange(4096)
        tok = u_ - 2048 + S0
        E = np.zeros((64, 4096), np.float32)
        ok = tok >= 0
        E[tok[ok] // 64, u_[ok]] = 1.0
        nmT = np.ascontiguousarray(negm[k].transpose(2, 3, 1, 0).reshape(2, 64, 2048))
        maps.append(dict(QT=QTs[k], KT=_split_k(KT), V=V, hflag=_hflag(half), jmat=_jmat(), fvec=fvec, emat=E.astype(BF), nmT=nmT))
    res = _run(nc, maps)
    U, Dn, G = [], [], []
    for k in range(8):
        b, half = k // 2, k % 2
        S0 = half * 2048
        us_, ds_ = _sel(res[k]["U"]), _sel(res[k]["Dn"])
        U.append(np.ascontiguousarray(np.stack([Uc[k], us_[0], us_[1]])))
        Dn.append(np.ascontiguousarray(np.stack([Dc[k], ds_[0], ds_[1]])))
        gt = gates[b, S0:S0 + 2048]
        gr = np.repeat(gt.transpose(1, 2, 0), 64, axis=1)
        G.append(np.ascontiguousarray(gr.reshape(3, 8, 128, 2048)))
    return _oproj("C", [[0], [1], [2]], U, Dn, h, gains[1], w_o, gate=G)


def _ffn(h, layer, inp):
    nc = _prog("ffn", build_ffn)
    g = inp['norm_gains'][layer]
    gains = np.ascontiguousarray(np.stack([g[2].reshape(8, 128).T, g[3].reshape(8, 128).T], axis=1).reshape(128, 16).astype(np.float32))
    cwf = np.concatenate([inp['ffn_conv_w'][layer], inp['ffn_conv_b'][layer][None]], axis=0)
    cw = np.ascontiguousarray(cwf.reshape(4, 44, 128).transpose(2, 1, 0).astype(np.float32))
    hT = _to_cores(h)
    maps = []
    for k in range(8):
        b, half = k // 2, k % 2
        if half == 1:
            halo = np.ascontiguousarray(h[b, 2046:2048].T.reshape(8, 128, 2))
        else:
            halo = np.zeros((8, 128, 2), np.float32)
        maps.append(dict(hT=hT[k], halo=halo, gains=gains, w_up=np.ascontiguousarray(inp['ffn_w_up'][layer]),
                         conv_wb=cw, w_down=np.ascontiguousarray(inp['ffn_w_down'][layer])))
    res = _run(nc, maps)
    return _from_cores([r["out"] for r in res])


def kernel(**inputs):
    inp = {k: np.asarray(v) for k, v in inputs.items()}
    h = np.ascontiguousarray(inp['x'].astype(np.float32))
    nc = _prog("bias", lambda: build_bias(LTOT))
    res = _run(nc, [dict(table=np.ascontiguousarray(inp['rel_table'].astype(np.float32)), onehot=_onehot()) for _ in range(8)])
    fvec = res[0]["out"]
    for layer in range(4):
        kind, j = layer % 3, layer // 3
        g = inp['norm_gains'][layer]
        if kind == 0:
            h = _mixer_A(h, g, inp['a_w_in'][j], inp['a_w_o'][j], fvec)
        elif kind == 1:
            h = _mixer_B(h, g, inp['b_w_in'][j], inp['b_sinks'][j], inp['b_w_o'][j], fvec)
        else:
            h = _mixer_C(h, g, inp['c_w_in'][j], inp['c_cmp_pos'][j], inp['c_cmp_w1'][j], inp['c_cmp_w2'][j], inp['c_w_o'][j], fvec)
        h = _ffn(h, layer, inp)
    return h.astype(np.float32)
```

```python
import contextlib
import numpy as np
import concourse.bass as bass
import concourse.mybir as mybir
from concourse.bass_utils import run_bass_kernel_spmd

F32 = mybir.dt.float32
BF16 = mybir.dt.bfloat16
AF = mybir.ActivationFunctionType
ALU = mybir.AluOpType
AX = mybir.AxisListType

SAME_ENGINE_SYNC = True
SEM_PAGE = 30000


class Prog:
    ENGS = ('pe', 'act', 'dve', 'pool', 'sp')

    def __init__(self, nc):
        self.nc = nc
        self.q = {e: [] for e in self.ENGS}
        self.lastw = {}
        self.readers = {}
        self.dma_cnt = {}
        self.seen = {e: {} for e in self.ENGS}
        self.needed = {e: set() for e in self.ENGS}
        self.final_events = []

    def _deps(self, eng, reads, writes):
        deps = []
        for k in reads:
            w = self.lastw.get(k)
            if w is not None:
                deps.append(w)
        for k in writes:
            w = self.lastw.get(k)
            if w is not None:
                deps.append(w)
            deps.extend(self.readers.get(k, ()))
        waits = []
        seen = self.seen[eng]
        for ev in deps:
            if ev[0] == 'eng':
                _, e2, j = ev
                if e2 == eng and (eng == 'pe' or not SAME_ENGINE_SYNC):
                    continue
                if seen.get(('eng', e2), -1) >= j:
                    continue
                seen[('eng', e2)] = j
                self.needed[e2].add(j)
                waits.append(ev)
            else:
                _, slot, cnt = ev
                if seen.get(('dma', slot), -1) >= cnt:
                    continue
                seen[('dma', slot)] = cnt
                waits.append(ev)
        best = {}
        for ev in waits:
            src = ev[:2]
            if src not in best or best[src][2] < ev[2]:
                best[src] = ev
        return list(best.values())

    def _commit(self, ev, reads, writes):
        for k in reads:
            self.readers.setdefault(k, []).append(ev)
        for k in writes:
            self.lastw[k] = ev
            self.readers[k] = []

    def op(self, eng, fn, reads=(), writes=()):
        waits = self._deps(eng, reads, writes)
        idx = len(self.q[eng])
        self.q[eng].append(dict(fn=fn, waits=waits, dma=None))
        ev = ('eng', eng, idx)
        self._commit(ev, reads, writes)
        return ev

    def dma(self, eng, out, in_, reads=(), writes=(), slot=None, **kw):
        waits = self._deps(eng, reads, writes)
        if slot is None:
            slot = writes[0]
        cnt = self.dma_cnt.get(slot, 0) + 16
        self.dma_cnt[slot] = cnt
        fn = (lambda e, out=out, in_=in_, kw=kw: e.dma_start(out=out, in_=in_, **kw))
        self.q[eng].append(dict(fn=fn, waits=waits, dma=slot))
        ev = ('dma', slot, cnt)
        self._commit(ev, reads, writes)
        return ev

    def finish_on(self, eng, events):
        events = list(events)
        for ev in events:
            if ev[0] == 'eng':
                self.needed[ev[1]].add(ev[2])
        self.final_events.append((eng, events))

    def emit(self):
        nc = self.nc
        import contextlib
        count_at = {}
        npages = {}
        for e in self.ENGS:
            c = 0
            for j in range(len(self.q[e])):
                if j in self.needed[e]:
                    c += 1
                    count_at[(e, j)] = c
            npages[e] = (c + SEM_PAGE - 1) // SEM_PAGE
        for eng, evs in self.final_events:
            pass
        with contextlib.ExitStack() as st:
            esems = {e: [st.enter_context(nc.semaphore(f"s_{e}_{i}")) for i in range(npages[e])]
                     for e in self.ENGS}
            dsems = {}
            for i, slot in enumerate(self.dma_cnt):
                dsems[slot] = st.enter_context(nc.semaphore(f"d_{i}"))
            self.n_sems = sum(npages.values()) + len(dsems)
            block = st.enter_context(nc.Block())

            def resolve(ev):
                if ev[0] == 'eng':
                    c = count_at[(ev[1], ev[2])]
                    return esems[ev[1]][(c - 1) // SEM_PAGE], (c - 1) % SEM_PAGE + 1
                return dsems[ev[1]], ev[2]

            def run(ename):
                def body(eobj):
                    for j, o in enumerate(self.q[ename]):
                        for ev in o['waits']:
                            s, v = resolve(ev)
                            eobj.wait_ge(s, v)
                        ins = o['fn'](eobj)
                        if o['dma'] is not None:
                            ins.then_inc(dsems[o['dma']], 16)
                        elif (ename, j) in count_at:
                            c = count_at[(ename, j)]
                            ins.then_inc(esems[ename][(c - 1) // SEM_PAGE], 1)
                    for eng, evs in self.final_events:
                        if eng == ename:
                            for ev in evs:
                                s, v = resolve(ev)
                                eobj.wait_ge(s, v)
                return body

            block.tensor(run('pe'))
            block.scalar(run('act'))
            block.vector(run('dve'))
            block.gpsimd(run('pool'))
            block.sync(run('sp'))


D = 1024
DFF = 2816
NT = 2048
TG = 512
NTG = NT // TG
KC = D // 128
NJ = DFF // 128
EPS = 1e-6


def emit_prenorm(p, nc, hT, xT, gcol, ncols, sq, rstd, ones_bf, ps_stat, epsc, xoff=0):
    nq = 0
    t0 = 0
    while t0 < ncols:
        n = min(TG, ncols - t0)
        for c in range(KC):
            s = sq[nq % 3]
            sk = s.name
            nq += 1
            p.op('act', lambda e, s=s, c=c, t0=t0, n=n: e.activation(out=s[:, 0:n], in_=hT[:, c, t0:t0 + n], func=AF.Square),
                 reads=[(hT.name, t0 // TG)], writes=[sk])
            p.op('pe', lambda e, s=s, c=c, n=n: e.matmul(ps_stat[:, 0:n], lhsT=ones_bf[:], rhs=s[:, 0:n], start=(c == 0), stop=(c == KC - 1)),
                 reads=[sk, 'ones'], writes=['ps_stat'])
        p.op('act', lambda e, n=n: e.activation(out=rstd[:, 0:n], in_=ps_stat[:, 0:n], func=AF.Sqrt, scale=1.0 / D, bias=epsc[:, 0:1]),
             reads=['ps_stat', 'epsc'], writes=[rstd.name])
        p.op('dve', lambda e, n=n: e.reciprocal(out=rstd[:, 0:n], in_=rstd[:, 0:n]),
             reads=[rstd.name], writes=[rstd.name])
        for c in range(KC):
            p.op('dve', lambda e, c=c, t0=t0, n=n: e.scalar_tensor_tensor(out=xT[:, c, xoff + t0:xoff + t0 + n], in0=hT[:, c, t0:t0 + n], scalar=gcol[:, c:c + 1], in1=rstd[:, 0:n], op0=ALU.mult, op1=ALU.mult),
                 reads=[(hT.name, t0 // TG), rstd.name, 'gains'], writes=[(xT.name, (xoff + t0) // TG)])
        t0 += n


def emit_postnorm_residual(p, nc, hT, ysb, gcol, tg, ones_bf, ps_stat, sqs, rstd, tmp, sqctr, epsc):
    for c in range(KC):
        s = sqs[sqctr[0] % len(sqs)]
        sk = s.name
        sqctr[0] += 1
        p.op('act', lambda e, s=s, c=c: e.activation(out=s[:], in_=ysb[:, c, :], func=AF.Square),
             reads=[('ysb', c)], writes=[sk])
        p.op('pe', lambda e, s=s, c=c: e.matmul(ps_stat[:], lhsT=ones_bf[:], rhs=s[:], start=(c == 0), stop=(c == KC - 1)),
             reads=[sk, 'ones'], writes=['ps_stat'])
    p.op('act', lambda e: e.activation(out=rstd[:], in_=ps_stat[:], func=AF.Sqrt, scale=1.0 / D, bias=epsc[:, 0:1]),
         reads=['ps_stat', 'epsc'], writes=[rstd.name])
    p.op('dve', lambda e: e.reciprocal(out=rstd[:], in_=rstd[:]),
         reads=[rstd.name], writes=[rstd.name])
    for c in range(KC):
        p.op('dve', lambda e, c=c: e.scalar_tensor_tensor(out=tmp[:], in0=ysb[:, c, :], scalar=gcol[:, c:c + 1], in1=rstd[:], op0=ALU.mult, op1=ALU.mult),
             reads=[('ysb', c), rstd.name, 'gains'], writes=[tmp.name])
        p.op('dve', lambda e, c=c: e.tensor_tensor(out=hT[:, c, tg * TG:(tg + 1) * TG], in0=hT[:, c, tg * TG:(tg + 1) * TG], in1=tmp[:], op=ALU.add),
             reads=[tmp.name, (hT.name, tg)], writes=[(hT.name, tg)])


def build_ffn():
    nc = bass.Bass("TRN2", target_bir_lowering=False)
    hT_d = nc.dram_tensor("hT", [KC, 128, NT], F32, kind="ExternalInput").ap()
    halo_d = nc.dram_tensor("halo", [KC, 128, 2], F32, kind="ExternalInput").ap()
    g_d = nc.dram_tensor("gains", [128, 2 * KC], F32, kind="ExternalInput").ap()
    wup_d = nc.dram_tensor("w_up", [D, 2 * DFF], F32, kind="ExternalInput").ap()
    cw_d = nc.dram_tensor("conv_wb", [128, 2 * NJ, 4], F32, kind="ExternalInput").ap()
    wdn_d = nc.dram_tensor("w_down", [DFF, D], F32, kind="ExternalInput").ap()
    out_d = nc.dram_tensor("out", [KC, 128, NT], F32, kind="ExternalOutput").ap()

    with contextlib.ExitStack() as st:
        sb = lambda name, shape, dt: st.enter_context(nc.sbuf_tensor(name, shape, dt))
        ps = lambda name, shape, dt: st.enter_context(nc.psum_tensor(name, shape, dt))
        hT = sb("hT_sb", [128, KC, NT], F32)
        hh = sb("hh_sb", [128, KC, 2], F32)
        xT = sb("xT_sb", [128, KC, NT], BF16)
        xh = sb("xh_sb", [128, KC, 2], BF16)
        gains = sb("gains_sb", [128, 2 * KC], F32)
        cw = sb("cw_sb", [128, 2 * NJ, 4], F32)
        ones_bf = sb("ones_bf", [128, 128], BF16)
        carry = sb("carry", [128, 2 * NJ, 2], F32)
        epsc = sb("epsc", [128, 1], F32)
        NWB = 4
        wu = [sb(f"wu{i}", [128, KC, 256], BF16) for i in range(NWB)]
        NDB = 3
        wd = [sb(f"wd{i}", [128, NJ, 128], BF16) for i in range(NDB)]
        ubuf = [sb(f"ubuf{i}", [128, TG + 2], F32) for i in range(4)]
        cbuf = [sb(f"cbuf{i}", [128, TG], F32) for i in range(4)]
        gbuf = [sb(f"gbuf{i}", [128, TG], F32) for i in range(2)]
        gT = sb("gT", [128, NJ, TG], BF16)
        ysb = sb("ysb", [128, KC, TG], F32)
        sqs = [sb(f"sqp{i}", [128, TG], BF16) for i in range(3)]
        rstd2 = sb("rstd2", [128, TG], F32)
        tmp = sb("tmpn", [128, TG], F32)
        ps_stat = ps("ps_stat", [128, TG], F32)
        ps_u = [ps(f"ps_u{i}", [128, TG], F32) for i in range(4)]
        ps_y = [ps(f"ps_y{i}", [128, TG], F32) for i in range(2)]
        ps_c = ps("ps_c", [128, 2 * NJ * 2], F32)

        p = Prog(nc)
        for c in range(KC):
            for tg in range(NTG):
                p.dma('sp', hT[:, c, tg * TG:(tg + 1) * TG], hT_d[c, :, tg * TG:(tg + 1) * TG], writes=[(hT.name, tg)], slot=f"ld_h{tg}")
        p.dma('sp', hh[:], halo_d.rearrange("c p t -> p c t"), writes=[(hh.name, 0)])
        p.dma('sp', gains[:], g_d[:, :], writes=['gains'])
        p.dma('sp', cw[:], cw_d[:, :, :], writes=['cw'])
        p.op('dve', lambda e: e.memset(ones_bf[:], 1.0), writes=['ones'])
        p.op('dve', lambda e: e.memset(epsc[:], EPS), writes=['epsc'])
        emit_prenorm(p, nc, hh, xh, gains[:, 0:KC], 2, sqs, rstd2, ones_bf, ps_stat, epsc)
        emit_prenorm(p, nc, hT, xT, gains[:, 0:KC], NT, sqs, rstd2, ones_bf, ps_stat, epsc)

        wu_n = [0]

        def load_wu(j):
            i = wu_n[0] % NWB
            wu_n[0] += 1
            w = wu[i]
            for half, col0 in ((0, j * 128), (1, DFF + j * 128)):
                p.dma('pool', w[:, :, half * 128:(half + 1) * 128],
                      wup_d[:, col0:col0 + 128].rearrange("(kc p) n -> p kc n", p=128),
                      writes=[w.name], slot=w.name)
            return w

        wd_n = [0]

        def load_wd(c):
            i = wd_n[0] % NDB
            wd_n[0] += 1
            w = wd[i]
            p.dma('pool', w[:], wdn_d[:, c * 128:(c + 1) * 128].rearrange("(j p) n -> p j n", p=128), writes=[w.name])
            return w

        for j in range(NJ):
            w = load_wu(j)
            for half in range(2):
                jg = half * NJ + j
                for kc in range(KC):
                    p.op('pe', lambda e, w=w, half=half, kc=kc, jg=jg: e.matmul(ps_c[:, jg * 2:jg * 2 + 2], lhsT=w[:, kc, half * 128:(half + 1) * 128], rhs=xh[:, kc, :], start=(kc == 0), stop=(kc == KC - 1)),
                         reads=[w.name, (xh.name, 0)], writes=['ps_c'])
        p.op('dve', lambda e: e.tensor_copy(out=carry[:].rearrange("p a b -> p (a b)"), in_=ps_c[:]), reads=['ps_c'], writes=['carry'])

        un = [0]
        sqctr = [0]
        for tg in range(NTG):
            tsl = slice(tg * TG, (tg + 1) * TG)
            for j in range(NJ):
                w = load_wu(j)
                cres = []
                for half in range(2):
                    jg = half * NJ + j
                    k = un[0] % 4
                    un[0] += 1
                    pu, ub, cb = ps_u[k], ubuf[k], cbuf[k]
                    for kc in range(KC):
                        p.op('pe', lambda e, w=w, half=half, kc=kc, pu=pu, tsl=tsl: e.matmul(pu[:], lhsT=w[:, kc, half * 128:(half + 1) * 128], rhs=xT[:, kc, tsl], start=(kc == 0), stop=(kc == KC - 1)),
                             reads=[w.name, (xT.name, tg)], writes=[pu.name])
                    p.op('dve', lambda e, ub=ub, jg=jg: e.tensor_copy(out=ub[:, 0:2], in_=carry[:, jg, :]), reads=['carry'], writes=[ub.name])
                    p.op('act', lambda e, ub=ub, pu=pu: e.activation(out=ub[:, 2:TG + 2], in_=pu[:], func=AF.Copy), reads=[pu.name], writes=[ub.name + "m"])
                    p.op('act', lambda e, cb=cb, pu=pu, jg=jg: e.activation(out=cb[:], in_=pu[:], func=AF.Identity, scale=cw[:, jg, 2:3], bias=cw[:, jg, 3:4]),
                         reads=[pu.name, 'cw'], writes=[cb.name])
                    p.op('dve', lambda e, cb=cb, ub=ub, jg=jg: e.scalar_tensor_tensor(out=cb[:], in0=ub[:, 1:TG + 1], scalar=cw[:, jg, 1:2], in1=cb[:], op0=ALU.mult, op1=ALU.add),
                         reads=[ub.name, ub.name + "m", cb.name, 'cw'], writes=[cb.name])
                    p.op('dve', lambda e, cb=cb, ub=ub, jg=jg: e.scalar_tensor_tensor(out=cb[:], in0=ub[:, 0:TG], scalar=cw[:, jg, 0:1], in1=cb[:], op0=ALU.mult, op1=ALU.add),
                         reads=[ub.name, ub.name + "m", cb.name, 'cw'], writes=[cb.name])
                    p.op('dve', lambda e, ub=ub, jg=jg: e.tensor_copy(out=carry[:, jg, :], in_=ub[:, TG:TG + 2]), reads=[ub.name + "m"], writes=['carry'])
                    cres.append(cb)
                gb = gbuf[j % 2]
                p.op('act', lambda e, gb=gb, cb=cres[0]: e.activation(out=gb[:], in_=cb[:], func=AF.Gelu_apprx_tanh), reads=[cres[0].name], writes=[gb.name])
                p.op('dve', lambda e, gb=gb, cb=cres[1], j=j: e.tensor_tensor(out=gT[:, j, :], in0=gb[:], in1=cb[:], op=ALU.mult),
                     reads=[gb.name, cres[1].name], writes=[('gT', j)])
            for c in range(KC):
                w = load_wd(c)
                py = ps_y[c % 2]
                for j in range(NJ):
                    p.op('pe', lambda e, w=w, j=j, py=py: e.matmul(py[:], lhsT=w[:, j, :], rhs=gT[:, j, :], start=(j == 0), stop=(j == NJ - 1)),
                         reads=[w.name, ('gT', j)], writes=[py.name])
                p.op('act', lambda e, c=c, py=py: e.activation(out=ysb[:, c, :], in_=py[:], func=AF.Copy), reads=[py.name], writes=[('ysb', c)])
            emit_postnorm_residual(p, nc, hT, ysb, gains[:, KC:2 * KC], tg, ones_bf, ps_stat, sqs, rstd2, tmp, sqctr, epsc)
        evs = []
        for c in range(KC):
            for tg in range(NTG):
                evs.append(p.dma('sp', out_d[c, :, tg * TG:(tg + 1) * TG], hT[:, c, tg * TG:(tg + 1) * TG], reads=[(hT.name, tg)], writes=[('out', c, tg)], slot="st_out"))
        p.finish_on('sp', [('dma', 'st_out', p.dma_cnt['st_out'])])
        p.emit()
    return nc


MASKV = -30000.0
DEBUG = {}
SCALE = 0.125
NQ = 2048
NQT = 16


def build_lin(nco):
    NOC = nco // 128
    nc = bass.Bass("TRN2", target_bir_lowering=False)
    hT_d = nc.dram_tensor("hT", [KC, 128, NT], F32, kind="ExternalInput").ap()
    g_d = nc.dram_tensor("gains", [128, KC], F32, kind="ExternalInput").ap()
    w_d = nc.dram_tensor("w", [D, nco], F32, kind="ExternalInput").ap()
    out_d = nc.dram_tensor("out", [NOC, 128, NT], BF16, kind="ExternalOutput").ap()
    with contextlib.ExitStack() as st:
        sb = lambda name, shape, dt: st.enter_context(nc.sbuf_tensor(name, shape, dt))
        ps = lambda name, shape, dt: st.enter_context(nc.psum_tensor(name, shape, dt))
        hT = sb("hT_sb", [128, KC, NT], F32)
        xT = sb("xT_sb", [128, KC, NT], BF16)
        gains = sb("gains_sb", [128, KC], F32)
        ones_bf = sb("ones_bf", [128, 128], BF16)
        epsc = sb("epsc", [128, 1], F32)
        sqs = [sb(f"sqp{i}", [128, TG], BF16) for i in range(3)]
        rstd = sb("rstd", [128, TG], F32)
        wb = [sb(f"wb{i}", [128, KC, 128], BF16) for i in range(4)]
        ob = [sb(f"ob{i}", [128, NT], BF16) for i in range(3)]
        ps_stat = ps("ps_stat", [128, TG], F32)
        ps_o = [ps(f"ps_o{i}", [128, TG], F32) for i in range(4)]
        p = Prog(nc)
        for c in range(KC):
            for tg in range(NTG):
                p.dma('sp', hT[:, c, tg * TG:(tg + 1) * TG], hT_d[c, :, tg * TG:(tg + 1) * TG], writes=[(hT.name, tg)], slot=f"ld_h{tg}")
        p.dma('sp', gains[:], g_d[:, :], writes=['gains'])
        p.op('dve', lambda e: e.memset(ones_bf[:], 1.0), writes=['ones'])
        p.op('dve', lambda e: e.memset(epsc[:], EPS), writes=['epsc'])
        emit_prenorm(p, nc, hT, xT, gains[:, 0:KC], NT, sqs, rstd, ones_bf, ps_stat, epsc)
        n = 0
        evs = []
        for oc in range(NOC):
            w = wb[oc % 4]
            p.dma('pool', w[:], w_d[:, oc * 128:(oc + 1) * 128].rearrange("(kc p) n -> p kc n", p=128), writes=[w.name])
            o = ob[oc % 3]
            for tg in range(NTG):
                po = ps_o[n % 4]
                n += 1
                for kc in range(KC):
                    p.op('pe', lambda e, w=w, kc=kc, po=po, tg=tg: e.matmul(po[:], lhsT=w[:, kc, :], rhs=xT[:, kc, tg * TG:(tg + 1) * TG], start=(kc == 0), stop=(kc == KC - 1)),
                         reads=[w.name, (xT.name, tg)], writes=[po.name])
                eng = 'act' if n % 2 else 'dve'
                if eng == 'act':
                    p.op('act', lambda e, o=o, po=po, tg=tg: e.activation(out=o[:, tg * TG:(tg + 1) * TG], in_=po[:], func=AF.Copy), reads=[po.name], writes=[(o.name, tg)])
                else:
                    p.op('dve', lambda e, o=o, po=po, tg=tg: e.tensor_copy(out=o[:, tg * TG:(tg + 1) * TG], in_=po[:]), reads=[po.name], writes=[(o.name, tg)])
            evs.append(p.dma('sp', out_d[oc, :, :], o[:], reads=[(o.name, tg) for tg in range(NTG)], writes=[(o.name + "st")], slot="st_" + o.name))
            for tg in range(NTG):
                p.readers.setdefault((o.name, tg), []).append(evs[-1])
        p.finish_on('sp', [('dma', "st_" + ob[i].name, p.dma_cnt["st_" + ob[i].name]) for i in range(3) if ("st_" + ob[i].name) in p.dma_cnt])
        p.emit()
    return nc


def build_bias(ltot):
    nc = bass.Bass("TRN2", target_bir_lowering=False)
    t_d = nc.dram_tensor("table", [32, 16], F32, kind="ExternalInput").ap()
    oh_d = nc.dram_tensor("onehot", [33, ltot], F32, kind="ExternalInput").ap()
    out_d = nc.dram_tensor("out", [16, ltot], F32, kind="ExternalOutput").ap()
    nchk = (ltot + 511) // 512
    with contextlib.ExitStack() as st:
        sb = lambda name, shape, dt: st.enter_context(nc.sbuf_tensor(name, shape, dt))
        ps = lambda name, shape, dt: st.enter_context(nc.psum_tensor(name, shape, dt))
        tf = sb("tf", [64, 16], F32)
        tb = sb("tb", [64, 16], BF16)
        oh = sb("oh", [64, ltot], BF16)
        fo = sb("fo", [16, ltot], F32)
        pp = [ps(f"pp{i}", [16, 512], F32) for i in range(2)]
        p = Prog(nc)
        p.op('dve', lambda e: e.memset(tf[32:64, :], MASKV), writes=['tfm'])
        p.dma('sp', tf[0:32, :], t_d[:, :], writes=['tf'])
        p.dma('pool', oh[0:33, :], oh_d[:, :], writes=['oh'])
        p.op('dve', lambda e: e.tensor_scalar(out=tb[0:32, :], in0=tf[0:32, :], scalar1=1.0 / SCALE, scalar2=None, op0=ALU.mult), reads=['tf'], writes=['tb'])
        p.op('dve', lambda e: e.tensor_copy(out=tb[32:64, :], in_=tf[32:64, :]), reads=['tfm'], writes=['tbm'])
        for k in range(nchk):
            n = min(512, ltot - k * 512)
            q = pp[k % 2]
            p.op('pe', lambda e, k=k, n=n, q=q: e.matmul(q[:, 0:n], lhsT=tb[0:33, :], rhs=oh[0:33, k * 512:k * 512 + n], start=True, stop=True),
                 reads=['tb', 'tbm', 'oh'], writes=[q.name])
            p.op('act', lambda e, k=k, n=n, q=q: e.activation(out=fo[:, k * 512:k * 512 + n], in_=q[:, 0:n], func=AF.Copy), reads=[q.name], writes=['fo'])
        ev = p.dma('sp', out_d[:, :], fo[:], reads=['fo'], writes=['out'])
        p.finish_on('sp', [ev])
        p.emit()
    return nc


def build_att(cfg):
    npr, nkch, nkt, nkeys = cfg['npr'], cfg['nkch'], cfg['nkt'], cfg['nkeys']
    ltot, lc = cfg.get('ltot', 0), cfg.get('lc', 0)
    imp = cfg.get('imp', False)
    anymask = cfg.get('mask', False)
    nc = bass.Bass("TRN2", target_bir_lowering=False)
    q_d = nc.dram_tensor("QT", [KC, 128, NQ], BF16, kind="ExternalInput").ap() if cfg.get('sharedq', False) else None
    qs_d = None if q_d is not None else nc.dram_tensor("QT", [npr, KC, 128, NQ], BF16, kind="ExternalInput").ap()
    k_d = nc.dram_tensor("KT", [npr, nkch, 128, 2, nkeys], BF16, kind="ExternalInput").ap()
    v_d = nc.dram_tensor("V", [npr, nkch, 128, nkt, 128], BF16, kind="ExternalInput").ap()
    hf_d = nc.dram_tensor("hflag", [128, 128], BF16, kind="ExternalInput").ap()
    jm_d = nc.dram_tensor("jmat", [128, 128], BF16, kind="ExternalInput").ap()
    fv_d = nc.dram_tensor("fvec", [16, ltot], F32, kind="ExternalInput") if ltot else None
    cv_d = nc.dram_tensor("cvec", [2, lc], F32, kind="ExternalInput") if lc else None
    if anymask:
        e_d = nc.dram_tensor("emat", [64, nkt * 128], BF16, kind="ExternalInput").ap()
        nm_d = nc.dram_tensor("nmT", [2, 64, NQ], BF16, kind="ExternalInput").ap()
    if imp:
        ov_d = nc.dram_tensor("ov65", [128, 2, 65], BF16, kind="ExternalInput").ap()
        sbias_d = nc.dram_tensor("selbias", [128, NQT, 64], F32, kind="ExternalInput").ap()
        nmo_d = nc.dram_tensor("negmask", [128, NQT, 2, 64], BF16, kind="ExternalOutput").ap()
    u_d = nc.dram_tensor("U", [npr, KC, 128, NQT, 128], F32, kind="ExternalOutput").ap()
    d_d = nc.dram_tensor("Dn", [npr, KC, 1, NQT, 256], F32, kind="ExternalOutput").ap()
    maxb = max(len(b) for b in cfg['bias'])
    with contextlib.ExitStack() as st:
        sb = lambda name, shape, dt: st.enter_context(nc.sbuf_tensor(name, shape, dt))
        ps = lambda name, shape, dt: st.enter_context(nc.psum_tensor(name, shape, dt))
        qt = [sb(f"qt{i}", [128, NQ], BF16) for i in range(2)]
        kt = [sb(f"kt{i}", [128, 2, nkeys], BF16) for i in range(2)]
        vt = [sb(f"vt{i}", [128, nkt, 128], BF16) for i in range(2)]
        bt = [sb(f"bt{i}", [128, maxb, 256], BF16) for i in range(2)]
        hflag = sb("hflag_sb", [128, 128], BF16)
        ones1 = sb("ones1", [128, 128], BF16)
        jm = sb("jm_sb", [128, 128], BF16)
        pb = [sb(f"pb{i}", [128, 512], BF16) for i in range(4)]
        ub = [sb(f"ub{i}", [128, NQT, 256], F32) for i in range(2)]
        db = [sb(f"db{i}", [128, NQT, 256], F32) for i in range(2)]
        if anymask:
            em = sb("em_sb", [64, nkt * 128], BF16)
            nm = sb("nm_sb", [64, 2, NQ], BF16)
        if imp:
            ov = sb("ov_sb", [128, 2, 65], BF16)
            sbias = sb("sbias_sb", [128, NQT, 64], F32)
            impacc = sb("impacc", [128, NQT, 2, 64], F32)
            nmb = sb("nmb", [128, NQT, 2, 64], BF16)
            rden = sb("rden", [128, 2], F32)
            mx = sb("mx", [128, 16], F32)
            scr = sb("scr", [128, 64], F32)
            scr2 = sb("scr2", [128, 64], F32)
            ps_i = [ps(f"ps_i{i}", [128, 2, 65], F32) for i in range(2)]
        ps_s = [ps(f"ps_s{i}", [128, 512], F32) for i in range(2)]
        ps_a = [ps(f"ps_a{i}", [128, 256], F32) for i in range(2)]
        ps_d = [ps(f"ps_d{i}", [128, 256], F32) for i in range(2)]

        p = Prog(nc)
        p.dma('sp', hflag[:], hf_d[:, :], writes=['hflag'])
        p.dma('sp', jm[:], jm_d[:, :], writes=['jm'])
        p.op('dve', lambda e: e.memset(ones1[:], 1.0), writes=['ones1'])
        if anymask:
            p.dma('sp', em[:], e_d[:, :], writes=['em'])
            p.dma('sp', nm[:], nm_d.rearrange("g j q -> j g q"), writes=['nm'])
        if imp:
            p.dma('sp', ov[:], ov_d[:, :, :], writes=['ov'])
            p.dma('sp', sbias[:], sbias_d[:, :, :], writes=['sbias'])
            p.op('pool', lambda e: e.memset(impacc[:], 0.0), writes=['impacc'])
        cnt = dict(q=0, k=0, b=0, s=0, a=0, pb=0, u=0, i=0)
        lastk = [None, None]
        out_evs = []
        for pr in range(npr):
            blist = cfg['bias'][pr]
            plan = cfg['plans'][pr]
            for c in range(KC):
                Q = qt[cnt['q'] % 2]
                cnt['q'] += 1
                src = q_d[c, :, :] if q_d is not None else qs_d[pr, c, :, :]
                p.dma('sp', Q[:], src, writes=[Q.name])
                kc = cfg['kch'](c)
                if lastk[0] != (pr, kc):
                    Kt = kt[cnt['k'] % 2]
                    Vt = vt[cnt['k'] % 2]
                    cnt['k'] += 1
                    for h_ in range(2):
                        for k0_ in range(0, nkeys, 2048):
                            k1_ = min(nkeys, k0_ + 2048)
                            p.dma('sp', Kt[:, h_, k0_:k1_], k_d[pr, kc, :, h_, k0_:k1_], writes=[Kt.name], slot=Kt.name)
                    for t0_ in range(0, nkt, 16):
                        t1_ = min(nkt, t0_ + 16)
                        p.dma('sp', Vt[:, t0_:t1_, :], v_d[pr, kc, :, t0_:t1_, :], writes=[Vt.name], slot=Vt.name)
                    lastk = [(pr, kc), (Kt, Vt)]
                Kt, Vt = lastk[1]
                B = bt[cnt['b'] % 2]
                cnt['b'] += 1
                for bi, (kind, a, b_) in enumerate(blist):
                    if DEBUG.get('nobias'):
                        p.dma('sp', B[:, bi, :], jm_d[:, :].rearrange("p (a q) -> p a q", a=1).broadcast_to([128, 2, 128]) if False else hf_d[:, :], writes=[B.name], slot=B.name) if False else None
                        continue
                    if kind == 'f':
                        srcb = bass.AP(fv_d, (2 * c) * ltot + a + b_, [[1, 128], [ltot, 2], [1, 128]])
                        p.dma('pool', B[:, bi, :].rearrange("p (h q) -> p h q", h=2), srcb, writes=[B.name], slot=B.name)
                    else:
                        for h in range(2):
                            srcb = bass.AP(cv_d, a * lc + b_, [[16, 128], [1, 128]])
                            p.dma('pool', B[:, bi, h * 128:(h + 1) * 128], srcb, writes=[B.name], slot=B.name)
                U = ub[cnt['u'] % 2]
                Dd = db[cnt['u'] % 2]
                cnt['u'] += 1
                g = c // 4
                pending = [None]

                def flush():
                    if pending[0] is not None:
                        pending[0]()
                        pending[0] = None

                def make_pv(acc, accd, P, grp, k0, nk, i, U, Dd, Vt):
                    def run():
                        for s_, (ktile, bidx, um, halo) in enumerate(grp):
                            kidx = k0 + s_
                            col = s_ * 256
                            on = hflag if halo else ones1
                            p.op('pe', lambda e, acc=acc, Vt=Vt, ktile=ktile, P=P, col=col, kidx=kidx, nk=nk: e.matmul(acc[:, 0:256], lhsT=Vt[:, ktile, :], rhs=P[:, col:col + 256], start=(kidx == 0), stop=(kidx == nk - 1)),
                                 reads=[Vt.name, P.name], writes=[acc.name])
                            p.op('pe', lambda e, accd=accd, on=on, P=P, col=col, kidx=kidx, nk=nk: e.matmul(accd[:, 0:256], lhsT=on[:], rhs=P[:, col:col + 256], start=(kidx == 0), stop=(kidx == nk - 1)),
                                 reads=['hflag', 'ones1', P.name], writes=[accd.name])
                        if imp:
                            pi = ps_i[cnt['i'] % 2]
                            cnt['i'] += 1
                            for h in range(2):
                                for s_, (ktile, bidx, um, halo) in enumerate(grp):
                                    p.op('pe', lambda e, pi=pi, h=h, P=P, s_=s_, ktile=ktile, ng=len(grp): e.matmul(pi[:, h, :], lhsT=P[:, s_ * 256 + h * 128:s_ * 256 + (h + 1) * 128], rhs=ov[:, ktile, :], start=(s_ == 0), stop=(s_ == ng - 1)),
                                         reads=[P.name, 'ov'], writes=[pi.name])
                            p.op('dve', lambda e, pi=pi: e.tensor_scalar(out=rden[:], in0=pi[:, :, 64], scalar1=1e-30, scalar2=None, op0=ALU.add), reads=[pi.name], writes=['rden'])
                            p.op('dve', lambda e: e.reciprocal(out=rden[:], in_=rden[:]), reads=['rden'], writes=['rden'])
                            for h in range(2):
                                p.op('dve', lambda e, pi=pi, h=h, i=i, g=g: e.scalar_tensor_tensor(out=impacc[:, i, g, :], in0=pi[:, h, 0:64], scalar=rden[:, h:h + 1], in1=impacc[:, i, g, :], op0=ALU.mult, op1=ALU.add),
                                     reads=[pi.name, 'rden', 'impacc'], writes=['impacc'])
                        if k0 + len(grp) == nk:
                            p.op('dve', lambda e, U=U, acc=acc, i=i: e.tensor_copy(out=U[:, i, :], in_=acc[:, 0:256]), reads=[acc.name], writes=[U.name])
                            p.op('dve', lambda e, Dd=Dd, accd=accd, i=i: e.tensor_copy(out=Dd[:, i, :], in_=accd[:, 0:256]), reads=[accd.name], writes=[Dd.name])
                    return run

                for i in range(NQT):
                    kts = plan(i)
                    acc = ps_a[cnt['a'] % 2]
                    accd = ps_d[cnt['a'] % 2]
                    cnt['a'] += 1
                    qsl = slice(i * 128, (i + 1) * 128)
                    nk = len(kts)
                    for k0 in range(0, nk, 2):
                        grp = kts[k0:k0 + 2]
                        S = ps_s[cnt['s'] % 2]
                        cnt['s'] += 1
                        for s_, (ktile, bidx, um, halo) in enumerate(grp):
                            ksl = slice(ktile * 128, (ktile + 1) * 128)
                            col = s_ * 256
                            p.op('pe', lambda e, S=S, col=col, B=B, bidx=bidx: e.matmul(S[:, col:col + 256], lhsT=jm[:], rhs=B[:, bidx, :], start=True, stop=False),
                                 reads=['jm', B.name], writes=[S.name])
                            for h in range(2):
                                p.op('pe', lambda e, S=S, col=col, h=h, Kt=Kt, Q=Q, ksl=ksl, qsl=qsl, last=((h == 1) and not um): e.matmul(S[:, col + h * 128:col + (h + 1) * 128], lhsT=Kt[:, h, ksl], rhs=Q[:, qsl], start=False, stop=last),
                                     reads=[Kt.name, Q.name], writes=[S.name])
                            if um:
                                for h in range(2):
                                    p.op('pe', lambda e, S=S, col=col, h=h, ksl=ksl, qsl=qsl, g=g: e.matmul(S[:, col + h * 128:col + (h + 1) * 128], lhsT=em[:, ksl], rhs=nm[:, g, qsl], start=False, stop=(h == 1)),
                                         reads=['em', 'nm'], writes=[S.name])
                        P = pb[cnt['pb'] % 4]
                        cnt['pb'] += 1
                        ncol = 256 * len(grp)
                        p.op('act', lambda e, P=P, S=S, ncol=ncol: e.activation(out=P[:, 0:ncol], in_=S[:, 0:ncol], func=AF.Exp, scale=SCALE),
                             reads=[S.name], writes=[P.name])
                        flush()
                        pending[0] = make_pv(acc, accd, P, grp, k0, nk, i, U, Dd, Vt)
                flush()
                for i0_ in range(0, NQT, 8):
                    ev1 = p.dma('sp', u_d[pr, c, 0:64, i0_:i0_ + 8, :], U[0:64, i0_:i0_ + 8, 0:128], reads=[U.name], writes=[U.name + "st"], slot="st_" + U.name)
                    ev1 = p.dma('sp', u_d[pr, c, 64:128, i0_:i0_ + 8, :], U[64:128, i0_:i0_ + 8, 128:256], reads=[U.name], writes=[U.name + "st"], slot="st_" + U.name)
                ev2 = p.dma('sp', d_d[pr, c, :, :, :], Dd[0:1, :, :], reads=[Dd.name], writes=[Dd.name + "st"], slot="st_" + Dd.name)
                out_evs += [ev1, ev2]
        if imp:
            for i in range(NQT):
                for g in range(2):
                    p.op('dve', lambda e, i=i, g=g: e.tensor_tensor(out=scr[:], in0=impacc[:, i, g, :], in1=sbias[:, i, :], op=ALU.add), reads=['impacc', 'sbias'], writes=['scr'])
                    p.op('dve', lambda e: e.max(out=mx[:, 0:8], in_=scr[:]), reads=['scr'], writes=['mx'])
                    p.op('dve', lambda e: e.match_replace(out=scr2[:], in_to_replace=mx[:, 0:8], in_values=scr[:], imm_value=-3e38), reads=['scr', 'mx'], writes=['scr2'])
                    p.op('dve', lambda e: e.max(out=mx[:, 8:16], in_=scr2[:]), reads=['scr2'], writes=['mx2'])
                    p.op('dve', lambda e, i=i, g=g: e.tensor_scalar(out=impacc[:, i, g, :], in0=scr[:], scalar1=mx[:, 15:16], scalar2=MASKV, op0=ALU.is_lt, op1=ALU.mult),
                         reads=['scr', 'mx2'], writes=['impacc'])
            p.op('dve', lambda e: e.tensor_copy(out=nmb[:], in_=impacc[:]), reads=['impacc'], writes=['nmb'])
            out_evs.append(p.dma('sp', nmo_d[:, :, :, :], nmb[:], reads=['nmb'], writes=['nmo']))
        p.finish_on('sp', out_evs[-1:] + [('dma', sl, p.dma_cnt[sl]) for sl in p.dma_cnt if str(sl).startswith('st_')])
        p.emit()
    return nc


def build_oproj(branches, has_gate, has_sink):
    nu = sum(len(b) for b in branches)
    nb = len(branches)
    nc = bass.Bass("TRN2", target_bir_lowering=False)
    u_d = nc.dram_tensor("U", [nu, KC, 128, NT], F32, kind="ExternalInput").ap()
    d_d = nc.dram_tensor("Dn", [nu, KC, 128, NT], F32, kind="ExternalInput").ap()
    if has_gate:
        gt_d = nc.dram_tensor("gate", [nb, KC, 128, NT], BF16, kind="ExternalInput").ap()
    if has_sink:
        sk_d = nc.dram_tensor("sink", [128, KC], F32, kind="ExternalInput").ap()
    wo_d = nc.dram_tensor("w_o", [D, D], F32, kind="ExternalInput").ap()
    hT_d = nc.dram_tensor("hT", [KC, 128, NT], F32, kind="ExternalInput").ap()
    g_d = nc.dram_tensor("gains", [128, KC], F32, kind="ExternalInput").ap()
    out_d = nc.dram_tensor("out", [KC, 128, NT], F32, kind="ExternalOutput").ap()
    with contextlib.ExitStack() as st:
        sb = lambda name, shape, dt: st.enter_context(nc.sbuf_tensor(name, shape, dt))
        ps = lambda name, shape, dt: st.enter_context(nc.psum_tensor(name, shape, dt))
        wo = sb("wo_sb", [128, KC, D], BF16)
        gains = sb("gains_sb", [128, KC], F32)
        ones_bf = sb("ones_bf", [128, 128], BF16)
        epsc = sb("epsc", [128, 1], F32)
        sink = sb("sink_sb", [128, KC], F32)
        ul = [sb(f"ul{i}", [128, TG], F32) for i in range(4)]
        dl = [sb(f"dl{i}", [128, TG], F32) for i in range(4)]
        gl = [sb(f"gl{i}", [128, TG], BF16) for i in range(2)]
        sg = sb("sg", [128, TG], F32)
        us = sb("us", [128, TG], F32)
        ds = sb("ds", [128, TG], F32)
        oacc = sb("oacc", [128, TG], F32)
        oT = sb("oT", [128, KC, TG], BF16)
        hres = sb("hres", [128, KC, TG], F32)
        ysb = sb("ysb", [128, KC, TG], F32)
        sqs = [sb(f"sqp{i}", [128, TG], BF16) for i in range(3)]
        rstd = sb("rstd", [128, TG], F32)
        tmp = sb("tmpn", [128, TG], F32)
        ps_stat = ps("ps_stat", [128, TG], F32)
        ps_y = [ps(f"ps_y{i}", [128, TG], F32) for i in range(2)]
        p = Prog(nc)
        for kc in range(KC):
            p.dma('pool', wo[:, kc, :], wo_d[kc * 128:(kc + 1) * 128, :], writes=['wo'], slot='wo')
        p.dma('sp', gains[:], g_d[:, :], writes=['gains'])
        p.op('dve', lambda e: e.memset(ones_bf[:], 1.0), writes=['ones'])
        p.op('dve', lambda e: e.memset(epsc[:], EPS), writes=['epsc'])
        if has_sink:
            p.dma('sp', sink[:], sk_d[:, :], writes=['sink'])
            p.op('act', lambda e: e.activation(out=sink[:], in_=sink[:], func=AF.Exp), reads=['sink'], writes=['sink'])
        n = dict(u=0, g=0)
        sqctr = [0]
        evs = []
        for tg in range(NTG):
            tsl = slice(tg * TG, (tg + 1) * TG)
            p.dma('sp', hres[:], hT_d[:, :, tsl].rearrange("c p t -> p c t"), writes=[(hres.name, 0)])
            for c in range(KC):
                ui = 0
                for b, idxs in enumerate(branches):
                    for k, _ in enumerate(idxs):
                        u_ = ul[n['u'] % 4]
                        d_ = dl[n['u'] % 4]
                        n['u'] += 1
                        p.dma('sp', u_[:], u_d[ui, c, :, tsl], writes=[u_.name])
                        p.dma('sp', d_[:], d_d[ui, c, :, tsl], writes=[d_.name])
                        ui += 1
                        if k == 0:
                            p.op('dve', lambda e, u_=u_: e.tensor_copy(out=us[:], in_=u_[:]), reads=[u_.name], writes=['us'])
                            p.op('pool', lambda e, d_=d_: e.tensor_scalar(out=ds[:], in0=d_[:], scalar1=1e-30, scalar2=None, op0=ALU.add), reads=[d_.name], writes=['ds'])
                        else:
                            p.op('dve', lambda e, u_=u_: e.tensor_tensor(out=us[:], in0=us[:], in1=u_[:], op=ALU.add), reads=[u_.name, 'us'], writes=['us'])
                            p.op('pool', lambda e, d_=d_: e.tensor_tensor(out=ds[:], in0=ds[:], in1=d_[:], op=ALU.add), reads=[d_.name, 'ds'], writes=['ds'])
                    if has_sink:
                        p.op('pool', lambda e, c=c: e.tensor_scalar(out=ds[:], in0=ds[:], scalar1=sink[:, c:c + 1], scalar2=None, op0=ALU.add), reads=['ds', 'sink'], writes=['ds'])
                    p.op('dve', lambda e: e.reciprocal(out=ds[:], in_=ds[:]), reads=['ds'], writes=['ds'])
                    p.op('dve', lambda e: e.tensor_tensor(out=us[:], in0=us[:], in1=ds[:], op=ALU.mult), reads=['us', 'ds'], writes=['us'])
                    if has_gate:
                        g_ = gl[n['g'] % 2]
                        n['g'] += 1
                        p.dma('sp', g_[:], gt_d[b, c, :, tsl], writes=[g_.name])
                        p.op('act', lambda e, g_=g_: e.activation(out=sg[:], in_=g_[:], func=AF.Sigmoid), reads=[g_.name], writes=['sg'])
                        p.op('dve', lambda e: e.tensor_tensor(out=us[:], in0=us[:], in1=sg[:], op=ALU.mult), reads=['us', 'sg'], writes=['us'])
                    last = (b == nb - 1)
                    dst = oT[:, c, :] if (last and nb == 1) else oacc[:]
                    dkey = ('oT', c) if (last and nb == 1) else 'oacc'
                    if b == 0:
                        p.op('dve', lambda e, dst=dst: e.tensor_copy(out=dst, in_=us[:]), reads=['us'], writes=[dkey])
                    elif not last:
                        p.op('dve', lambda e: e.tensor_tensor(out=oacc[:], in0=oacc[:], in1=us[:], op=ALU.add), reads=['us', 'oacc'], writes=['oacc'])
                    else:
                        p.op('dve', lambda e, c=c: e.tensor_tensor(out=oT[:, c, :], in0=oacc[:], in1=us[:], op=ALU.add), reads=['us', 'oacc'], writes=[('oT', c)])
            for oc in range(KC):
                py = ps_y[oc % 2]
                for c in range(KC):
                    p.op('pe', lambda e, py=py, c=c, oc=oc: e.matmul(py[:], lhsT=wo[:, c, oc * 128:(oc + 1) * 128], rhs=oT[:, c, :], start=(c == 0), stop=(c == KC - 1)),
                         reads=['wo', ('oT', c)], writes=[py.name])
                p.op('act', lambda e, oc=oc, py=py: e.activation(out=ysb[:, oc, :], in_=py[:], func=AF.Copy), reads=[py.name], writes=[('ysb', oc)])
            emit_postnorm_residual(p, nc, hres, ysb, gains[:, 0:KC], 0, ones_bf, ps_stat, sqs, rstd, tmp, sqctr, epsc)
            evs.append(p.dma('sp', out_d[:, :, tsl].rearrange("c p t -> p c t"), hres[:], reads=[(hres.name, 0)], writes=[('out', tg)], slot='st_out'))
            p.readers.setdefault((hres.name, 0), []).append(evs[-1])
        p.finish_on('sp', [('dma', 'st_out', p.dma_cnt['st_out'])])
        p.emit()
    return nc


def build_cmp():
    NB = 512
    nc = bass.Bass("TRN2", target_bir_lowering=False)
    b_d = nc.dram_tensor("blocksT", [16, 128, NB], BF16, kind="ExternalInput").ap()
    pos_d = nc.dram_tensor("posT", [128, 16], F32, kind="ExternalInput").ap()
    w1_d = nc.dram_tensor("w1", [2048, 256], F32, kind="ExternalInput").ap()
    w2_d = nc.dram_tensor("w2", [256, 64], F32, kind="ExternalInput").ap()
    out_d = nc.dram_tensor("out", [64, NB], BF16, kind="ExternalOutput").ap()
    with contextlib.ExitStack() as st:
        sb = lambda name, shape, dt: st.enter_context(nc.sbuf_tensor(name, shape, dt))
        ps = lambda name, shape, dt: st.enter_context(nc.psum_tensor(name, shape, dt))
        bl = sb("bl", [128, 16, NB], BF16)
        posf = sb("posf", [128, 16], F32)
        posb = sb("posb", [128, 16], BF16)
        w1 = sb("w1_sb", [128, 16, 256], BF16)
        w2 = sb("w2_sb", [128, 2, 64], BF16)
        b1 = sb("b1", [128, 2], F32)
        g1 = sb("g1", [128, 2, NB], BF16)
        ob = sb("ob", [64, NB], BF16)
        ps_h = [ps(f"ps_h{i}", [128, NB], F32) for i in range(2)]
        ps_b = ps("ps_b", [128, 2], F32)
        ps_o = ps("ps_o", [64, NB], F32)
        p = Prog(nc)
        p.dma('sp', bl[:], b_d.rearrange("c p n -> p c n"), writes=['bl'])
        p.dma('sp', posf[:], pos_d[:, :], writes=['posf'])
        p.dma('pool', w1[:], w1_d.rearrange("(c p) n -> p c n", p=128), writes=['w1'])
        p.dma('pool', w2[:], w2_d.rearrange("(c p) n -> p c n", p=128), writes=['w2'])
        p.op('dve', lambda e: e.tensor_copy(out=posb[:], in_=posf[:]), reads=['posf'], writes=['posb'])
        for m in range(2):
            for c in range(16):
                p.op('pe', lambda e, m=m, c=c: e.matmul(ps_b[:, m:m + 1], lhsT=w1[:, c, m * 128:(m + 1) * 128], rhs=posb[:, c:c + 1], start=(c == 0), stop=(c == 15)),
                     reads=['w1', 'posb'], writes=['ps_b'])
        p.op('dve', lambda e: e.tensor_copy(out=b1[:], in_=ps_b[:]), reads=['ps_b'], writes=['b1'])
        for m in range(2):
            for c in range(16):
                p.op('pe', lambda e, m=m, c=c: e.matmul(ps_h[m][:], lhsT=w1[:, c, m * 128:(m + 1) * 128], rhs=bl[:, c, :], start=(c == 0), stop=(c == 15)),
                     reads=['w1', 'bl'], writes=[ps_h[m].name])
            p.op('act', lambda e, m=m: e.activation(out=g1[:, m, :], in_=ps_h[m][:], func=AF.Gelu_apprx_tanh, bias=b1[:, m:m + 1]),
                 reads=[ps_h[m].name, 'b1'], writes=[('g1', m)])
        for m in range(2):
            p.op('pe', lambda e, m=m: e.matmul(ps_o[:], lhsT=w2[:, m, :], rhs=g1[:, m, :], start=(m == 0), stop=(m == 1)), reads=['w2', ('g1', m)], writes=['ps_o'])
        p.op('act', lambda e: e.activation(out=ob[:], in_=ps_o[:], func=AF.Copy), reads=['ps_o'], writes=['ob'])
        ev = p.dma('sp', out_d[:, :], ob[:], reads=['ob'], writes=['out'])
        p.finish_on('sp', [ev])
        p.emit()
    return nc


import ml_dtypes
BF = ml_dtypes.bfloat16

DIL = (1, 4, 16)
VARIANTS = [("A1", 1, 128, 2), ("A2", 4, 128, 2), ("A3", 16, 128, 2), ("B", 1, 127, 2), ("CW", 1, 511, 5), ("CS", 1, 10 ** 9, 14)]
VBASE = {}
_o = 0
for _n, _s, _m, _k in VARIANTS:
    VBASE[_n] = _o
    _o += 128 * _k + 128
LTOT = _o
LC = 4224
_PROGS = {}


def _prog(key, fn):
    if key not in _PROGS:
        _PROGS[key] = fn()
    return _PROGS[key]


def _run(nc, maps):
    res = run_bass_kernel_spmd(nc, maps, core_ids=list(range(8)))
    return res.results


def _t5_bucket(d):
    d = np.maximum(d, 0)
    df = np.maximum(d, 1).astype(np.float32)
    large = 16 + (np.log(df / np.float32(16)) / np.float32(np.log(128.0)) * np.float32(16)).astype(np.int32)
    large = np.minimum(large, 31)
    return np.where(d < 16, d, large)


def _onehot():
    oh = np.zeros((33, LTOT), np.float32)
    for name, stride, maxd, noff in VARIANTS:
        L = 128 * noff + 128
        d = np.arange(L) - 127
        ok = (d >= 0) & (d <= maxd)
        b = _t5_bucket(np.clip(d, 0, None) * stride)
        rows = np.where(ok, b, 32)
        oh[rows, VBASE[name] + np.arange(L)] = 1.0
    return oh


def _to_cores(h):
    out = []
    for k in range(8):
        b, half = k // 2, k % 2
        own = h[b, half * 2048:(half + 1) * 2048]
        out.append(np.ascontiguousarray(own.T.reshape(8, 128, 2048)))
    return out


def _from_cores(lst, dtype=np.float32):
    ncol = lst[0].shape[0] * 128
    out = np.zeros((4, 4096, ncol), dtype)
    for k in range(8):
        b, half = k // 2, k % 2
        out[b, half * 2048:(half + 1) * 2048] = lst[k].reshape(ncol, 2048).T
    return out


def _gcol(g):
    return np.ascontiguousarray(g.reshape(8, 128).T.astype(np.float32))


def _lin(h, gain, w):
    nco = ((w.shape[1] + 127) // 128) * 128
    if nco != w.shape[1]:
        w = np.concatenate([w, np.zeros((w.shape[0], nco - w.shape[1]), np.float32)], axis=1)
    nc = _prog(("lin", nco), lambda: build_lin(nco))
    hT = _to_cores(h)
    w = np.ascontiguousarray(w)
    res = _run(nc, [dict(hT=hT[k], gains=_gcol(gain), w=w) for k in range(8)])
    return _from_cores([r["out"] for r in res], BF)


def _jmat():
    return np.ascontiguousarray(np.eye(128, dtype=np.float32)[::-1]).astype(BF)


def _hflag(half):
    return np.full((128, 128), float(half), np.float32).astype(BF)


def _oproj(key, branches, U, Dn, h, gain, w_o, gate=None, sink=None):
    nc = _prog(("oproj", key), lambda: build_oproj(branches, gate is not None, sink is not None))
    hT = _to_cores(h)
    maps = []
    for k in range(8):
        m = dict(U=U[k], Dn=Dn[k], w_o=np.ascontiguousarray(w_o), hT=hT[k], gains=_gcol(gain))
        if gate is not None:
            m['gate'] = gate[k]
        if sink is not None:
            m['sink'] = sink
        maps.append(m)
    res = _run(nc, maps)
    return _from_cores([r["out"] for r in res])


def _split_k(kT):
    out = np.zeros(kT.shape[:-1] + (2, kT.shape[-1]), kT.dtype)
    out[..., :64, 0, :] = kT[..., :64, :]
    out[..., 64:, 1, :] = kT[..., 64:, :]
    return out


def _sel(u):
    return np.ascontiguousarray(u.reshape(u.shape[0], 8, 128, 2048))


def _seld(d):
    npr = d.shape[0]
    t = d.reshape(npr, 8, 16, 2, 128).transpose(0, 1, 3, 2, 4)
    t = np.broadcast_to(t[:, :, :, None], (npr, 8, 2, 64, 16, 128))
    return np.ascontiguousarray(t.reshape(npr, 8, 128, 2048))


def _dup_kT(kk):
    t = np.ascontiguousarray(kk.T)
    return np.concatenate([t, t], axis=0)


def _v_tiles(vv128, nkt_pad):
    nkt = vv128.shape[0] // 128
    out = np.zeros((128, nkt_pad, 128), vv128.dtype)
    out[:, :nkt] = vv128.reshape(nkt, 128, 128).transpose(1, 0, 2)
    return out


def _mixer_A(h, gains, w_in, w_o, fvec):
    P = _lin(h, gains[0], w_in)
    qkv = P.reshape(4, 4096, 3, 3, 16, 64)
    plans, biases = [], []
    for gi, dil in enumerate(DIL):
        tpr = 1 + 16 // dil
        nq = 16 // dil

        def plan(i, tpr=tpr, nq=nq):
            r, jq = i // nq, i % nq
            return [(r * tpr + jq, 1, False, jq == 0), (r * tpr + jq + 1, 0, False, False)]
        plans.append(plan)
        vb = VBASE["A%d" % (gi + 1)]
        biases.append([('f', vb, 0), ('f', vb, 128)])
    cfg = dict(npr=3, nkch=8, nkt=32, nkeys=4096, kch=lambda c: c, plans=plans, bias=biases, ltot=LTOT)
    nc = _prog("attA", lambda: build_att(cfg))
    maps = []
    for k in range(8):
        b, half = k // 2, k % 2
        QT = np.zeros((3, 8, 128, 2048), BF)
        KT = np.zeros((3, 8, 128, 4096), BF)
        V = np.zeros((3, 8, 128, 32, 128), BF)
        for gi, dil in enumerate(DIL):
            L, Lo = 4096 // dil, 2048 // dil

            def strided(t):
                return t.reshape(L, dil, 16, 64).transpose(1, 0, 2, 3)
            q, kk, vv = (strided(qkv[b, :, gi, i]) for i in range(3))
            own = slice(half * Lo, (half + 1) * Lo)
            QT[gi] = q[:, own].reshape(2048, 8, 128).transpose(1, 2, 0)
            if half == 0:
                kh = np.zeros((dil, 128, 16, 64), BF)
                vh = np.zeros((dil, 128, 16, 64), BF)
            else:
                kh, vh = kk[:, Lo - 128:Lo], vv[:, Lo - 128:Lo]
            kc = np.concatenate([kh, kk[:, own]], axis=1)
            vc = np.concatenate([vh, vv[:, own]], axis=1)
            nkeys = dil * (128 + Lo)
            KT[gi, :, :, :nkeys] = kc.reshape(nkeys, 8, 128).transpose(1, 2, 0)
            nkt = nkeys // 128
            V[gi, :, :, :nkt] = vc.reshape(nkt, 128, 8, 128).transpose(2, 1, 0, 3)
        maps.append(dict(QT=QT, KT=_split_k(KT), V=V, hflag=_hflag(half), jmat=_jmat(), fvec=fvec))
    res = _run(nc, maps)
    U, Dn = [], []
    for k in range(8):
        u, d = _sel(res[k]["U"]), _seld(res[k]["Dn"])
        uu = np.zeros_like(u)
        dd = np.zeros_like(d)
        for gi, dil in enumerate(DIL):
            Lo = 2048 // dil
            uu[gi] = u[gi].reshape(8, 128, dil, Lo).transpose(0, 1, 3, 2).reshape(8, 128, 2048)
            dd[gi] = d[gi].reshape(8, 128, dil, Lo).transpose(0, 1, 3, 2).reshape(8, 128, 2048)
        U.append(uu)
        Dn.append(dd)
    return _oproj("A", [[0, 1, 2]], U, Dn, h, gains[1], w_o)


def _mixer_B(h, gains, w_in, sinks, w_o, fvec):
    P = _lin(h, gains[0], w_in)
    q = P[..., :1024]
    kx = P[..., 1024:1152].reshape(4, 4096, 2, 64)
    vx = P[..., 1152:1280].reshape(4, 4096, 2, 64)
    vb = VBASE["B"]
    cfg = dict(npr=1, nkch=2, nkt=17, nkeys=2176, kch=lambda c: c // 4,
               plans=[lambda i: [(i, 1, False, i == 0), (i + 1, 0, False, False)]],
               bias=[[('f', vb, 0), ('f', vb, 128)]], ltot=LTOT)
    nc = _prog("attB", lambda: build_att(cfg))
    maps = []
    for k in range(8):
        b, half = k // 2, k % 2
        S0 = half * 2048
        QT = np.ascontiguousarray(q[b, S0:S0 + 2048].reshape(2048, 8, 128).transpose(1, 2, 0))[None]
        KT = np.zeros((1, 2, 128, 2176), BF)
        V = np.zeros((1, 2, 128, 17, 128), BF)
        for g in range(2):
            if half == 0:
                kh, vh = np.zeros((128, 64), BF), np.zeros((128, 64), BF)
            else:
                kh, vh = kx[b, S0 - 128:S0, g], vx[b, S0 - 128:S0, g]
            kk = np.concatenate([kh, kx[b, S0:S0 + 2048, g]], axis=0)
            vv = np.concatenate([vh, vx[b, S0:S0 + 2048, g]], axis=0)
            KT[0, g] = _dup_kT(kk)
            V[0, g] = _v_tiles(np.concatenate([vv, vv], axis=1), 17)
        maps.append(dict(QT=QT, KT=_split_k(KT), V=V, hflag=_hflag(half), jmat=_jmat(), fvec=fvec))
    res = _run(nc, maps)
    U = [_sel(r["U"]) for r in res]
    Dn = [_seld(r["Dn"]) for r in res]
    sink = np.ascontiguousarray(np.repeat(sinks.reshape(8, 2), 64, axis=1).T.astype(np.float32))
    return _oproj("B", [[0]], U, Dn, h, gains[1], w_o, sink=sink)


def _mixer_C(h, gains, w_in, cmp_pos, cmp_w1, cmp_w2, w_o, fvec):
    P = _lin(h, gains[0], w_in)
    q = P[..., :1024]
    kv = P[..., 1024:1792].reshape(4, 4096, 6, 2, 64)
    gates = P[..., 1792:1840].reshape(4, 4096, 3, 16)
    nc = _prog("cmp", build_cmp)
    idx = np.arange(255)[:, None] * 16 + np.arange(32)[None, :]
    maps = []
    for k in range(8):
        b, i = k // 2, k % 2
        t = kv[b, :, i]
        blk = t[idx]
        blk = blk.transpose(2, 0, 1, 3).reshape(2, 255, 2048)
        full = np.zeros((2, 256, 2048), BF)
        full[:, :255] = blk
        blocksT = np.ascontiguousarray(full.reshape(512, 2048).T.reshape(16, 128, 512))
        posT = np.ascontiguousarray(cmp_pos[i].reshape(16, 128).T.astype(np.float32))
        maps.append(dict(blocksT=blocksT, posT=posT, w1=np.ascontiguousarray(cmp_w1[i]), w2=np.ascontiguousarray(cmp_w2[i])))
    res = _run(nc, maps)
    kcv = [[res[2 * b + i]["out"].reshape(64, 2, 256) for i in range(2)] for b in range(4)]
    cfg = dict(npr=1, nkch=2, nkt=2, nkeys=256, kch=lambda c: c // 4,
               plans=[lambda i: [(0, 2 * i, False, False), (1, 2 * i + 1, False, False)]],
               bias=[[('c', t, 128 * i) for i in range(16) for t in range(2)]], lc=LC, imp=True)
    nc = _prog("attCc", lambda: build_att(cfg))
    n_ = np.arange(256)
    j_ = np.arange(64)
    ovl = ((16 * n_[:, None] < (j_[None, :] + 1) * 64) & (16 * n_[:, None] + 32 > j_[None, :] * 64) & (n_[:, None] < 255)).astype(np.float32)
    ov65 = np.ones((128, 2, 65), np.float32)
    ov65[:, :, :64] = ovl.reshape(2, 128, 64).transpose(1, 0, 2)
    ov65 = ov65.astype(BF)
    maps = []
    QTs = []
    for k in range(8):
        b, half = k // 2, k % 2
        S0 = half * 2048
        QT = np.ascontiguousarray(q[b, S0:S0 + 2048].reshape(2048, 8, 128).transpose(1, 2, 0))
        QTs.append(QT)
        KT = np.zeros((1, 2, 128, 256), BF)
        V = np.zeros((1, 2, 128, 2, 128), BF)
        for g in range(2):
            kc = kcv[b][0][:, g, :]
            KT[0, g] = np.concatenate([kc, kc], axis=0)
            vc = kcv[b][1][:, g, :].T
            V[0, g] = _v_tiles(np.concatenate([vc, vc], axis=1), 2)
        cvec = np.zeros((2, LC), np.float32)
        for t in range(2):
            Z = 2048 * t + 2063 - S0
            cvec[t, :max(0, min(LC, Z))] = MASKV
        pos = S0 + np.arange(16)[None, :] * 128 + np.arange(128)[:, None]
        cur = pos // 64
        jj = np.arange(64)[None, None, :]
        forced = (jj == 0) | (jj == cur[..., None]) | (jj == cur[..., None] - 1)
        allowed = jj <= cur[..., None]
        selbias = np.where(forced, 1e9, np.where(allowed, 0.0, -1e30)).astype(np.float32)
        maps.append(dict(QT=QT[None], KT=_split_k(KT), V=V, hflag=_hflag(half), jmat=_jmat(), cvec=cvec, ov65=ov65, selbias=selbias))
    res = _run(nc, maps)
    Uc = [_sel(r["U"])[0] for r in res]
    Dc = [_seld(r["Dn"])[0] for r in res]
    negm = [r["negmask"] for r in res]
    sel_plan = lambda i: [(t, min(16 + i - t, 13), True, t < 16) for t in range(17 + i)]
    win_plan = lambda i: [(t, 4 + i - t, False, t < 4) for t in range(i, i + 5)]
    cfg = dict(npr=2, nkch=2, nkt=32, nkeys=4096, kch=lambda c: c // 4, plans=[sel_plan, win_plan],
               bias=[[('f', VBASE["CS"], 128 * o) for o in range(14)], [('f', VBASE["CW"], 128 * o) for o in range(5)]],
               ltot=LTOT, mask=True, sharedq=True)
    nc = _prog("attCs", lambda: build_att(cfg))
    maps = []
    for k in range(8):
        b, half = k // 2, k % 2
        S0 = half * 2048
        KT = np.zeros((2, 2, 128, 4096), BF)
        V = np.zeros((2, 2, 128, 32, 128), BF)
        for g in range(2):
            ks, vs, kw, vw = kv[b, :, 2, g], kv[b, :, 3, g], kv[b, :, 4, g], kv[b, :, 5, g]
            if half == 0:
                z = np.zeros((2048, 64), BF)
                kk, vv = np.concatenate([z, ks[:2048]], 0), np.concatenate([z, vs[:2048]], 0)
                kk2, vv2 = np.concatenate([z[:512], kw[:2048]], 0), np.concatenate([z[:512], vw[:2048]], 0)
            else:
                kk, vv = ks, vs
                kk2, vv2 = kw[S0 - 512:], vw[S0 - 512:]
            KT[0, g] = _dup_kT(kk)
            V[0, g] = _v_tiles(np.concatenate([vv, vv], axis=1), 32)
            KT[1, g, :, :2560] = _dup_kT(kk2)
            V[1, g] = _v_tiles(np.concatenate([vv2, vv2], axis=1), 32)
        u_ = np.arange(4096)
        tok = u_ - 2048 + S0
        E = np.zeros((64, 4096), np.float32)
        ok = tok >= 0
        E[tok[ok] // 64, u_[ok]] = 1.0
        nmT = np.ascontiguousarray(negm[k].transpose(2, 3, 1, 0).reshape(2, 64, 2048))
        maps.append(dict(QT=QTs[k], KT=_split_k(KT), V=V, hflag=_hflag(half), jmat=_jmat(), fvec=fvec, emat=E.astype(BF), nmT=nmT))
    res = _run(nc, maps)
    U, Dn, G = [], [], []
    for k in range(8):
        b, half = k // 2, k % 2
        S0 = half * 2048
        us_, ds_ = _sel(res[k]["U"]), _seld(res[k]["Dn"])
        U.append(np.ascontiguousarray(np.stack([Uc[k], us_[0], us_[1]])))
        Dn.append(np.ascontiguousarray(np.stack([Dc[k], ds_[0], ds_[1]])))
        gt = gates[b, S0:S0 + 2048]
        gr = np.repeat(gt.transpose(1, 2, 0), 64, axis=1)
        G.append(np.ascontiguousarray(gr.reshape(3, 8, 128, 2048)))
    return _oproj("C", [[0], [1], [2]], U, Dn, h, gains[1], w_o, gate=G)


def _ffn(h, layer, inp):
    nc = _prog("ffn", build_ffn)
    g = inp['norm_gains'][layer]
    gains = np.ascontiguousarray(np.stack([g[2].reshape(8, 128).T, g[3].reshape(8, 128).T], axis=1).reshape(128, 16).astype(np.float32))
    cwf = np.concatenate([inp['ffn_conv_w'][layer], inp['ffn_conv_b'][layer][None]], axis=0)
    cw = np.ascontiguousarray(cwf.reshape(4, 44, 128).transpose(2, 1, 0).astype(np.float32))
    hT = _to_cores(h)
    maps = []
    for k in range(8):
        b, half = k // 2, k % 2
        if half == 1:
            halo = np.ascontiguousarray(h[b, 2046:2048].T.reshape(8, 128, 2))
        else:
            halo = np.zeros((8, 128, 2), np.float32)
        maps.append(dict(hT=hT[k], halo=halo, gains=gains, w_up=np.ascontiguousarray(inp['ffn_w_up'][layer]),
                         conv_wb=cw, w_down=np.ascontiguousarray(inp['ffn_w_down'][layer])))
    res = _run(nc, maps)
    return _from_cores([r["out"] for r in res])


def kernel(**inputs):
    inp = {k: np.asarray(v) for k, v in inputs.items()}
    h = np.ascontiguousarray(inp['x'].astype(np.float32))
    nc = _prog("bias", lambda: build_bias(LTOT))
    res = _run(nc, [dict(table=np.ascontiguousarray(inp['rel_table'].astype(np.float32)), onehot=_onehot()) for _ in range(8)])
    fvec = res[0]["out"]
    for layer in range(4):
        kind, j = layer % 3, layer // 3
        g = inp['norm_gains'][layer]
        if kind == 0:
            h = _mixer_A(h, g, inp['a_w_in'][j], inp['a_w_o'][j], fvec)
        elif kind == 1:
            h = _mixer_B(h, g, inp['b_w_in'][j], inp['b_sinks'][j], inp['b_w_o'][j], fvec)
        else:
            h = _mixer_C(h, g, inp['c_w_in'][j], inp['c_cmp_pos'][j], inp['c_cmp_w1'][j], inp['c_cmp_w2'][j], inp['c_w_o'][j], fvec)
        h = _ffn(h, layer, inp)
    return h.astype(np.float32)
```

```python
import contextlib
import numpy as np
import concourse.bass as bass
import concourse.mybir as mybir
from concourse.bass_utils import run_bass_kernel_spmd

F32 = mybir.dt.float32
BF16 = mybir.dt.bfloat16
AF = mybir.ActivationFunctionType
ALU = mybir.AluOpType
AX = mybir.AxisListType

SAME_ENGINE_SYNC = True
SEM_PAGE = 30000


class Prog:
    ENGS = ('pe', 'act', 'dve', 'pool', 'sp')

    def __init__(self, nc):
        self.nc = nc
        self.q = {e: [] for e in self.ENGS}
        self.lastw = {}
        self.readers = {}
        self.dma_cnt = {}
        self.seen = {e: {} for e in self.ENGS}
        self.needed = {e: set() for e in self.ENGS}
        self.final_events = []

    def _deps(self, eng, reads, writes):
        deps = []
        for k in reads:
            w = self.lastw.get(k)
            if w is not None:
                deps.append(w)
        for k in writes:
            w = self.lastw.get(k)
            if w is not None:
                deps.append(w)
            deps.extend(self.readers.get(k, ()))
        waits = []
        seen = self.seen[eng]
        for ev in deps:
            if ev[0] == 'eng':
                _, e2, j = ev
                if e2 == eng and (eng == 'pe' or not SAME_ENGINE_SYNC):
                    continue
                if seen.get(('eng', e2), -1) >= j:
                    continue
                seen[('eng', e2)] = j
                self.needed[e2].add(j)
                waits.append(ev)
            else:
                _, slot, cnt = ev
                if seen.get(('dma', slot), -1) >= cnt:
                    continue
                seen[('dma', slot)] = cnt
                waits.append(ev)
        best = {}
        for ev in waits:
            src = ev[:2]
            if src not in best or best[src][2] < ev[2]:
                best[src] = ev
        return list(best.values())

    def _commit(self, ev, reads, writes):
        for k in reads:
            self.readers.setdefault(k, []).append(ev)
        for k in writes:
            self.lastw[k] = ev
            self.readers[k] = []

    def op(self, eng, fn, reads=(), writes=()):
        waits = self._deps(eng, reads, writes)
        idx = len(self.q[eng])
        self.q[eng].append(dict(fn=fn, waits=waits, dma=None))
        ev = ('eng', eng, idx)
        self._commit(ev, reads, writes)
        return ev

    def dma(self, eng, out, in_, reads=(), writes=(), slot=None, **kw):
        waits = self._deps(eng, reads, writes)
        if slot is None:
            slot = writes[0]
        cnt = self.dma_cnt.get(slot, 0) + 16
        self.dma_cnt[slot] = cnt
        fn = (lambda e, out=out, in_=in_, kw=kw: e.dma_start(out=out, in_=in_, **kw))
        self.q[eng].append(dict(fn=fn, waits=waits, dma=slot))
        ev = ('dma', slot, cnt)
        self._commit(ev, reads, writes)
        return ev

    def finish_on(self, eng, events):
        events = list(events)
        for ev in events:
            if ev[0] == 'eng':
                self.needed[ev[1]].add(ev[2])
        self.final_events.append((eng, events))

    def emit(self):
        nc = self.nc
        import contextlib
        count_at = {}
        npages = {}
        for e in self.ENGS:
            c = 0
            for j in range(len(self.q[e])):
                if j in self.needed[e]:
                    c += 1
                    count_at[(e, j)] = c
            npages[e] = (c + SEM_PAGE - 1) // SEM_PAGE
        for eng, evs in self.final_events:
            pass
        with contextlib.ExitStack() as st:
            esems = {e: [st.enter_context(nc.semaphore(f"s_{e}_{i}")) for i in range(npages[e])]
                     for e in self.ENGS}
            dsems = {}
            for i, slot in enumerate(self.dma_cnt):
                dsems[slot] = st.enter_context(nc.semaphore(f"d_{i}"))
            self.n_sems = sum(npages.values()) + len(dsems)
            block = st.enter_context(nc.Block())

            def resolve(ev):
                if ev[0] == 'eng':
                    c = count_at[(ev[1], ev[2])]
                    return esems[ev[1]][(c - 1) // SEM_PAGE], (c - 1) % SEM_PAGE + 1
                return dsems[ev[1]], ev[2]

            def run(ename):
                def body(eobj):
                    for j, o in enumerate(self.q[ename]):
                        for ev in o['waits']:
                            s, v = resolve(ev)
                            eobj.wait_ge(s, v)
                        ins = o['fn'](eobj)
                        if o['dma'] is not None:
                            ins.then_inc(dsems[o['dma']], 16)
                        elif (ename, j) in count_at:
                            c = count_at[(ename, j)]
                            ins.then_inc(esems[ename][(c - 1) // SEM_PAGE], 1)
                    for eng, evs in self.final_events:
                        if eng == ename:
                            for ev in evs:
                                s, v = resolve(ev)
                                eobj.wait_ge(s, v)
                return body

            block.tensor(run('pe'))
            block.scalar(run('act'))
            block.vector(run('dve'))
            block.gpsimd(run('pool'))
            block.sync(run('sp'))


D = 1024
DFF = 2816
NT = 2048
TG = 512
NTG = NT // TG
KC = D // 128
NJ = DFF // 128
EPS = 1e-6


def emit_prenorm(p, nc, hT, xT, gcol, ncols, sq, rstd, ones_bf, ps_stat, epsc, xoff=0):
    nq = 0
    t0 = 0
    while t0 < ncols:
        n = min(TG, ncols - t0)
        for c in range(KC):
            s = sq[nq % 3]
            sk = s.name
            nq += 1
            p.op('act', lambda e, s=s, c=c, t0=t0, n=n: e.activation(out=s[:, 0:n], in_=hT[:, c, t0:t0 + n], func=AF.Square),
                 reads=[(hT.name, t0 // TG)], writes=[sk])
            p.op('pe', lambda e, s=s, c=c, n=n: e.matmul(ps_stat[:, 0:n], lhsT=ones_bf[:], rhs=s[:, 0:n], start=(c == 0), stop=(c == KC - 1)),
                 reads=[sk, 'ones'], writes=['ps_stat'])
        p.op('act', lambda e, n=n: e.activation(out=rstd[:, 0:n], in_=ps_stat[:, 0:n], func=AF.Sqrt, scale=1.0 / D, bias=epsc[:, 0:1]),
             reads=['ps_stat', 'epsc'], writes=[rstd.name])
        p.op('dve', lambda e, n=n: e.reciprocal(out=rstd[:, 0:n], in_=rstd[:, 0:n]),
             reads=[rstd.name], writes=[rstd.name])
        for c in range(KC):
            p.op('dve', lambda e, c=c, t0=t0, n=n: e.scalar_tensor_tensor(out=xT[:, c, xoff + t0:xoff + t0 + n], in0=hT[:, c, t0:t0 + n], scalar=gcol[:, c:c + 1], in1=rstd[:, 0:n], op0=ALU.mult, op1=ALU.mult),
                 reads=[(hT.name, t0 // TG), rstd.name, 'gains'], writes=[(xT.name, (xoff + t0) // TG)])
        t0 += n


def emit_postnorm_residual(p, nc, hT, ysb, gcol, tg, ones_bf, ps_stat, sqs, rstd, tmp, sqctr, epsc):
    for c in range(KC):
        s = sqs[sqctr[0] % len(sqs)]
        sk = s.name
        sqctr[0] += 1
        p.op('act', lambda e, s=s, c=c: e.activation(out=s[:], in_=ysb[:, c, :], func=AF.Square),
             reads=[('ysb', c)], writes=[sk])
        p.op('pe', lambda e, s=s, c=c: e.matmul(ps_stat[:], lhsT=ones_bf[:], rhs=s[:], start=(c == 0), stop=(c == KC - 1)),
             reads=[sk, 'ones'], writes=['ps_stat'])
    p.op('act', lambda e: e.activation(out=rstd[:], in_=ps_stat[:], func=AF.Sqrt, scale=1.0 / D, bias=epsc[:, 0:1]),
         reads=['ps_stat', 'epsc'], writes=[rstd.name])
    p.op('dve', lambda e: e.reciprocal(out=rstd[:], in_=rstd[:]),
         reads=[rstd.name], writes=[rstd.name])
    for c in range(KC):
        p.op('dve', lambda e, c=c: e.scalar_tensor_tensor(out=tmp[:], in0=ysb[:, c, :], scalar=gcol[:, c:c + 1], in1=rstd[:], op0=ALU.mult, op1=ALU.mult),
             reads=[('ysb', c), rstd.name, 'gains'], writes=[tmp.name])
        p.op('dve', lambda e, c=c: e.tensor_tensor(out=hT[:, c, tg * TG:(tg + 1) * TG], in0=hT[:, c, tg * TG:(tg + 1) * TG], in1=tmp[:], op=ALU.add),
             reads=[tmp.name, (hT.name, tg)], writes=[(hT.name, tg)])


def build_ffn():
    nc = bass.Bass("TRN2", target_bir_lowering=False)
    hT_d = nc.dram_tensor("hT", [KC, 128, NT], F32, kind="ExternalInput").ap()
    halo_d = nc.dram_tensor("halo", [KC, 128, 2], F32, kind="ExternalInput").ap()
    g_d = nc.dram_tensor("gains", [128, 2 * KC], F32, kind="ExternalInput").ap()
    wup_d = nc.dram_tensor("w_up", [D, 2 * DFF], F32, kind="ExternalInput").ap()
    cw_d = nc.dram_tensor("conv_wb", [128, 2 * NJ, 4], F32, kind="ExternalInput").ap()
    wdn_d = nc.dram_tensor("w_down", [DFF, D], F32, kind="ExternalInput").ap()
    out_d = nc.dram_tensor("out", [KC, 128, NT], F32, kind="ExternalOutput").ap()

    with contextlib.ExitStack() as st:
        sb = lambda name, shape, dt: st.enter_context(nc.sbuf_tensor(name, shape, dt))
        ps = lambda name, shape, dt: st.enter_context(nc.psum_tensor(name, shape, dt))
        hT = sb("hT_sb", [128, KC, NT], F32)
        hh = sb("hh_sb", [128, KC, 2], F32)
        xT = sb("xT_sb", [128, KC, NT], BF16)
        xh = sb("xh_sb", [128, KC, 2], BF16)
        gains = sb("gains_sb", [128, 2 * KC], F32)
        cw = sb("cw_sb", [128, 2 * NJ, 4], F32)
        ones_bf = sb("ones_bf", [128, 128], BF16)
        carry = sb("carry", [128, 2 * NJ, 2], F32)
        epsc = sb("epsc", [128, 1], F32)
        NWB = 4
        wu = [sb(f"wu{i}", [128, KC, 256], BF16) for i in range(NWB)]
        NDB = 3
        wd = [sb(f"wd{i}", [128, NJ, 128], BF16) for i in range(NDB)]
        ubuf = [sb(f"ubuf{i}", [128, TG + 2], F32) for i in range(4)]
        cbuf = [sb(f"cbuf{i}", [128, TG], F32) for i in range(4)]
        gbuf = [sb(f"gbuf{i}", [128, TG], F32) for i in range(2)]
        gT = sb("gT", [128, NJ, TG], BF16)
        ysb = sb("ysb", [128, KC, TG], F32)
        sqs = [sb(f"sqp{i}", [128, TG], BF16) for i in range(3)]
        rstd2 = sb("rstd2", [128, TG], F32)
        tmp = sb("tmpn", [128, TG], F32)
        ps_stat = ps("ps_stat", [128, TG], F32)
        ps_u = [ps(f"ps_u{i}", [128, TG], F32) for i in range(4)]
        ps_y = [ps(f"ps_y{i}", [128, TG], F32) for i in range(2)]
        ps_c = ps("ps_c", [128, 2 * NJ * 2], F32)

        p = Prog(nc)
        for c in range(KC):
            for tg in range(NTG):
                p.dma('sp', hT[:, c, tg * TG:(tg + 1) * TG], hT_d[c, :, tg * TG:(tg + 1) * TG], writes=[(hT.name, tg)], slot=f"ld_h{tg}")
        p.dma('sp', hh[:], halo_d.rearrange("c p t -> p c t"), writes=[(hh.name, 0)])
        p.dma('sp', gains[:], g_d[:, :], writes=['gains'])
        p.dma('sp', cw[:], cw_d[:, :, :], writes=['cw'])
        p.op('dve', lambda e: e.memset(ones_bf[:], 1.0), writes=['ones'])
        p.op('dve', lambda e: e.memset(epsc[:], EPS), writes=['epsc'])
        emit_prenorm(p, nc, hh, xh, gains[:, 0:KC], 2, sqs, rstd2, ones_bf, ps_stat, epsc)
        emit_prenorm(p, nc, hT, xT, gains[:, 0:KC], NT, sqs, rstd2, ones_bf, ps_stat, epsc)

        wu_n = [0]

        def load_wu(j):
            i = wu_n[0] % NWB
            wu_n[0] += 1
            w = wu[i]
            for half, col0 in ((0, j * 128), (1, DFF + j * 128)):
                p.dma('pool', w[:, :, half * 128:(half + 1) * 128],
                      wup_d[:, col0:col0 + 128].rearrange("(kc p) n -> p kc n", p=128),
                      writes=[w.name], slot=w.name)
            return w

        wd_n = [0]

        def load_wd(c):
            i = wd_n[0] % NDB
            wd_n[0] += 1
            w = wd[i]
            p.dma('pool', w[:], wdn_d[:, c * 128:(c + 1) * 128].rearrange("(j p) n -> p j n", p=128), writes=[w.name])
            return w

        for j in range(NJ):
            w = load_wu(j)
            for half in range(2):
                jg = half * NJ + j
                for kc in range(KC):
                    p.op('pe', lambda e, w=w, half=half, kc=kc, jg=jg: e.matmul(ps_c[:, jg * 2:jg * 2 + 2], lhsT=w[:, kc, half * 128:(half + 1) * 128], rhs=xh[:, kc, :], start=(kc == 0), stop=(kc == KC - 1)),
                         reads=[w.name, (xh.name, 0)], writes=['ps_c'])
        p.op('dve', lambda e: e.tensor_copy(out=carry[:].rearrange("p a b -> p (a b)"), in_=ps_c[:]), reads=['ps_c'], writes=['carry'])

        un = [0]
        sqctr = [0]
        for tg in range(NTG):
            tsl = slice(tg * TG, (tg + 1) * TG)
            for j in range(NJ):
                w = load_wu(j)
                cres = []
                for half in range(2):
                    jg = half * NJ + j
                    k = un[0] % 4
                    un[0] += 1
                    pu, ub, cb = ps_u[k], ubuf[k], cbuf[k]
                    for kc in range(KC):
                        p.op('pe', lambda e, w=w, half=half, kc=kc, pu=pu, tsl=tsl: e.matmul(pu[:], lhsT=w[:, kc, half * 128:(half + 1) * 128], rhs=xT[:, kc, tsl], start=(kc == 0), stop=(kc == KC - 1)),
                             reads=[w.name, (xT.name, tg)], writes=[pu.name])
                    p.op('dve', lambda e, ub=ub, jg=jg: e.tensor_copy(out=ub[:, 0:2], in_=carry[:, jg, :]), reads=['carry'], writes=[ub.name])
                    p.op('act', lambda e, ub=ub, pu=pu: e.activation(out=ub[:, 2:TG + 2], in_=pu[:], func=AF.Copy), reads=[pu.name], writes=[ub.name + "m"])
                    p.op('act', lambda e, cb=cb, pu=pu, jg=jg: e.activation(out=cb[:], in_=pu[:], func=AF.Identity, scale=cw[:, jg, 2:3], bias=cw[:, jg, 3:4]),
                         reads=[pu.name, 'cw'], writes=[cb.name])
                    p.op('dve', lambda e, cb=cb, ub=ub, jg=jg: e.scalar_tensor_tensor(out=cb[:], in0=ub[:, 1:TG + 1], scalar=cw[:, jg, 1:2], in1=cb[:], op0=ALU.mult, op1=ALU.add),
                         reads=[ub.name, ub.name + "m", cb.name, 'cw'], writes=[cb.name])
                    p.op('dve', lambda e, cb=cb, ub=ub, jg=jg: e.scalar_tensor_tensor(out=cb[:], in0=ub[:, 0:TG], scalar=cw[:, jg, 0:1], in1=cb[:], op0=ALU.mult, op1=ALU.add),
                         reads=[ub.name, ub.name + "m", cb.name, 'cw'], writes=[cb.name])
                    p.op('dve', lambda e, ub=ub, jg=jg: e.tensor_copy(out=carry[:, jg, :], in_=ub[:, TG:TG + 2]), reads=[ub.name + "m"], writes=['carry'])
                    cres.append(cb)
                gb = gbuf[j % 2]
                p.op('act', lambda e, gb=gb, cb=cres[0]: e.activation(out=gb[:], in_=cb[:], func=AF.Gelu_apprx_tanh), reads=[cres[0].name], writes=[gb.name])
                p.op('dve', lambda e, gb=gb, cb=cres[1], j=j: e.tensor_tensor(out=gT[:, j, :], in0=gb[:], in1=cb[:], op=ALU.mult),
                     reads=[gb.name, cres[1].name], writes=[('gT', j)])
            for c in range(KC):
                w = load_wd(c)
                py = ps_y[c % 2]
                for j in range(NJ):
                    p.op('pe', lambda e, w=w, j=j, py=py: e.matmul(py[:], lhsT=w[:, j, :], rhs=gT[:, j, :], start=(j == 0), stop=(j == NJ - 1)),
                         reads=[w.name, ('gT', j)], writes=[py.name])
                p.op('act', lambda e, c=c, py=py: e.activation(out=ysb[:, c, :], in_=py[:], func=AF.Copy), reads=[py.name], writes=[('ysb', c)])
            emit_postnorm_residual(p, nc, hT, ysb, gains[:, KC:2 * KC], tg, ones_bf, ps_stat, sqs, rstd2, tmp, sqctr, epsc)
        evs = []
        for c in range(KC):
            for tg in range(NTG):
                evs.append(p.dma('sp', out_d[c, :, tg * TG:(tg + 1) * TG], hT[:, c, tg * TG:(tg + 1) * TG], reads=[(hT.name, tg)], writes=[('out', c, tg)], slot="st_out"))
        p.finish_on('sp', [('dma', 'st_out', p.dma_cnt['st_out'])])
        p.emit()
    return nc


MASKV = -30000.0
DEBUG = {}
SCALE = 0.125
NQ = 2048
NQT = 16


def build_lin(nco):
    NOC = nco // 128
    nc = bass.Bass("TRN2", target_bir_lowering=False)
    hT_d = nc.dram_tensor("hT", [KC, 128, NT], F32, kind="ExternalInput").ap()
    g_d = nc.dram_tensor("gains", [128, KC], F32, kind="ExternalInput").ap()
    w_d = nc.dram_tensor("w", [D, nco], F32, kind="ExternalInput").ap()
    out_d = nc.dram_tensor("out", [NOC, 128, NT], BF16, kind="ExternalOutput").ap()
    with contextlib.ExitStack() as st:
        sb = lambda name, shape, dt: st.enter_context(nc.sbuf_tensor(name, shape, dt))
        ps = lambda name, shape, dt: st.enter_context(nc.psum_tensor(name, shape, dt))
        hT = sb("hT_sb", [128, KC, NT], F32)
        xT = sb("xT_sb", [128, KC, NT], BF16)
        gains = sb("gains_sb", [128, KC], F32)
        ones_bf = sb("ones_bf", [128, 128], BF16)
        epsc = sb("epsc", [128, 1], F32)
        sqs = [sb(f"sqp{i}", [128, TG], BF16) for i in range(3)]
        rstd = sb("rstd", [128, TG], F32)
        wb = [sb(f"wb{i}", [128, KC, 128], BF16) for i in range(4)]
        ob = [sb(f"ob{i}", [128, NT], BF16) for i in range(3)]
        ps_stat = ps("ps_stat", [128, TG], F32)
        ps_o = [ps(f"ps_o{i}", [128, TG], F32) for i in range(4)]
        p = Prog(nc)
        for c in range(KC):
            for tg in range(NTG):
                p.dma('sp', hT[:, c, tg * TG:(tg + 1) * TG], hT_d[c, :, tg * TG:(tg + 1) * TG], writes=[(hT.name, tg)], slot=f"ld_h{tg}")
        p.dma('sp', gains[:], g_d[:, :], writes=['gains'])
        p.op('dve', lambda e: e.memset(ones_bf[:], 1.0), writes=['ones'])
        p.op('dve', lambda e: e.memset(epsc[:], EPS), writes=['epsc'])
        emit_prenorm(p, nc, hT, xT, gains[:, 0:KC], NT, sqs, rstd, ones_bf, ps_stat, epsc)
        n = 0
        evs = []
        for oc in range(NOC):
            w = wb[oc % 4]
            p.dma('pool', w[:], w_d[:, oc * 128:(oc + 1) * 128].rearrange("(kc p) n -> p kc n", p=128), writes=[w.name])
            o = ob[oc % 3]
            for tg in range(NTG):
                po = ps_o[n % 4]
                n += 1
                for kc in range(KC):
                    p.op('pe', lambda e, w=w, kc=kc, po=po, tg=tg: e.matmul(po[:], lhsT=w[:, kc, :], rhs=xT[:, kc, tg * TG:(tg + 1) * TG], start=(kc == 0), stop=(kc == KC - 1)),
                         reads=[w.name, (xT.name, tg)], writes=[po.name])
                eng = 'act' if n % 2 else 'dve'
                if eng == 'act':
                    p.op('act', lambda e, o=o, po=po, tg=tg: e.activation(out=o[:, tg * TG:(tg + 1) * TG], in_=po[:], func=AF.Copy), reads=[po.name], writes=[(o.name, tg)])
                else:
                    p.op('dve', lambda e, o=o, po=po, tg=tg: e.tensor_copy(out=o[:, tg * TG:(tg + 1) * TG], in_=po[:]), reads=[po.name], writes=[(o.name, tg)])
            evs.append(p.dma('sp', out_d[oc, :, :], o[:], reads=[(o.name, tg) for tg in range(NTG)], writes=[(o.name + "st")], slot="st_" + o.name))
            for tg in range(NTG):
                p.readers.setdefault((o.name, tg), []).append(evs[-1])
        p.finish_on('sp', [('dma', "st_" + ob[i].name, p.dma_cnt["st_" + ob[i].name]) for i in range(3) if ("st_" + ob[i].name) in p.dma_cnt])
        p.emit()
    return nc


def build_bias(ltot):
    nc = bass.Bass("TRN2", target_bir_lowering=False)
    t_d = nc.dram_tensor("table", [32, 16], F32, kind="ExternalInput").ap()
    oh_d = nc.dram_tensor("onehot", [33, ltot], F32, kind="ExternalInput").ap()
    out_d = nc.dram_tensor("out", [16, ltot], F32, kind="ExternalOutput").ap()
    nchk = (ltot + 511) // 512
    with contextlib.ExitStack() as st:
        sb = lambda name, shape, dt: st.enter_context(nc.sbuf_tensor(name, shape, dt))
        ps = lambda name, shape, dt: st.enter_context(nc.psum_tensor(name, shape, dt))
        tf = sb("tf", [64, 16], F32)
        tb = sb("tb", [64, 16], BF16)
        oh = sb("oh", [64, ltot], BF16)
        fo = sb("fo", [16, ltot], F32)
        pp = [ps(f"pp{i}", [16, 512], F32) for i in range(2)]
        p = Prog(nc)
        p.op('dve', lambda e: e.memset(tf[32:64, :], MASKV), writes=['tfm'])
        p.dma('sp', tf[0:32, :], t_d[:, :], writes=['tf'])
        p.dma('pool', oh[0:33, :], oh_d[:, :], writes=['oh'])
        p.op('dve', lambda e: e.tensor_scalar(out=tb[0:32, :], in0=tf[0:32, :], scalar1=1.0 / SCALE, scalar2=None, op0=ALU.mult), reads=['tf'], writes=['tb'])
        p.op('dve', lambda e: e.tensor_copy(out=tb[32:64, :], in_=tf[32:64, :]), reads=['tfm'], writes=['tbm'])
        for k in range(nchk):
            n = min(512, ltot - k * 512)
            q = pp[k % 2]
            p.op('pe', lambda e, k=k, n=n, q=q: e.matmul(q[:, 0:n], lhsT=tb[0:33, :], rhs=oh[0:33, k * 512:k * 512 + n], start=True, stop=True),
                 reads=['tb', 'tbm', 'oh'], writes=[q.name])
            p.op('act', lambda e, k=k, n=n, q=q: e.activation(out=fo[:, k * 512:k * 512 + n], in_=q[:, 0:n], func=AF.Copy), reads=[q.name], writes=['fo'])
        ev = p.dma('sp', out_d[:, :], fo[:], reads=['fo'], writes=['out'])
        p.finish_on('sp', [ev])
        p.emit()
    return nc


def build_att(cfg):
    npr, nkch, nkt, nkeys = cfg['npr'], cfg['nkch'], cfg['nkt'], cfg['nkeys']
    ltot, lc = cfg.get('ltot', 0), cfg.get('lc', 0)
    imp = cfg.get('imp', False)
    anymask = cfg.get('mask', False)
    nc = bass.Bass("TRN2", target_bir_lowering=False)
    q_d = nc.dram_tensor("QT", [KC, 128, NQ], BF16, kind="ExternalInput").ap() if cfg.get('sharedq', False) else None
    qs_d = None if q_d is not None else nc.dram_tensor("QT", [npr, KC, 128, NQ], BF16, kind="ExternalInput").ap()
    k_d = nc.dram_tensor("KT", [npr, nkch, 128, 2, nkeys], BF16, kind="ExternalInput").ap()
    v_d = nc.dram_tensor("V", [npr, nkch, 128, nkt, 128], BF16, kind="ExternalInput").ap()
    hf_d = nc.dram_tensor("hflag", [128, 128], BF16, kind="ExternalInput").ap()
    jm_d = nc.dram_tensor("jmat", [128, 128], BF16, kind="ExternalInput").ap()
    fv_d = nc.dram_tensor("fvec", [16, ltot], F32, kind="ExternalInput") if ltot else None
    cv_d = nc.dram_tensor("cvec", [2, lc], F32, kind="ExternalInput") if lc else None
    if anymask:
        e_d = nc.dram_tensor("emat", [64, nkt * 128], BF16, kind="ExternalInput").ap()
        nm_d = nc.dram_tensor("nmT", [2, 64, NQ], BF16, kind="ExternalInput").ap()
    if imp:
        ov_d = nc.dram_tensor("ov65", [128, 2, 65], BF16, kind="ExternalInput").ap()
        sbias_d = nc.dram_tensor("selbias", [128, NQT, 64], F32, kind="ExternalInput").ap()
        nmo_d = nc.dram_tensor("negmask", [128, NQT, 2, 64], BF16, kind="ExternalOutput").ap()
    u_d = nc.dram_tensor("U", [npr, KC, 128, NQT, 128], F32, kind="ExternalOutput").ap()
    d_d = nc.dram_tensor("Dn", [npr, KC, 1, NQT, 256], F32, kind="ExternalOutput").ap()
    maxb = max(len(b) for b in cfg['bias'])
    with contextlib.ExitStack() as st:
        sb = lambda name, shape, dt: st.enter_context(nc.sbuf_tensor(name, shape, dt))
        ps = lambda name, shape, dt: st.enter_context(nc.psum_tensor(name, shape, dt))
        qt = [sb(f"qt{i}", [128, NQ], BF16) for i in range(2)]
        kt = [sb(f"kt{i}", [128, 2, nkeys], BF16) for i in range(2)]
        vt = [sb(f"vt{i}", [128, nkt, 128], BF16) for i in range(2)]
        bt = [sb(f"bt{i}", [128, maxb, 256], BF16) for i in range(2)]
        hflag = sb("hflag_sb", [128, 128], BF16)
        ones1 = sb("ones1", [128, 128], BF16)
        jm = sb("jm_sb", [128, 128], BF16)
        pb = [sb(f"pb{i}", [128, 512], BF16) for i in range(4)]
        ub = [sb(f"ub{i}", [128, NQT, 256], F32) for i in range(2)]
        db = [sb(f"db{i}", [128, NQT, 256], F32) for i in range(2)]
        if anymask:
            em = sb("em_sb", [64, nkt * 128], BF16)
            nm = sb("nm_sb", [64, 2, NQ], BF16)
        if imp:
            ov = sb("ov_sb", [128, 2, 65], BF16)
            sbias = sb("sbias_sb", [128, NQT, 64], F32)
            impacc = sb("impacc", [128, NQT, 2, 64], F32)
            nmb = sb("nmb", [128, NQT, 2, 64], BF16)
            rden = sb("rden", [128, 2], F32)
            mx = sb("mx", [128, 16], F32)
            scr = sb("scr", [128, 64], F32)
            scr2 = sb("scr2", [128, 64], F32)
            ps_i = [ps(f"ps_i{i}", [128, 2, 65], F32) for i in range(2)]
        ps_s = [ps(f"ps_s{i}", [128, 512], F32) for i in range(2)]
        ps_a = [ps(f"ps_a{i}", [128, 256], F32) for i in range(2)]
        ps_d = [ps(f"ps_d{i}", [128, 256], F32) for i in range(2)]

        p = Prog(nc)
        p.dma('sp', hflag[:], hf_d[:, :], writes=['hflag'])
        p.dma('sp', jm[:], jm_d[:, :], writes=['jm'])
        p.op('dve', lambda e: e.memset(ones1[:], 1.0), writes=['ones1'])
        if anymask:
            p.dma('sp', em[:], e_d[:, :], writes=['em'])
            p.dma('sp', nm[:], nm_d.rearrange("g j q -> j g q"), writes=['nm'])
        if imp:
            p.dma('sp', ov[:], ov_d[:, :, :], writes=['ov'])
            p.dma('sp', sbias[:], sbias_d[:, :, :], writes=['sbias'])
            p.op('pool', lambda e: e.memset(impacc[:], 0.0), writes=['impacc'])
        cnt = dict(q=0, k=0, b=0, s=0, a=0, pb=0, u=0, i=0)
        lastk = [None, None]
        out_evs = []
        for pr in range(npr):
            blist = cfg['bias'][pr]
            plan = cfg['plans'][pr]
            for c in range(KC):
                Q = qt[cnt['q'] % 2]
                cnt['q'] += 1
                src = q_d[c, :, :] if q_d is not None else qs_d[pr, c, :, :]
                p.dma('sp', Q[:], src, writes=[Q.name])
                kc = cfg['kch'](c)
                if lastk[0] != (pr, kc):
                    Kt = kt[cnt['k'] % 2]
                    Vt = vt[cnt['k'] % 2]
                    cnt['k'] += 1
                    for h_ in range(2):
                        for k0_ in range(0, nkeys, 2048):
                            k1_ = min(nkeys, k0_ + 2048)
                            p.dma('sp', Kt[:, h_, k0_:k1_], k_d[pr, kc, :, h_, k0_:k1_], writes=[Kt.name], slot=Kt.name)
                    for t0_ in range(0, nkt, 16):
                        t1_ = min(nkt, t0_ + 16)
                        p.dma('sp', Vt[:, t0_:t1_, :], v_d[pr, kc, :, t0_:t1_, :], writes=[Vt.name], slot=Vt.name)
                    lastk = [(pr, kc), (Kt, Vt)]
                Kt, Vt = lastk[1]
                B = bt[cnt['b'] % 2]
                cnt['b'] += 1
                for bi, (kind, a, b_) in enumerate(blist):
                    if DEBUG.get('nobias'):
                        p.dma('sp', B[:, bi, :], jm_d[:, :].rearrange("p (a q) -> p a q", a=1).broadcast_to([128, 2, 128]) if False else hf_d[:, :], writes=[B.name], slot=B.name) if False else None
                        continue
                    if kind == 'f':
                        srcb = bass.AP(fv_d, (2 * c) * ltot + a + b_, [[1, 128], [ltot, 2], [1, 128]])
                        p.dma('pool', B[:, bi, :].rearrange("p (h q) -> p h q", h=2), srcb, writes=[B.name], slot=B.name)
                    else:
                        for h in range(2):
                            srcb = bass.AP(cv_d, a * lc + b_, [[16, 128], [1, 128]])
                            p.dma('pool', B[:, bi, h * 128:(h + 1) * 128], srcb, writes=[B.name], slot=B.name)
                U = ub[cnt['u'] % 2]
                Dd = db[cnt['u'] % 2]
                cnt['u'] += 1
                g = c // 4
                pending = [None]

                def flush():
                    if pending[0] is not None:
                        pending[0]()
                        pending[0] = None

                def make_pv(acc, accd, P, grp, k0, nk, i, U, Dd, Vt):
                    def run():
                        for s_, (ktile, bidx, um, halo) in enumerate(grp):
                            kidx = k0 + s_
                            col = s_ * 256
                            on = hflag if halo else ones1
                            p.op('pe', lambda e, acc=acc, Vt=Vt, ktile=ktile, P=P, col=col, kidx=kidx, nk=nk: e.matmul(acc[:, 0:256], lhsT=Vt[:, ktile, :], rhs=P[:, col:col + 256], start=(kidx == 0), stop=(kidx == nk - 1)),
                                 reads=[Vt.name, P.name], writes=[acc.name])
                            p.op('pe', lambda e, accd=accd, on=on, P=P, col=col, kidx=kidx, nk=nk: e.matmul(accd[:, 0:256], lhsT=on[:], rhs=P[:, col:col + 256], start=(kidx == 0), stop=(kidx == nk - 1)),
                                 reads=['hflag', 'ones1', P.name], writes=[accd.name])
                        if imp:
                            pi = ps_i[cnt['i'] % 2]
                            cnt['i'] += 1
                            for h in range(2):
                                for s_, (ktile, bidx, um, halo) in enumerate(grp):
                                    p.op('pe', lambda e, pi=pi, h=h, P=P, s_=s_, ktile=ktile, ng=len(grp): e.matmul(pi[:, h, :], lhsT=P[:, s_ * 256 + h * 128:s_ * 256 + (h + 1) * 128], rhs=ov[:, ktile, :], start=(s_ == 0), stop=(s_ == ng - 1)),
                                         reads=[P.name, 'ov'], writes=[pi.name])
                            p.op('dve', lambda e, pi=pi: e.tensor_scalar(out=rden[:], in0=pi[:, :, 64], scalar1=1e-30, scalar2=None, op0=ALU.add), reads=[pi.name], writes=['rden'])
                            p.op('dve', lambda e: e.reciprocal(out=rden[:], in_=rden[:]), reads=['rden'], writes=['rden'])
                            for h in range(2):
                                p.op('dve', lambda e, pi=pi, h=h, i=i, g=g: e.scalar_tensor_tensor(out=impacc[:, i, g, :], in0=pi[:, h, 0:64], scalar=rden[:, h:h + 1], in1=impacc[:, i, g, :], op0=ALU.mult, op1=ALU.add),
                                     reads=[pi.name, 'rden', 'impacc'], writes=['impacc'])
                        if k0 + len(grp) == nk:
                            p.op('dve', lambda e, U=U, acc=acc, i=i: e.tensor_copy(out=U[:, i, :], in_=acc[:, 0:256]), reads=[acc.name], writes=[U.name])
                            p.op('dve', lambda e, Dd=Dd, accd=accd, i=i: e.tensor_copy(out=Dd[:, i, :], in_=accd[:, 0:256]), reads=[accd.name], writes=[Dd.name])
                    return run

                for i in range(NQT):
                    kts = plan(i)
                    acc = ps_a[cnt['a'] % 2]
                    accd = ps_d[cnt['a'] % 2]
                    cnt['a'] += 1
                    qsl = slice(i * 128, (i + 1) * 128)
                    nk = len(kts)
                    for k0 in range(0, nk, 2):
                        grp = kts[k0:k0 + 2]
                        S = ps_s[cnt['s'] % 2]
                        cnt['s'] += 1
                        for s_, (ktile, bidx, um, halo) in enumerate(grp):
                            ksl = slice(ktile * 128, (ktile + 1) * 128)
                            col = s_ * 256
                            p.op('pe', lambda e, S=S, col=col, B=B, bidx=bidx: e.matmul(S[:, col:col + 256], lhsT=jm[:], rhs=B[:, bidx, :], start=True, stop=False),
                                 reads=['jm', B.name], writes=[S.name])
                            for h in range(2):
                                p.op('pe', lambda e, S=S, col=col, h=h, Kt=Kt, Q=Q, ksl=ksl, qsl=qsl, last=((h == 1) and not um): e.matmul(S[:, col + h * 128:col + (h + 1) * 128], lhsT=Kt[:, h, ksl], rhs=Q[:, qsl], start=False, stop=last),
                                     reads=[Kt.name, Q.name], writes=[S.name])
                            if um:
                                for h in range(2):
                                    p.op('pe', lambda e, S=S, col=col, h=h, ksl=ksl, qsl=qsl, g=g: e.matmul(S[:, col + h * 128:col + (h + 1) * 128], lhsT=em[:, ksl], rhs=nm[:, g, qsl], start=False, stop=(h == 1)),
                                         reads=['em', 'nm'], writes=[S.name])
                        P = pb[cnt['pb'] % 4]
                        cnt['pb'] += 1
                        ncol = 256 * len(grp)
                        p.op('act', lambda e, P=P, S=S, ncol=ncol: e.activation(out=P[:, 0:ncol], in_=S[:, 0:ncol], func=AF.Exp, scale=SCALE),
                             reads=[S.name], writes=[P.name])
                        flush()
                        pending[0] = make_pv(acc, accd, P, grp, k0, nk, i, U, Dd, Vt)
                flush()
                for i0_ in range(0, NQT, 8):
                    ev1 = p.dma('act', u_d[pr, c, 0:64, i0_:i0_ + 8, :], U[0:64, i0_:i0_ + 8, 0:128], reads=[U.name], writes=[U.name + "st"], slot="st_" + U.name)
                    ev1 = p.dma('act', u_d[pr, c, 64:128, i0_:i0_ + 8, :], U[64:128, i0_:i0_ + 8, 128:256], reads=[U.name], writes=[U.name + "st"], slot="st_" + U.name)
                ev2 = p.dma('act', d_d[pr, c, :, :, :], Dd[0:1, :, :], reads=[Dd.name], writes=[Dd.name + "st"], slot="st_" + Dd.name)
                out_evs += [ev1, ev2]
        if imp:
            for i in range(NQT):
                for g in range(2):
                    p.op('dve', lambda e, i=i, g=g: e.tensor_tensor(out=scr[:], in0=impacc[:, i, g, :], in1=sbias[:, i, :], op=ALU.add), reads=['impacc', 'sbias'], writes=['scr'])
                    p.op('dve', lambda e: e.max(out=mx[:, 0:8], in_=scr[:]), reads=['scr'], writes=['mx'])
                    p.op('dve', lambda e: e.match_replace(out=scr2[:], in_to_replace=mx[:, 0:8], in_values=scr[:], imm_value=-3e38), reads=['scr', 'mx'], writes=['scr2'])
                    p.op('dve', lambda e: e.max(out=mx[:, 8:16], in_=scr2[:]), reads=['scr2'], writes=['mx2'])
                    p.op('dve', lambda e, i=i, g=g: e.tensor_scalar(out=impacc[:, i, g, :], in0=scr[:], scalar1=mx[:, 15:16], scalar2=MASKV, op0=ALU.is_lt, op1=ALU.mult),
                         reads=['scr', 'mx2'], writes=['impacc'])
            p.op('dve', lambda e: e.tensor_copy(out=nmb[:], in_=impacc[:]), reads=['impacc'], writes=['nmb'])
            out_evs.append(p.dma('sp', nmo_d[:, :, :, :], nmb[:], reads=['nmb'], writes=['nmo']))
        p.finish_on('sp', out_evs[-1:] + [('dma', sl, p.dma_cnt[sl]) for sl in p.dma_cnt if str(sl).startswith('st_')])
        p.emit()
    return nc


def build_oproj(branches, has_gate, has_sink):
    nu = sum(len(b) for b in branches)
    nb = len(branches)
    nc = bass.Bass("TRN2", target_bir_lowering=False)
    u_d = nc.dram_tensor("U", [nu, KC, 128, NT], F32, kind="ExternalInput").ap()
    d_d = nc.dram_tensor("Dn", [nu, KC, 128, NT], F32, kind="ExternalInput").ap()
    if has_gate:
        gt_d = nc.dram_tensor("gate", [nb, KC, 128, NT], BF16, kind="ExternalInput").ap()
    if has_sink:
        sk_d = nc.dram_tensor("sink", [128, KC], F32, kind="ExternalInput").ap()
    wo_d = nc.dram_tensor("w_o", [D, D], F32, kind="ExternalInput").ap()
    hT_d = nc.dram_tensor("hT", [KC, 128, NT], F32, kind="ExternalInput").ap()
    g_d = nc.dram_tensor("gains", [128, KC], F32, kind="ExternalInput").ap()
    out_d = nc.dram_tensor("out", [KC, 128, NT], F32, kind="ExternalOutput").ap()
    with contextlib.ExitStack() as st:
        sb = lambda name, shape, dt: st.enter_context(nc.sbuf_tensor(name, shape, dt))
        ps = lambda name, shape, dt: st.enter_context(nc.psum_tensor(name, shape, dt))
        wo = sb("wo_sb", [128, KC, D], BF16)
        gains = sb("gains_sb", [128, KC], F32)
        ones_bf = sb("ones_bf", [128, 128], BF16)
        epsc = sb("epsc", [128, 1], F32)
        sink = sb("sink_sb", [128, KC], F32)
        ul = [sb(f"ul{i}", [128, TG], F32) for i in range(4)]
        dl = [sb(f"dl{i}", [128, TG], F32) for i in range(4)]
        gl = [sb(f"gl{i}", [128, TG], BF16) for i in range(2)]
        sg = sb("sg", [128, TG], F32)
        us = sb("us", [128, TG], F32)
        ds = sb("ds", [128, TG], F32)
        oacc = sb("oacc", [128, TG], F32)
        oT = sb("oT", [128, KC, TG], BF16)
        hres = sb("hres", [128, KC, TG], F32)
        ysb = sb("ysb", [128, KC, TG], F32)
        sqs = [sb(f"sqp{i}", [128, TG], BF16) for i in range(3)]
        rstd = sb("rstd", [128, TG], F32)
        tmp = sb("tmpn", [128, TG], F32)
        ps_stat = ps("ps_stat", [128, TG], F32)
        ps_y = [ps(f"ps_y{i}", [128, TG], F32) for i in range(2)]
        p = Prog(nc)
        for kc in range(KC):
            p.dma('pool', wo[:, kc, :], wo_d[kc * 128:(kc + 1) * 128, :], writes=['wo'], slot='wo')
        p.dma('sp', gains[:], g_d[:, :], writes=['gains'])
        p.op('dve', lambda e: e.memset(ones_bf[:], 1.0), writes=['ones'])
        p.op('dve', lambda e: e.memset(epsc[:], EPS), writes=['epsc'])
        if has_sink:
            p.dma('sp', sink[:], sk_d[:, :], writes=['sink'])
            p.op('act', lambda e: e.activation(out=sink[:], in_=sink[:], func=AF.Exp), reads=['sink'], writes=['sink'])
        n = dict(u=0, g=0)
        sqctr = [0]
        evs = []
        for tg in range(NTG):
            tsl = slice(tg * TG, (tg + 1) * TG)
            for c in range(KC):
                ui = 0
                for b, idxs in enumerate(branches):
                    for k, _ in enumerate(idxs):
                        u_ = ul[n['u'] % 4]
                        d_ = dl[n['u'] % 4]
                        n['u'] += 1
                        p.dma('sp', u_[:], u_d[ui, c, :, tsl], writes=[u_.name])
                        p.dma('sp', d_[:], d_d[ui, c, :, tsl], writes=[d_.name])
                        ui += 1
                        if k == 0:
                            p.op('dve', lambda e, u_=u_: e.tensor_copy(out=us[:], in_=u_[:]), reads=[u_.name], writes=['us'])
                            p.op('pool', lambda e, d_=d_: e.tensor_scalar(out=ds[:], in0=d_[:], scalar1=1e-30, scalar2=None, op0=ALU.add), reads=[d_.name], writes=['ds'])
                        else:
                            p.op('dve', lambda e, u_=u_: e.tensor_tensor(out=us[:], in0=us[:], in1=u_[:], op=ALU.add), reads=[u_.name, 'us'], writes=['us'])
                            p.op('pool', lambda e, d_=d_: e.tensor_tensor(out=ds[:], in0=ds[:], in1=d_[:], op=ALU.add), reads=[d_.name, 'ds'], writes=['ds'])
                    if has_sink:
                        p.op('pool', lambda e, c=c: e.tensor_scalar(out=ds[:], in0=ds[:], scalar1=sink[:, c:c + 1], scalar2=None, op0=ALU.add), reads=['ds', 'sink'], writes=['ds'])
                    p.op('dve', lambda e: e.reciprocal(out=ds[:], in_=ds[:]), reads=['ds'], writes=['ds'])
                    p.op('dve', lambda e: e.tensor_tensor(out=us[:], in0=us[:], in1=ds[:], op=ALU.mult), reads=['us', 'ds'], writes=['us'])
                    if has_gate:
                        g_ = gl[n['g'] % 2]
                        n['g'] += 1
                        p.dma('sp', g_[:], gt_d[b, c, :, tsl], writes=[g_.name])
                        p.op('act', lambda e, g_=g_: e.activation(out=sg[:], in_=g_[:], func=AF.Sigmoid), reads=[g_.name], writes=['sg'])
                        p.op('dve', lambda e: e.tensor_tensor(out=us[:], in0=us[:], in1=sg[:], op=ALU.mult), reads=['us', 'sg'], writes=['us'])
                    last = (b == nb - 1)
                    dst = oT[:, c, :] if (last and nb == 1) else oacc[:]
                    dkey = ('oT', c) if (last and nb == 1) else 'oacc'
                    if b == 0:
                        p.op('dve', lambda e, dst=dst: e.tensor_copy(out=dst, in_=us[:]), reads=['us'], writes=[dkey])
                    elif not last:
                        p.op('dve', lambda e: e.tensor_tensor(out=oacc[:], in0=oacc[:], in1=us[:], op=ALU.add), reads=['us', 'oacc'], writes=['oacc'])
                    else:
                        p.op('dve', lambda e, c=c: e.tensor_tensor(out=oT[:, c, :], in0=oacc[:], in1=us[:], op=ALU.add), reads=['us', 'oacc'], writes=[('oT', c)])
            p.dma('sp', hres[:], hT_d[:, :, tsl].rearrange("c p t -> p c t"), writes=[(hres.name, 0)])
            for oc in range(KC):
                py = ps_y[oc % 2]
                for c in range(KC):
                    p.op('pe', lambda e, py=py, c=c, oc=oc: e.matmul(py[:], lhsT=wo[:, c, oc * 128:(oc + 1) * 128], rhs=oT[:, c, :], start=(c == 0), stop=(c == KC - 1)),
                         reads=['wo', ('oT', c)], writes=[py.name])
                p.op('act', lambda e, oc=oc, py=py: e.activation(out=ysb[:, oc, :], in_=py[:], func=AF.Copy), reads=[py.name], writes=[('ysb', oc)])
            emit_postnorm_residual(p, nc, hres, ysb, gains[:, 0:KC], 0, ones_bf, ps_stat, sqs, rstd, tmp, sqctr, epsc)
            evs.append(p.dma('act', out_d[:, :, tsl].rearrange("c p t -> p c t"), hres[:], reads=[(hres.name, 0)], writes=[('out', tg)], slot='st_out'))
            p.readers.setdefault((hres.name, 0), []).append(evs[-1])
        p.finish_on('sp', [('dma', 'st_out', p.dma_cnt['st_out'])])
        p.emit()
    return nc


def build_cmp():
    NB = 512
    nc = bass.Bass("TRN2", target_bir_lowering=False)
    b_d = nc.dram_tensor("blocksT", [16, 128, NB], BF16, kind="ExternalInput").ap()
    pos_d = nc.dram_tensor("posT", [128, 16], F32, kind="ExternalInput").ap()
    w1_d = nc.dram_tensor("w1", [2048, 256], F32, kind="ExternalInput").ap()
    w2_d = nc.dram_tensor("w2", [256, 64], F32, kind="ExternalInput").ap()
    out_d = nc.dram_tensor("out", [64, NB], BF16, kind="ExternalOutput").ap()
    with contextlib.ExitStack() as st:
        sb = lambda name, shape, dt: st.enter_context(nc.sbuf_tensor(name, shape, dt))
        ps = lambda name, shape, dt: st.enter_context(nc.psum_tensor(name, shape, dt))
        bl = sb("bl", [128, 16, NB], BF16)
        posf = sb("posf", [128, 16], F32)
        posb = sb("posb", [128, 16], BF16)
        w1 = sb("w1_sb", [128, 16, 256], BF16)
        w2 = sb("w2_sb", [128, 2, 64], BF16)
        b1 = sb("b1", [128, 2], F32)
        g1 = sb("g1", [128, 2, NB], BF16)
        ob = sb("ob", [64, NB], BF16)
        ps_h = [ps(f"ps_h{i}", [128, NB], F32) for i in range(2)]
        ps_b = ps("ps_b", [128, 2], F32)
        ps_o = ps("ps_o", [64, NB], F32)
        p = Prog(nc)
        p.dma('sp', bl[:], b_d.rearrange("c p n -> p c n"), writes=['bl'])
        p.dma('sp', posf[:], pos_d[:, :], writes=['posf'])
        p.dma('pool', w1[:], w1_d.rearrange("(c p) n -> p c n", p=128), writes=['w1'])
        p.dma('pool', w2[:], w2_d.rearrange("(c p) n -> p c n", p=128), writes=['w2'])
        p.op('dve', lambda e: e.tensor_copy(out=posb[:], in_=posf[:]), reads=['posf'], writes=['posb'])
        for m in range(2):
            for c in range(16):
                p.op('pe', lambda e, m=m, c=c: e.matmul(ps_b[:, m:m + 1], lhsT=w1[:, c, m * 128:(m + 1) * 128], rhs=posb[:, c:c + 1], start=(c == 0), stop=(c == 15)),
                     reads=['w1', 'posb'], writes=['ps_b'])
        p.op('dve', lambda e: e.tensor_copy(out=b1[:], in_=ps_b[:]), reads=['ps_b'], writes=['b1'])
        for m in range(2):
            for c in range(16):
                p.op('pe', lambda e, m=m, c=c: e.matmul(ps_h[m][:], lhsT=w1[:, c, m * 128:(m + 1) * 128], rhs=bl[:, c, :], start=(c == 0), stop=(c == 15)),
                     reads=['w1', 'bl'], writes=[ps_h[m].name])
            p.op('act', lambda e, m=m: e.activation(out=g1[:, m, :], in_=ps_h[m][:], func=AF.Gelu_apprx_tanh, bias=b1[:, m:m + 1]),
                 reads=[ps_h[m].name, 'b1'], writes=[('g1', m)])
        for m in range(2):
            p.op('pe', lambda e, m=m: e.matmul(ps_o[:], lhsT=w2[:, m, :], rhs=g1[:, m, :], start=(m == 0), stop=(m == 1)), reads=['w2', ('g1', m)], writes=['ps_o'])
        p.op('act', lambda e: e.activation(out=ob[:], in_=ps_o[:], func=AF.Copy), reads=['ps_o'], writes=['ob'])
        ev = p.dma('sp', out_d[:, :], ob[:], reads=['ob'], writes=['out'])
        p.finish_on('sp', [ev])
        p.emit()
    return nc


import ml_dtypes
BF = ml_dtypes.bfloat16

DIL = (1, 4, 16)
VARIANTS = [("A1", 1, 128, 2), ("A2", 4, 128, 2), ("A3", 16, 128, 2), ("B", 1, 127, 2), ("CW", 1, 511, 5), ("CS", 1, 10 ** 9, 14)]
VBASE = {}
_o = 0
for _n, _s, _m, _k in VARIANTS:
    VBASE[_n] = _o
    _o += 128 * _k + 128
LTOT = _o
LC = 4224
_PROGS = {}


def _prog(key, fn):
    if key not in _PROGS:
        _PROGS[key] = fn()
    return _PROGS[key]


def _run(nc, maps):
    res = run_bass_kernel_spmd(nc, maps, core_ids=list(range(8)))
    return res.results


def _t5_bucket(d):
    d = np.maximum(d, 0)
    df = np.maximum(d, 1).astype(np.float32)
    large = 16 + (np.log(df / np.float32(16)) / np.float32(np.log(128.0)) * np.float32(16)).astype(np.int32)
    large = np.minimum(large, 31)
    return np.where(d < 16, d, large)


def _onehot():
    oh = np.zeros((33, LTOT), np.float32)
    for name, stride, maxd, noff in VARIANTS:
        L = 128 * noff + 128
        d = np.arange(L) - 127
        ok = (d >= 0) & (d <= maxd)
        b = _t5_bucket(np.clip(d, 0, None) * stride)
        rows = np.where(ok, b, 32)
        oh[rows, VBASE[name] + np.arange(L)] = 1.0
    return oh


def _to_cores(h):
    out = []
    for k in range(8):
        b, half = k // 2, k % 2
        own = h[b, half * 2048:(half + 1) * 2048]
        out.append(np.ascontiguousarray(own.T.reshape(8, 128, 2048)))
    return out


def _from_cores(lst, dtype=np.float32):
    ncol = lst[0].shape[0] * 128
    out = np.zeros((4, 4096, ncol), dtype)
    for k in range(8):
        b, half = k // 2, k % 2
        out[b, half * 2048:(half + 1) * 2048] = lst[k].reshape(ncol, 2048).T
    return out


def _gcol(g):
    return np.ascontiguousarray(g.reshape(8, 128).T.astype(np.float32))


def _lin(h, gain, w):
    nco = ((w.shape[1] + 127) // 128) * 128
    if nco != w.shape[1]:
        w = np.concatenate([w, np.zeros((w.shape[0], nco - w.shape[1]), np.float32)], axis=1)
    nc = _prog(("lin", nco), lambda: build_lin(nco))
    hT = _to_cores(h)
    w = np.ascontiguousarray(w)
    res = _run(nc, [dict(hT=hT[k], gains=_gcol(gain), w=w) for k in range(8)])
    return _from_cores([r["out"] for r in res], BF)


def _jmat():
    return np.ascontiguousarray(np.eye(128, dtype=np.float32)[::-1]).astype(BF)


def _hflag(half):
    return np.full((128, 128), float(half), np.float32).astype(BF)


def _oproj(key, branches, U, Dn, h, gain, w_o, gate=None, sink=None):
    nc = _prog(("oproj", key), lambda: build_oproj(branches, gate is not None, sink is not None))
    hT = _to_cores(h)
    maps = []
    for k in range(8):
        m = dict(U=U[k], Dn=Dn[k], w_o=np.ascontiguousarray(w_o), hT=hT[k], gains=_gcol(gain))
        if gate is not None:
            m['gate'] = gate[k]
        if sink is not None:
            m['sink'] = sink
        maps.append(m)
    res = _run(nc, maps)
    return _from_cores([r["out"] for r in res])


def _split_k(kT):
    out = np.zeros(kT.shape[:-1] + (2, kT.shape[-1]), kT.dtype)
    out[..., :64, 0, :] = kT[..., :64, :]
    out[..., 64:, 1, :] = kT[..., 64:, :]
    return out


def _sel(u):
    return np.ascontiguousarray(u.reshape(u.shape[0], 8, 128, 2048))


def _seld(d):
    npr = d.shape[0]
    t = d.reshape(npr, 8, 16, 2, 128).transpose(0, 1, 3, 2, 4)
    t = np.broadcast_to(t[:, :, :, None], (npr, 8, 2, 64, 16, 128))
    return np.ascontiguousarray(t.reshape(npr, 8, 128, 2048))


def _dup_kT(kk):
    t = np.ascontiguousarray(kk.T)
    return np.concatenate([t, t], axis=0)


def _v_tiles(vv128, nkt_pad):
    nkt = vv128.shape[0] // 128
    out = np.zeros((128, nkt_pad, 128), vv128.dtype)
    out[:, :nkt] = vv128.reshape(nkt, 128, 128).transpose(1, 0, 2)
    return out


def _mixer_A(h, gains, w_in, w_o, fvec):
    P = _lin(h, gains[0], w_in)
    qkv = P.reshape(4, 4096, 3, 3, 16, 64)
    plans, biases = [], []
    for gi, dil in enumerate(DIL):
        tpr = 1 + 16 // dil
        nq = 16 // dil

        def plan(i, tpr=tpr, nq=nq):
            r, jq = i // nq, i % nq
            return [(r * tpr + jq, 1, False, jq == 0), (r * tpr + jq + 1, 0, False, False)]
        plans.append(plan)
        vb = VBASE["A%d" % (gi + 1)]
        biases.append([('f', vb, 0), ('f', vb, 128)])
    cfg = dict(npr=3, nkch=8, nkt=32, nkeys=4096, kch=lambda c: c, plans=plans, bias=biases, ltot=LTOT)
    nc = _prog("attA", lambda: build_att(cfg))
    maps = []
    for k in range(8):
        b, half = k // 2, k % 2
        QT = np.zeros((3, 8, 128, 2048), BF)
        KT = np.zeros((3, 8, 128, 4096), BF)
        V = np.zeros((3, 8, 128, 32, 128), BF)
        for gi, dil in enumerate(DIL):
            L, Lo = 4096 // dil, 2048 // dil

            def strided(t):
                return t.reshape(L, dil, 16, 64).transpose(1, 0, 2, 3)
            q, kk, vv = (strided(qkv[b, :, gi, i]) for i in range(3))
            own = slice(half * Lo, (half + 1) * Lo)
            QT[gi] = q[:, own].reshape(2048, 8, 128).transpose(1, 2, 0)
            if half == 0:
                kh = np.zeros((dil, 128, 16, 64), BF)
                vh = np.zeros((dil, 128, 16, 64), BF)
            else:
                kh, vh = kk[:, Lo - 128:Lo], vv[:, Lo - 128:Lo]
            kc = np.concatenate([kh, kk[:, own]], axis=1)
            vc = np.concatenate([vh, vv[:, own]], axis=1)
            nkeys = dil * (128 + Lo)
            KT[gi, :, :, :nkeys] = kc.reshape(nkeys, 8, 128).transpose(1, 2, 0)
            nkt = nkeys // 128
            V[gi, :, :, :nkt] = vc.reshape(nkt, 128, 8, 128).transpose(2, 1, 0, 3)
        maps.append(dict(QT=QT, KT=_split_k(KT), V=V, hflag=_hflag(half), jmat=_jmat(), fvec=fvec))
    res = _run(nc, maps)
    U, Dn = [], []
    for k in range(8):
        u, d = _sel(res[k]["U"]), _seld(res[k]["Dn"])
        uu = np.zeros_like(u)
        dd = np.zeros_like(d)
        for gi, dil in enumerate(DIL):
            Lo = 2048 // dil
            uu[gi] = u[gi].reshape(8, 128, dil, Lo).transpose(0, 1, 3, 2).reshape(8, 128, 2048)
            dd[gi] = d[gi].reshape(8, 128, dil, Lo).transpose(0, 1, 3, 2).reshape(8, 128, 2048)
        U.append(uu)
        Dn.append(dd)
    return _oproj("A", [[0, 1, 2]], U, Dn, h, gains[1], w_o)


def _mixer_B(h, gains, w_in, sinks, w_o, fvec):
    P = _lin(h, gains[0], w_in)
    q = P[..., :1024]
    kx = P[..., 1024:1152].reshape(4, 4096, 2, 64)
    vx = P[..., 1152:1280].reshape(4, 4096, 2, 64)
    vb = VBASE["B"]
    cfg = dict(npr=1, nkch=2, nkt=17, nkeys=2176, kch=lambda c: c // 4,
               plans=[lambda i: [(i, 1, False, i == 0), (i + 1, 0, False, False)]],
               bias=[[('f', vb, 0), ('f', vb, 128)]], ltot=LTOT)
    nc = _prog("attB", lambda: build_att(cfg))
    maps = []
    for k in range(8):
        b, half = k // 2, k % 2
        S0 = half * 2048
        QT = np.ascontiguousarray(q[b, S0:S0 + 2048].reshape(2048, 8, 128).transpose(1, 2, 0))[None]
        KT = np.zeros((1, 2, 128, 2176), BF)
        V = np.zeros((1, 2, 128, 17, 128), BF)
        for g in range(2):
            if half == 0:
                kh, vh = np.zeros((128, 64), BF), np.zeros((128, 64), BF)
            else:
                kh, vh = kx[b, S0 - 128:S0, g], vx[b, S0 - 128:S0, g]
            kk = np.concatenate([kh, kx[b, S0:S0 + 2048, g]], axis=0)
            vv = np.concatenate([vh, vx[b, S0:S0 + 2048, g]], axis=0)
            KT[0, g] = _dup_kT(kk)
            V[0, g] = _v_tiles(np.concatenate([vv, vv], axis=1), 17)
        maps.append(dict(QT=QT, KT=_split_k(KT), V=V, hflag=_hflag(half), jmat=_jmat(), fvec=fvec))
    res = _run(nc, maps)
    U = [_sel(r["U"]) for r in res]
    Dn = [_seld(r["Dn"]) for r in res]
    sink = np.ascontiguousarray(np.repeat(sinks.reshape(8, 2), 64, axis=1).T.astype(np.float32))
    return _oproj("B", [[0]], U, Dn, h, gains[1], w_o, sink=sink)


def _mixer_C(h, gains, w_in, cmp_pos, cmp_w1, cmp_w2, w_o, fvec):
    P = _lin(h, gains[0], w_in)
    q = P[..., :1024]
    kv = P[..., 1024:1792].reshape(4, 4096, 6, 2, 64)
    gates = P[..., 1792:1840].reshape(4, 4096, 3, 16)
    nc = _prog("cmp", build_cmp)
    idx = np.arange(255)[:, None] * 16 + np.arange(32)[None, :]
    maps = []
    for k in range(8):
        b, i = k // 2, k % 2
        t = kv[b, :, i]
        blk = t[idx]
        blk = blk.transpose(2, 0, 1, 3).reshape(2, 255, 2048)
        full = np.zeros((2, 256, 2048), BF)
        full[:, :255] = blk
        blocksT = np.ascontiguousarray(full.reshape(512, 2048).T.reshape(16, 128, 512))
        posT = np.ascontiguousarray(cmp_pos[i].reshape(16, 128).T.astype(np.float32))
        maps.append(dict(blocksT=blocksT, posT=posT, w1=np.ascontiguousarray(cmp_w1[i]), w2=np.ascontiguousarray(cmp_w2[i])))
    res = _run(nc, maps)
    kcv = [[res[2 * b + i]["out"].reshape(64, 2, 256) for i in range(2)] for b in range(4)]
    cfg = dict(npr=1, nkch=2, nkt=2, nkeys=256, kch=lambda c: c // 4,
               plans=[lambda i: [(0, 2 * i, False, False), (1, 2 * i + 1, False, False)]],
               bias=[[('c', t, 128 * i) for i in range(16) for t in range(2)]], lc=LC, imp=True)
    nc = _prog("attCc", lambda: build_att(cfg))
    n_ = np.arange(256)
    j_ = np.arange(64)
    ovl = ((16 * n_[:, None] < (j_[None, :] + 1) * 64) & (16 * n_[:, None] + 32 > j_[None, :] * 64) & (n_[:, None] < 255)).astype(np.float32)
    ov65 = np.ones((128, 2, 65), np.float32)
    ov65[:, :, :64] = ovl.reshape(2, 128, 64).transpose(1, 0, 2)
    ov65 = ov65.astype(BF)
    maps = []
    QTs = []
    for k in range(8):
        b, half = k // 2, k % 2
        S0 = half * 2048
        QT = np.ascontiguousarray(q[b, S0:S0 + 2048].reshape(2048, 8, 128).transpose(1, 2, 0))
        QTs.append(QT)
        KT = np.zeros((1, 2, 128, 256), BF)
        V = np.zeros((1, 2, 128, 2, 128), BF)
        for g in range(2):
            kc = kcv[b][0][:, g, :]
            KT[0, g] = np.concatenate([kc, kc], axis=0)
            vc = kcv[b][1][:, g, :].T
            V[0, g] = _v_tiles(np.concatenate([vc, vc], axis=1), 2)
        cvec = np.zeros((2, LC), np.float32)
        for t in range(2):
            Z = 2048 * t + 2063 - S0
            cvec[t, :max(0, min(LC, Z))] = MASKV
        pos = S0 + np.arange(16)[None, :] * 128 + np.arange(128)[:, None]
        cur = pos // 64
        jj = np.arange(64)[None, None, :]
        forced = (jj == 0) | (jj == cur[..., None]) | (jj == cur[..., None] - 1)
        allowed = jj <= cur[..., None]
        selbias = np.where(forced, 1e9, np.where(allowed, 0.0, -1e30)).astype(np.float32)
        maps.append(dict(QT=QT[None], KT=_split_k(KT), V=V, hflag=_hflag(half), jmat=_jmat(), cvec=cvec, ov65=ov65, selbias=selbias))
    res = _run(nc, maps)
    Uc = [_sel(r["U"])[0] for r in res]
    Dc = [_seld(r["Dn"])[0] for r in res]
    negm = [r["negmask"] for r in res]
    sel_plan = lambda i: [(t, min(16 + i - t, 13), True, t < 16) for t in range(17 + i)]
    win_plan = lambda i: [(t, 4 + i - t, False, t < 4) for t in range(i, i + 5)]
    cfg = dict(npr=2, nkch=2, nkt=32, nkeys=4096, kch=lambda c: c // 4, plans=[sel_plan, win_plan],
               bias=[[('f', VBASE["CS"], 128 * o) for o in range(14)], [('f', VBASE["CW"], 128 * o) for o in range(5)]],
               ltot=LTOT, mask=True, sharedq=True)
    nc = _prog("attCs", lambda: build_att(cfg))
    maps = []
    for k in range(8):
        b, half = k // 2, k % 2
        S0 = half * 2048
        KT = np.zeros((2, 2, 128, 4096), BF)
        V = np.zeros((2, 2, 128, 32, 128), BF)
        for g in range(2):
            ks, vs, kw, vw = kv[b, :, 2, g], kv[b, :, 3, g], kv[b, :, 4, g], kv[b, :, 5, g]
            if half == 0:
                z = np.zeros((2048, 64), BF)
                kk, vv = np.concatenate([z, ks[:2048]], 0), np.concatenate([z, vs[:2048]], 0)
                kk2, vv2 = np.concatenate([z[:512], kw[:2048]], 0), np.concatenate([z[:512], vw[:2048]], 0)
            else:
                kk, vv = ks, vs
                kk2, vv2 = kw[S0 - 512:], vw[S0 - 512:]
            KT[0, g] = _dup_kT(kk)
            V[0, g] = _v_tiles(np.concatenate([vv, vv], axis=1), 32)
            KT[1, g, :, :2560] = _dup_kT(kk2)
            V[1, g] = _v_tiles(np.concatenate([vv2, vv2], axis=1), 32)
        u_ = np.arange(4096)
        tok = u_ - 2048 + S0
        E = np.zeros((64, 4096), np.float32)
        ok = tok >= 0
        E[tok[ok] // 64, u_[ok]] = 1.0
        nmT = np.ascontiguousarray(negm[k].transpose(2, 3, 1, 0).reshape(2, 64, 2048))
        maps.append(dict(QT=QTs[k], KT=_split_k(KT), V=V, hflag=_hflag(half), jmat=_jmat(), fvec=fvec, emat=E.astype(BF), nmT=nmT))
    res = _run(nc, maps)
    U, Dn, G = [], [], []
    for k in range(8):
        b, half = k // 2, k % 2
        S0 = half * 2048
        us_, ds_ = _sel(res[k]["U"]), _seld(res[k]["Dn"])
        U.append(np.ascontiguousarray(np.stack([Uc[k], us_[0], us_[1]])))
        Dn.append(np.ascontiguousarray(np.stack([Dc[k], ds_[0], ds_[1]])))
        gt = gates[b, S0:S0 + 2048]
        gr = np.repeat(gt.transpose(1, 2, 0), 64, axis=1)
        G.append(np.ascontiguousarray(gr.reshape(3, 8, 128, 2048)))
    return _oproj("C", [[0], [1], [2]], U, Dn, h, gains[1], w_o, gate=G)


def _ffn(h, layer, inp):
    nc = _prog("ffn", build_ffn)
    g = inp['norm_gains'][layer]
    gains = np.ascontiguousarray(np.stack([g[2].reshape(8, 128).T, g[3].reshape(8, 128).T], axis=1).reshape(128, 16).astype(np.float32))
    cwf = np.concatenate([inp['ffn_conv_w'][layer], inp['ffn_conv_b'][layer][None]], axis=0)
    cw = np.ascontiguousarray(cwf.reshape(4, 44, 128).transpose(2, 1, 0).astype(np.float32))
    hT = _to_cores(h)
    maps = []
    for k in range(8):
        b, half = k // 2, k % 2
        if half == 1:
            halo = np.ascontiguousarray(h[b, 2046:2048].T.reshape(8, 128, 2))
        else:
            halo = np.zeros((8, 128, 2), np.float32)
        maps.append(dict(hT=hT[k], halo=halo, gains=gains, w_up=np.ascontiguousarray(inp['ffn_w_up'][layer]),
                         conv_wb=cw, w_down=np.ascontiguousarray(inp['ffn_w_down'][layer])))
    res = _run(nc, maps)
    return _from_cores([r["out"] for r in res])


def kernel(**inputs):
    inp = {k: np.asarray(v) for k, v in inputs.items()}
    h = np.ascontiguousarray(inp['x'].astype(np.float32))
    nc = _prog("bias", lambda: build_bias(LTOT))
    res = _run(nc, [dict(table=np.ascontiguousarray(inp['rel_table'].astype(np.float32)), onehot=_onehot()) for _ in range(8)])
    fvec = res[0]["out"]
    for layer in range(4):
        kind, j = layer % 3, layer // 3
        g = inp['norm_gains'][layer]
        if kind == 0:
            h = _mixer_A(h, g, inp['a_w_in'][j], inp['a_w_o'][j], fvec)
        elif kind == 1:
            h = _mixer_B(h, g, inp['b_w_in'][j], inp['b_sinks'][j], inp['b_w_o'][j], fvec)
        else:
            h = _mixer_C(h, g, inp['c_w_in'][j], inp['c_cmp_pos'][j], inp['c_cmp_w1'][j], inp['c_cmp_w2'][j], inp['c_w_o'][j], fvec)
        h = _ffn(h, layer, inp)
    return h.astype(np.float32)
```
